# Optimizing a Trainium2 kernel written in Bass

```python
import math
import jax, jax.numpy as jnp
from jax import lax
import numpy as np

D_MODEL = 1024
BATCH = 16
SEQ = 2048
DEPTH = 1

HEAD_DIM = 64
N_HEADS_A = 8
N_HEADS_B = 8
WIDTH_A = N_HEADS_A * HEAD_DIM
WIDTH_B = N_HEADS_B * HEAD_DIM
MIX_WIDTH = WIDTH_A + WIDTH_B
DILATED_PATTERNS = ((128, 1), (512, 4), (2048, 16))
BLOCK = 128
N_EXPERTS = 32
TOP_K = 4
D_EXPERT = D_MODEL
SWIGLU_LIMIT = 7.0
SWIGLU_ALPHA = 1.702
PLE_DIM = 256
FORGET_BIAS_INIT = 2.0
NORM_EPS = 1e-6

IN_COLS = 3 * WIDTH_A + 3 * WIDTH_B + N_HEADS_B

kernel_name = "hybrid_dilated_fox_moe_ple_block"


def rms_norm(x, g):
    xf = x.astype(jnp.float32)
    y = xf * lax.rsqrt(jnp.mean(xf * xf, axis=-1, keepdims=True) + NORM_EPS)
    return (y * g.astype(jnp.float32)).astype(x.dtype)


def alibi_slopes(n_heads):
    return jnp.asarray(2.0 ** (-8.0 * np.arange(1, n_heads + 1) / n_heads), dtype=jnp.float32)


def dilated_window_attention(q, k, v, slopes, window, dilation):
    B, S, H, Dh = q.shape
    steps = window // dilation
    n = -(-S // (dilation * BLOCK)) * BLOCK
    sp = n * dilation
    nb = n // BLOCK
    pad = ((0, 0), (0, sp - S), (0, 0), (0, 0))

    def to_blocks(t):
        t = jnp.pad(t, pad).reshape(B, n, dilation, H, Dh).transpose(0, 2, 1, 3, 4)
        return t.reshape(B, dilation, nb, BLOCK, H, Dh)

    def with_prev(t):
        prev = jnp.pad(t[:, :, :-1], ((0, 0), (0, 0), (1, 0), (0, 0), (0, 0), (0, 0)))
        return jnp.concatenate([prev, t], axis=3)

    qb = to_blocks(q)
    kk = with_prev(to_blocks(k))
    vv = with_prev(to_blocks(v))

    s = jnp.einsum('brnqhd,brnkhd->brnhqk', qb, kk).astype(jnp.float32) * (Dh ** -0.5)
    qi = jnp.arange(BLOCK)[:, None]
    kj = jnp.arange(2 * BLOCK)[None, :]
    rel = qi + BLOCK - kj
    band = (rel >= 0) & (rel <= steps)
    has_key = (jnp.arange(nb)[:, None, None] > 0) | (kj[None] >= BLOCK)
    mask = band[None] & has_key
    alibi = -slopes[:, None, None] * (rel * dilation).astype(jnp.float32)[None]
    s = s + alibi[None, None, None]
    s = jnp.where(mask[None, None, :, None], s, -jnp.inf)
    lse = jax.nn.logsumexp(s, axis=-1)
    pr = jnp.exp(s - lse[..., None]).astype(v.dtype)
    o = jnp.einsum('brnhqk,brnkhd->brnqhd', pr, vv)

    o = o.reshape(B, dilation, n, H, Dh).transpose(0, 2, 1, 3, 4).reshape(B, sp, H, Dh)[:, :S]
    lse = lse.transpose(0, 1, 2, 4, 3).reshape(B, dilation, n, H).transpose(0, 2, 1, 3)
    lse = lse.reshape(B, sp, H)[:, :S]
    return o, lse


def longnet_mixture(q, k, v):
    slopes = alibi_slopes(q.shape[2])
    outs, lses = [], []
    for window, dilation in DILATED_PATTERNS:
        o, l = dilated_window_attention(q, k, v, slopes, window, dilation)
        outs.append(o)
        lses.append(l)
    w = jax.nn.softmax(jnp.stack(lses, axis=0), axis=0)
    o = jnp.sum(w[..., None] * jnp.stack(outs, axis=0).astype(jnp.float32), axis=0)
    return o.astype(q.dtype)


def forgetting_attention(q, k, v, log_f):
    B, S, H, Dh = q.shape
    c = jnp.cumsum(log_f.astype(jnp.float32), axis=1).transpose(0, 2, 1)
    kpos = jnp.arange(S)
    scale = Dh ** -0.5

    def one_block(i):
        start = i * BLOCK
        qb = lax.dynamic_slice_in_dim(q, start, BLOCK, axis=1)
        cq = lax.dynamic_slice_in_dim(c, start, BLOCK, axis=2)
        s = jnp.einsum('bqhd,bkhd->bhqk', qb, k).astype(jnp.float32) * scale
        s = s + cq[..., :, None] - c[..., None, :]
        qpos = start + jnp.arange(BLOCK)
        s = jnp.where(kpos[None, :] <= qpos[:, None], s, -jnp.inf)
        pr = jax.nn.softmax(s, axis=-1).astype(v.dtype)
        return jnp.einsum('bhqk,bkhd->bqhd', pr, v)

    o = lax.map(one_block, jnp.arange(S // BLOCK))
    return o.transpose(1, 0, 2, 3, 4).reshape(B, S, H, Dh)


def clamped_swiglu(h):
    x_glu = jnp.minimum(h[..., ::2], SWIGLU_LIMIT)
    x_lin = jnp.clip(h[..., 1::2], -SWIGLU_LIMIT, SWIGLU_LIMIT)
    return x_glu * jax.nn.sigmoid(SWIGLU_ALPHA * x_glu) * (x_lin + 1.0)


def moe_ffn(u, w_router, b_router, w_gate_up, b_gate_up, w_down, b_down):
    B, S, D = u.shape
    t = u.reshape(B * S, D)
    logits = (t @ w_router).astype(jnp.float32) + b_router.astype(jnp.float32)
    top_val, top_idx = lax.top_k(logits, TOP_K)
    gates = jax.nn.softmax(top_val, axis=-1)
    comb = jnp.sum(jax.nn.one_hot(top_idx, N_EXPERTS, dtype=jnp.float32) * gates[..., None], axis=1)
    y = jnp.zeros((B * S, D), jnp.float32)
    for e in range(N_EXPERTS):
        h = t @ w_gate_up[e] + b_gate_up[e]
        out = clamped_swiglu(h) @ w_down[e] + b_down[e]
        y = y + comb[:, e:e + 1] * out.astype(jnp.float32)
    return y.astype(u.dtype).reshape(B, S, D)


def setup_inputs(seed: int = 0) -> dict:
    key = jax.random.key(seed)
    ks = jax.random.split(key, 20)
    f32 = jnp.float32
    nrm = lambda k, shape, s: (jax.random.normal(k, shape, f32) * s).astype(f32)
    return {
        "x": nrm(ks[0], (BATCH, SEQ, D_MODEL), 1.0),
        "p": nrm(ks[1], (DEPTH, BATCH, SEQ, PLE_DIM), 1.0),
        "g_mix": 1.0 + nrm(ks[2], (DEPTH, D_MODEL), 0.02),
        "w_in": nrm(ks[3], (DEPTH, D_MODEL, IN_COLS), D_MODEL ** -0.5),
        "b_f": FORGET_BIAS_INIT + nrm(ks[4], (DEPTH, N_HEADS_B), 0.5),
        "g_qa": 1.0 + nrm(ks[5], (DEPTH, HEAD_DIM), 0.02),
        "g_ka": 1.0 + nrm(ks[6], (DEPTH, HEAD_DIM), 0.02),
        "g_qb": 1.0 + nrm(ks[7], (DEPTH, HEAD_DIM), 0.02),
        "g_kb": 1.0 + nrm(ks[8], (DEPTH, HEAD_DIM), 0.02),
        "w_o": nrm(ks[9], (DEPTH, MIX_WIDTH, D_MODEL), MIX_WIDTH ** -0.5),
        "g_ffn": 1.0 + nrm(ks[10], (DEPTH, D_MODEL), 0.02),
        "w_router": nrm(ks[11], (DEPTH, D_MODEL, N_EXPERTS), D_MODEL ** -0.5),
        "b_router": nrm(ks[12], (DEPTH, N_EXPERTS), 0.01),
        "w_gate_up": nrm(ks[13], (DEPTH, N_EXPERTS, D_MODEL, 2 * D_EXPERT), D_MODEL ** -0.5),
        "b_gate_up": nrm(ks[14], (DEPTH, N_EXPERTS, 2 * D_EXPERT), 0.01),
        "w_down": nrm(ks[15], (DEPTH, N_EXPERTS, D_EXPERT, D_MODEL), D_EXPERT ** -0.5),
        "b_down": nrm(ks[16], (DEPTH, N_EXPERTS, D_MODEL), 0.01),
        "g_ple": 1.0 + nrm(ks[17], (DEPTH, D_MODEL), 0.02),
        "w_ple_gate": nrm(ks[18], (DEPTH, D_MODEL, D_MODEL), D_MODEL ** -0.5),
        "w_ple_proj": nrm(ks[19], (DEPTH, PLE_DIM, D_MODEL), PLE_DIM ** -0.5),
    }


def reference(x, p, g_mix, w_in, b_f, g_qa, g_ka, g_qb, g_kb, w_o, g_ffn,
              w_router, b_router, w_gate_up, b_gate_up, w_down, b_down,
              g_ple, w_ple_gate, w_ple_proj):
    B, S, D = x.shape
    h = x
    for i in range(DEPTH):
        u = rms_norm(h, g_mix[i])
        z = u @ w_in[i]
        o0 = 0
        qa = z[..., o0:o0 + WIDTH_A].reshape(B, S, N_HEADS_A, HEAD_DIM); o0 += WIDTH_A
        ka = z[..., o0:o0 + WIDTH_A].reshape(B, S, N_HEADS_A, HEAD_DIM); o0 += WIDTH_A
        va = z[..., o0:o0 + WIDTH_A].reshape(B, S, N_HEADS_A, HEAD_DIM); o0 += WIDTH_A
        qb = z[..., o0:o0 + WIDTH_B].reshape(B, S, N_HEADS_B, HEAD_DIM); o0 += WIDTH_B
        kb = z[..., o0:o0 + WIDTH_B].reshape(B, S, N_HEADS_B, HEAD_DIM); o0 += WIDTH_B
        vb = z[..., o0:o0 + WIDTH_B].reshape(B, S, N_HEADS_B, HEAD_DIM); o0 += WIDTH_B
        f_logit = z[..., o0:o0 + N_HEADS_B].astype(jnp.float32) + b_f[i].astype(jnp.float32)

        out_a = longnet_mixture(rms_norm(qa, g_qa[i]), rms_norm(ka, g_ka[i]), va)
        out_b = forgetting_attention(rms_norm(qb, g_qb[i]), rms_norm(kb, g_kb[i]), vb,
                                     jax.nn.log_sigmoid(f_logit))
        mix = jnp.concatenate([out_a.reshape(B, S, WIDTH_A).astype(h.dtype),
                               out_b.reshape(B, S, WIDTH_B).astype(h.dtype)], axis=-1)
        h = h + mix @ w_o[i]

        h = h + moe_ffn(rms_norm(h, g_ffn[i]), w_router[i], b_router[i],
                        w_gate_up[i], b_gate_up[i], w_down[i], b_down[i])

        gate = jax.nn.sigmoid((rms_norm(h, g_ple[i]) @ w_ple_gate[i]).astype(jnp.float32))
        h = h + (gate * (p[i] @ w_ple_proj[i]).astype(jnp.float32)).astype(h.dtype)
    return h
```

```python
import math
from contextlib import ExitStack

import numpy as np
import ml_dtypes

import concourse.bass as bass
import concourse.mybir as mybir
from concourse.bass_utils import run_bass_kernel_spmd

F32 = mybir.dt.float32
BF16 = mybir.dt.bfloat16
I32 = mybir.dt.int32
ALU = mybir.AluOpType
AF = mybir.ActivationFunctionType
AX = mybir.AxisListType

NCORES = 8
D = 1024
SEQ = 2048
NSEQ = 2
NTOK = NSEQ * SEQ
NT = NTOK // 128
NE = 32
TOPK = 4
TS = 512
NMT = NTOK * TOPK // TS + NE
NSLOT = NMT * TS
EPS = 1e-6
NEG = -30000.0
XW = 2432
CBW = 896

DEBUG = False
import os
STAGE = os.environ.get('KSTAGE', '')


class StopBuild(Exception):
    pass


def checkpoint(name):
    if STAGE == name:
        raise StopBuild()


class Buf:
    __slots__ = ("name", "writers", "readers")

    def __init__(self, name):
        self.name = name
        self.writers = []
        self.readers = []


class Op:
    __slots__ = ("eng", "fn", "deps", "is_dma", "sem", "val", "signal", "extra_waits")

    def __init__(self, eng, fn):
        self.eng = eng
        self.fn = fn
        self.deps = []
        self.is_dma = False
        self.sem = None
        self.val = 0
        self.signal = False


class Prog:
    ENG = ["pe", "act", "dve", "pool", "sp"]
    SAME_ENGINE_SYNC = {"act", "dve", "pool"}

    def __init__(self, nc, stack):
        self.nc = nc
        self.stack = stack
        self.ops = {e: [] for e in self.ENG}
        self.esem = {e: stack.enter_context(nc.semaphore("sem_" + e)) for e in self.ENG}
        self.dma_sems = {}
        self.last = {e: None for e in self.ENG}

    def dma_sem(self, name):
        if name not in self.dma_sems:
            self.dma_sems[name] = [self.stack.enter_context(self.nc.semaphore("dq_" + name)), 0, None]
        return self.dma_sems[name]

    def add(self, eng, fn, reads=(), writes=(), accum=(), dma=None):
        op = Op(eng, fn)
        deps = []
        for b in reads:
            deps.extend(b.writers)
        for b in writes:
            deps.extend(b.writers)
            deps.extend(b.readers)
        for b in accum:
            deps.extend(b.readers)
        seen = set()
        for d in deps:
            if id(d) not in seen and d is not op:
                seen.add(id(d))
                op.deps.append(d)
        if dma is not None:
            rec = self.dma_sem(dma)
            rec[1] += 16
            rec[2] = op
            op.is_dma = True
            op.sem = rec[0]
            op.val = rec[1]
        for b in reads:
            b.readers.append(op)
        for b in writes:
            b.writers = [op]
            b.readers = []
        for b in accum:
            if b.writers and all((w.eng == eng and not w.is_dma and not op.is_dma) for w in b.writers):
                b.writers = [op]
            else:
                b.writers.append(op)
            b.readers = []
        self.ops[eng].append(op)
        if not op.is_dma:
            self.last[eng] = op
        return op

    def barrier(self):
        deps = [o for o in self.last.values() if o is not None]
        deps += [rec[2] for rec in self.dma_sems.values() if rec[2] is not None]
        for e in self.ENG:
            op = Op(e, None)
            op.deps = [d for d in deps]
            self.ops[e].append(op)

    def emit(self, block):
        for e in self.ENG:
            for op in self.ops[e]:
                for d in op.deps:
                    if d.is_dma:
                        continue
                    if d.eng == op.eng and not op.is_dma and op.fn is not None and d.eng not in self.SAME_ENGINE_SYNC:
                        continue
                    d.signal = True
        for e in self.ENG:
            cnt = 0
            for op in self.ops[e]:
                if not op.is_dma and op.signal:
                    cnt += 1
                    op.val = cnt
                    op.sem = self.esem[e]
        self.counts = {}

        def run(e, engobj):
            seen = {}
            n = 0
            for op in self.ops[e]:
                need = {}
                for d in op.deps:
                    if not d.is_dma and not d.signal:
                        continue
                    key = id(d.sem)
                    if d.val > need.get(key, (0, None))[0]:
                        need[key] = (d.val, d.sem)
                for key, (val, sem) in need.items():
                    if seen.get(key, 0) >= val:
                        continue
                    seen[key] = val
                    engobj.wait_ge(sem, val)
                    n += 1
                if op.fn is None:
                    continue
                ins = op.fn(engobj)
                n += 1
                if op.is_dma:
                    ins.then_inc(op.sem, 16)
                elif op.signal:
                    ins.then_inc(op.sem, 1)
            self.counts[e] = n

        @block.tensor
        def _(eng):
            run("pe", eng)

        @block.scalar
        def _(eng):
            run("act", eng)

        @block.vector
        def _(eng):
            run("dve", eng)

        @block.gpsimd
        def _(eng):
            run("pool", eng)

        @block.sync
        def _(eng):
            run("sp", eng)


class Arena:
    def __init__(self, ap, nwords):
        self.ap = ap
        self.n = nwords
        self.off = 0
        self.cnt = 0

    def mark(self):
        return self.off

    def reset(self, m=0):
        self.off = m

    def alloc(self, free_shape, dtype, name=None):
        esz = 4 if dtype in (F32, I32) else 2
        nel = int(np.prod(free_shape))
        n32 = (nel * esz + 3) // 4
        assert self.off + n32 <= self.n, f"arena overflow {name} {self.off}+{n32}>{self.n}"
        v = self.ap[:, self.off:self.off + n32]
        self.off += n32
        if dtype != F32:
            v = v.bitcast(dtype)
        if len(free_shape) == 2:
            v = v.rearrange("p (a b) -> p a b", a=free_shape[0])
        elif len(free_shape) == 3:
            v = v.rearrange("p (a b c) -> p a b c", a=free_shape[0], b=free_shape[1])
        self.cnt += 1
        return v, Buf(name or f"buf{self.cnt}")


def build_program(debug=False):
    nc = bass.Bass("TRN2", target_bir_lowering=False)

    def din(name, shape, dt=F32):
        return nc.dram_tensor(name, list(shape), dt, kind="ExternalInput").ap()

    x_d = din("x", [NTOK, D])
    p_d = din("p", [NTOK, 256])
    w_in_d = din("w_in", [D, 3080])
    w_o_d = din("w_o", [D, D])
    w_r_d = din("w_router", [D, NE])
    NEW = NE if STAGE in ('', 'full') else 1
    wgu_d = din("wgu", [NEW * 128 * 4, 4096])
    wd_d = din("wd", [NEW * 128 * 2, 4096])
    bgu_d = din("bgu", [NE * 128, 16])
    bd_d = din("b_down", [NE, D])
    wpg_d = din("w_ple_gate", [D, D])
    wpp_d = din("w_ple_proj", [256, D])
    gvec_d = din("gvec", [3, D])
    gvecT_d = din("gvecT", [128, 24])
    gqk_d = din("gqk", [128, 4])
    nbf_d = din("bf", [8, 1])
    br_d = din("b_router", [1, NE])
    ident_d = din("ident", [128, 128])
    ustrict_d = din("ustrict", [128, 128])
    blk64_d = din("blk64", [128, 128])
    btabA_d = din("btabA", [8, 128, XW])
    cbtab_d = din("cbtab", [128, CBW])
    kpos_d = din("kpos", [128, 16])
    sel8_d = din("sel8", [8, 8 * 128])
    tokid_d = din("tokid", [128, NT], I32)
    misc_d = din("misc", [128, 128])
    out_d = nc.dram_tensor("out", [NTOK, D], F32, kind="ExternalOutput").ap()

    h1_scr = nc.dram_tensor("h1_scr", [NTOK, D], F32, kind="Internal").ap()
    u2_scr = nc.dram_tensor("u2_scr", [NTOK, D], BF16, kind="Internal").ap()
    xs_scr = nc.dram_tensor("xs_scr", [NSLOT, D], BF16, kind="Internal").ap()
    y_scr = nc.dram_tensor("y_scr", [NSLOT, D], F32, kind="Internal").ap()
    dbg = {}
    if debug:
        dbg["mix"] = nc.dram_tensor("dbg_mix", [NSEQ, 128, 8 * SEQ], BF16, kind="ExternalOutput").ap()
        dbg["logits"] = nc.dram_tensor("dbg_logits", [128, NT * NE], F32, kind="ExternalOutput").ap()
        dbg["route"] = nc.dram_tensor("dbg_route", [128, 1024], F32, kind="ExternalOutput").ap()
        dbg["h1"] = h1_scr
    stack = ExitStack()
    with stack:
        print("sbuf bytes remaining", nc.sbuf_bytes_remaining)
        ARW = 47 * 1024
        CAW = 4608
        arena_t = stack.enter_context(nc.sbuf_tensor("arena", [128, ARW], F32))
        const_t = stack.enter_context(nc.sbuf_tensor("consts", [128, CAW], F32))
        AR = Arena(arena_t[:, :], ARW)
        CA = Arena(const_t[:, :], CAW)
        psum = []
        for i in range(8):
            t = stack.enter_context(nc.psum_tensor(f"ps{i}", [128, 512], F32))
            psum.append((t[:, :], Buf(f"ps{i}")))
        P = Prog(nc, stack)
        dram_u2 = Buf("u2_scr")
        dram_h1 = Buf("h1_scr")
        dram_out = Buf("out")

        def load_const(src_ap, free_shape, dtype, name, eng="sp"):
            v, b = CA.alloc(free_shape, dtype, name)
            P.add(eng, lambda e, v=v, s=src_ap: e.dma_start(out=v, in_=s), writes=[b], dma="c_" + name)
            return v, b

        ident_f, ident_f_b = load_const(ident_d, [128], F32, "ident_f")
        ident_b, ident_b_b = load_const(ident_d, [128], BF16, "ident_b", eng="pool")
        ustrict, ustrict_b = load_const(ustrict_d, [128], BF16, "ustrict", eng="pool")
        blk64, blk64_b = load_const(blk64_d, [128], BF16, "blk64", eng="pool")
        ones_b, ones_b_b = CA.alloc([128], BF16, "ones_b")
        P.add("dve", lambda e: e.memset(ones_b, 1.0), writes=[ones_b_b])
        ccol, ccol_b = CA.alloc([4], F32, "ccol")
        P.add("dve", lambda e: e.memset(ccol[:, 0:1], 64.0 * EPS), writes=[ccol_b])
        P.add("dve", lambda e: e.memset(ccol[:, 1:2], 1.0), writes=[ccol_b])
        P.add("dve", lambda e: e.memset(ccol[:, 2:3], EPS), writes=[ccol_b])
        gT, gT_b = load_const(gvecT_d, [3, 8], F32, "gT")
        gqk, gqk_b = load_const(gqk_d, [4], F32, "gqk")
        P.add("dve", lambda e: e.tensor_scalar(out=gqk[:, 1:2], in0=gqk[:, 1:2], scalar1=8.0, scalar2=None, op0=ALU.mult), writes=[gqk_b])
        P.add("dve", lambda e: e.tensor_scalar(out=gqk[:, 3:4], in0=gqk[:, 3:4], scalar1=8.0, scalar2=None, op0=ALU.mult), writes=[gqk_b])
        nbf, nbf_b = CA.alloc([1], F32, "nbf")
        P.add("sp", lambda e: e.dma_start(out=nbf[0:8, :], in_=nbf_d), writes=[nbf_b], dma="c_nbf")
        P.add("dve", lambda e: e.tensor_scalar(out=nbf[0:8, :], in0=nbf[0:8, :], scalar1=-1.0, scalar2=None, op0=ALU.mult), writes=[nbf_b])
        kpos, kpos_b = load_const(kpos_d, [16], F32, "kpos")
        misc, misc_b = load_const(misc_d, [128], F32, "misc")
        tokid, tokid_b = load_const(tokid_d, [NT], I32, "tokid")


        zt, zt_b = CA.alloc([2048], BF16, "zt")
        P.add("pool", lambda e: e.memset(zt, 0.0), writes=[zt_b])
        dram_xs0 = Buf("xs_zero")
        xs_flat = xs_scr.rearrange("(p a) d -> p (a d)", p=128)
        logits_all, logits_b = CA.alloc([NT, NE], F32, "logits_all")

        uT, uT_b = AR.alloc([8, SEQ], BF16, "uT")
        mixT, mixT_b = AR.alloc([8, SEQ], BF16, "mixT")
        wr_sb, wr_b = AR.alloc([8, NE], F32, "w_r")
        P.add("sp", lambda e: e.dma_start(out=wr_sb, in_=w_r_d.rearrange("(c p) n -> p c n", p=128)), writes=[wr_b], dma="wr")
        brb, brb_b = AR.alloc([NE], F32, "brb")
        P.add("sp", lambda e: e.dma_start(out=brb, in_=br_d.partition_broadcast(128)), writes=[brb_b], dma="brb")
        cbtab, cbtab_b = AR.alloc([CBW], F32, "cbtab")
        P.add("sp", lambda e: e.dma_start(out=cbtab, in_=cbtab_d), writes=[cbtab_b], dma="cbtab")
        sel8, sel8_b = AR.alloc([8 * 128], F32, "sel8")
        P.add("sp", lambda e: e.dma_start(out=sel8[0:8, :], in_=sel8_d), writes=[sel8_b], dma="sel8")
        onespad, onespad_b = AR.alloc([2, 128], BF16, "onespad")
        P.add("pool", lambda e: e.memset(onespad, 0.0), writes=[onespad_b])
        P.add("pool", lambda e: e.memset(onespad[:, 0, 0:64], 1.0), writes=[onespad_b])
        P.add("pool", lambda e: e.memset(onespad[:, 1, 64:128], 1.0), writes=[onespad_b])
        wf_sb, wf_b = AR.alloc([8, 8], BF16, "wf")
        P.add("pool", lambda e: e.dma_start(out=wf_sb, in_=w_in_d.rearrange("(c p) n -> p c n", p=128)[:, :, 3072:3080]), writes=[wf_b], dma="wf")
        csum, csum_b = AR.alloc([SEQ], F32, "csum")
        ones8, ones8_b = AR.alloc([512], F32, "ones8")
        P.add("pool", lambda e: e.memset(ones8, 1.0), writes=[ones8_b])
        nck, nck_b = AR.alloc([16, 8], F32, "nck")
        NR = 5
        sq_t = [AR.alloc([512], BF16, f"sq{i}") for i in range(2)]
        rs_t = [AR.alloc([512], F32, f"rs{i}") for i in range(2)]
        tmp_t = [AR.alloc([512], F32, f"tmp{i}") for i in range(NR)]
        pt_t = [AR.alloc([512], BF16, f"pt{i}") for i in range(NR)]
        rden_t = [AR.alloc([512], F32, f"rden{i}") for i in range(1)]
        st_t = [AR.alloc([4], F32, f"st{i}") for i in range(4)]
        fl_t, fl_b = AR.alloc([2, 512], F32, "fl")
        x_mark = AR.mark()
        xt_t = [AR.alloc([D], F32, f"xt{i}") for i in range(2)]
        xn_t = [AR.alloc([D], BF16, f"xn{i}") for i in range(2)]
        junk, junk_b = AR.alloc([D], BF16, "junk")
        wo_sb, wo_b = AR.alloc([8, D], BF16, "w_o")
        gffn_b, gffn_bb = AR.alloc([D], F32, "gffn_b")
        h1_t = [AR.alloc([D], F32, f"h1t{i}") for i in range(2)]
        u2f_t = [AR.alloc([D], F32, f"u2f{i}") for i in range(2)]
        u2b_t = [AR.alloc([D], BF16, f"u2b{i}") for i in range(2)]
        u2T_t = [AR.alloc([8, 128], F32, f"u2T{i}") for i in range(2)]
        x_end1 = AR.mark()
        AR.reset(x_mark)
        btab = [AR.alloc([XW], F32, f"btab{i}") for i in range(2)]
        NPB = 2
        wq_sb = [AR.alloc([3, 8, 128], BF16, f"wqkv{i}") for i in range(NPB)]
        qT = [AR.alloc([SEQ], BF16, f"qT{i}") for i in range(NPB)]
        kT = [AR.alloc([SEQ], BF16, f"kT{i}") for i in range(NPB)]
        vpad = [AR.alloc([2, 16, 128], BF16, f"vpad{i}") for i in range(NPB)]
        AR.reset(max(x_end1, AR.mark()))

        PS_TR = [psum[0], psum[1]]
        PS_PJ = [psum[2], psum[3]]
        PS_S = [psum[4], psum[5]]
        PS_S4 = [psum[4], psum[5], psum[0], psum[1]]
        tmp4 = tmp_t + [(fl_t[:, 0, :], fl_b)]
        pt4 = pt_t + [(fl_t[:, 1, 0:256].bitcast(BF16), fl_b)]
        NB4 = len(tmp4)
        PS_NUM = psum[6]
        PS_DEN = psum[7]
        ctr = {"pj": 0, "s": 0, "tmp": 0, "sq": 0, "st": 0, "x": 0, "h1": 0}

        def rot(key, lst):
            i = ctr[key]
            ctr[key] = i + 1
            return lst[i % len(lst)]

        def rms_stats(src, src_b, st, st_b):
            P.add("act", lambda e: e.activation(out=junk, in_=src, func=AF.Square, accum_out=st[:, 0:1]),
                  reads=[src_b], writes=[junk_b, st_b])
            P.add("act", lambda e: e.activation(out=st[:, 1:2], in_=st[:, 0:1], func=AF.Ln, bias=ccol[:, 2:3], scale=1.0 / D),
                  reads=[ccol_b], writes=[st_b])
            P.add("act", lambda e: e.activation(out=st[:, 2:3], in_=st[:, 1:2], func=AF.Exp, scale=-0.5),
                  reads=[], writes=[st_b])

        def transpose8(src, src_b, ident, ident_buf, nchunks=8):
            for half in range((nchunks + 3) // 4):
                pv, pb = PS_TR[half]
                for c in range(4):
                    cc = half * 4 + c
                    if cc >= nchunks:
                        break
                    kw = dict(writes=[pb]) if c == 0 else dict(accum=[pb])
                    P.add("pe", lambda e, pv=pv, c=c, cc=cc: e.matmul(pv[:, c * 128:(c + 1) * 128], lhsT=src[:, cc * 128:(cc + 1) * 128], rhs=ident, start=True, stop=True),
                          reads=[src_b, ident_buf], **kw)

        def phase_a(s):
            tok0 = s * SEQ
            for i in range(16):
                xt, xt_b = rot("x", xt_t)
                xn, xn_b = xn_t[(ctr["x"] - 1) % 2]
                st, st_b = rot("st", st_t)
                r0 = tok0 + i * 128
                P.add("sp", lambda e, xt=xt, r0=r0: e.dma_start(out=xt, in_=x_d[r0:r0 + 128, :]), writes=[xt_b], dma=f"x{(ctr['x'] - 1) % 2}")
                rms_stats(xt, xt_b, st, st_b)
                P.add("act", lambda e, xn=xn, xt=xt, st=st: e.activation(out=xn, in_=xt, func=AF.Copy, scale=st[:, 2:3]),
                      reads=[xt_b, st_b], writes=[xn_b])
                transpose8(xn, xn_b, ident_b, ident_b_b)
                for half in range(2):
                    pv, pb = PS_TR[half]
                    P.add("dve", lambda e, pv=pv, half=half, i=i: e.tensor_tensor(
                        out=uT[:, half * 4:(half + 1) * 4, i * 128:(i + 1) * 128],
                        in0=pv.rearrange("p (c t) -> p c t", c=4),
                        in1=gT[:, 0, half * 4:(half + 1) * 4].unsqueeze(2).to_broadcast([128, 4, 128]), op=ALU.mult),
                        reads=[pb, gT_b], accum=[uT_b])
            checkpoint('A0')
            P.barrier()
            if s == 0:
                for c in range(NSLOT * D // 128 // 2048):
                    P.add("sp", lambda e, c=c: e.dma_start(out=xs_flat[:, c * 2048:(c + 1) * 2048], in_=zt), reads=[zt_b], accum=[dram_xs0], dma="xszero")
            for i in range(NPB):
                P.add("pool", lambda e, i=i: e.memset(vpad[i][0], 1.0), writes=[vpad[i][1]])
            for g in range(4):
                pv, pb = rot("pj", PS_PJ)
                for kc in range(8):
                    kw = dict(writes=[pb]) if kc == 0 else dict(accum=[pb])
                    P.add("pe", lambda e, pv=pv, kc=kc, g=g: e.matmul(pv[0:8, :], lhsT=wf_sb[:, kc, :], rhs=uT[:, kc, g * 512:(g + 1) * 512], start=(kc == 0), stop=(kc == 7)),
                          reads=[wf_b, uT_b], **kw)
                P.add("act", lambda e, pv=pv: e.activation(out=fl_t[0:8, 0, :], in_=pv[0:8, :], func=AF.Exp, bias=nbf[0:8, :], scale=-1.0),
                      reads=[pb, nbf_b], writes=[fl_b])
                P.add("act", lambda e: e.activation(out=fl_t[0:8, 1, :], in_=fl_t[0:8, 0, :], func=AF.Ln, bias=ccol[0:8, 1:2], scale=1.0),
                      reads=[fl_b, ccol_b], writes=[fl_b])
                if g == 0:
                    P.add("dve", lambda e: e.tensor_tensor_scan(out=csum[0:8, 0:512], data0=ones8[0:8, 0:512], data1=fl_t[0:8, 1, :], initial=0.0, op0=ALU.mult, op1=ALU.subtract),
                          reads=[fl_b, ones8_b], writes=[csum_b])
                else:
                    P.add("dve", lambda e, g=g: e.tensor_tensor_scan(out=csum[0:8, g * 512:(g + 1) * 512], data0=ones8[0:8, 0:512], data1=fl_t[0:8, 1, :],
                                                                initial=csum[0:8, g * 512 - 1:g * 512], op0=ALU.mult, op1=ALU.subtract),
                          reads=[fl_b, ones8_b, csum_b], writes=[csum_b])
            pv, pb = rot("pj", PS_PJ)
            for j in range(16):
                kw = dict(writes=[pb]) if j == 0 else dict(accum=[pb])
                P.add("pe", lambda e, pv=pv, j=j: e.matmul(pv[:, j * 8:(j + 1) * 8], lhsT=csum[0:8, j * 128:(j + 1) * 128], rhs=ident_f[0:8, 0:8], start=True, stop=True),
                      reads=[csum_b, ident_f_b], **kw)
            P.add("act", lambda e, pv=pv: e.activation(out=nck.rearrange("p a b -> p (a b)"), in_=pv[:, 0:128], func=AF.Copy, scale=-1.0),
                  reads=[pb], writes=[nck_b])

            checkpoint('fox')
            w_in_v = w_in_d.rearrange("(c p) n -> p c n", p=128)

            def proj_chunks(hp):
                isA_ = hp < 4
                hpl_ = hp % 4
                base_ = 0 if isA_ else 1536
                wq, wq_b = wq_sb[hp % NPB]
                q_sb_, q_b_ = qT[hp % NPB]
                k_sb_, k_b_ = kT[hp % NPB]
                v_sb_, v_b_ = vpad[hp % NPB]
                chunks = []

                def c_load():
                    for t in range(3):
                        c0 = base_ + t * 512 + hpl_ * 128
                        P.add("pool", lambda e, t=t, c0=c0: e.dma_start(out=wq[:, t], in_=w_in_v[:, :, c0:c0 + 128]),
                              **(dict(writes=[wq_b]) if t == 0 else dict(accum=[wq_b])), dma=f"wqkv{hp % NPB}")
                chunks.append(c_load)

                def mk_qk(t, dst, dst_b, gcol, g):
                    def c_qk():
                        pv, pb = rot("pj", PS_PJ)
                        for kc in range(8):
                            kw = dict(writes=[pb]) if kc == 0 else dict(accum=[pb])
                            P.add("pe", lambda e, pv=pv, kc=kc: e.matmul(pv, lhsT=wq[:, t, kc, :], rhs=uT[:, kc, g * 512:(g + 1) * 512], start=(kc == 0), stop=(kc == 7)),
                                  reads=[wq_b, uT_b], **kw)
                        sq, sq_b = rot("sq", sq_t)
                        rs, rs_b = rs_t[(ctr["sq"] - 1) % 2]
                        P.add("act", lambda e, pv=pv, sq=sq: e.activation(out=sq, in_=pv, func=AF.Square), reads=[pb], writes=[sq_b])
                        pv2, pb2 = rot("pj", PS_PJ)
                        P.add("pe", lambda e, pv2=pv2, sq=sq: e.matmul(pv2, lhsT=blk64, rhs=sq, start=True, stop=True), reads=[sq_b, blk64_b], writes=[pb2])
                        P.add("act", lambda e, pv2=pv2, rs=rs: e.activation(out=rs, in_=pv2, func=AF.Ln, bias=ccol[:, 0:1], scale=1.0),
                              reads=[pb2, ccol_b], writes=[rs_b])
                        P.add("act", lambda e, rs=rs: e.activation(out=rs, in_=rs, func=AF.Exp, scale=-0.5),
                              reads=[rs_b], writes=[rs_b])
                        P.add("dve", lambda e, pv=pv, rs=rs: e.scalar_tensor_tensor(
                            out=dst[:, g * 512:(g + 1) * 512], in0=pv, scalar=gqk[:, gcol:gcol + 1], in1=rs, op0=ALU.mult, op1=ALU.mult),
                            reads=[pb, rs_b, gqk_b], accum=[dst_b])
                    return c_qk
                for t, (dst, dst_b, gcol) in enumerate([(q_sb_, q_b_, 0 if isA_ else 2), (k_sb_, k_b_, 1 if isA_ else 3)]):
                    for g in range(4):
                        chunks.append(mk_qk(t, dst, dst_b, gcol, g))

                def mk_v(g):
                    def c_v():
                        pv, pb = rot("pj", PS_PJ)
                        for tt in range(4):
                            j = g * 4 + tt
                            for kc in range(8):
                                kw = dict(writes=[pb]) if (tt == 0 and kc == 0) else dict(accum=[pb])
                                P.add("pe", lambda e, pv=pv, kc=kc, j=j, tt=tt: e.matmul(pv[:, tt * 128:(tt + 1) * 128], lhsT=uT[:, kc, j * 128:(j + 1) * 128], rhs=wq[:, 2, kc, :], start=(kc == 0), stop=(kc == 7)),
                                      reads=[wq_b, uT_b], **kw)
                        pvv = pv.rearrange("p (t c) -> p t c", t=4)
                        P.add("act", lambda e, pvv=pvv: e.activation(out=v_sb_[:, 0, g * 4:(g + 1) * 4, 0:64], in_=pvv[:, :, 0:64], func=AF.Copy),
                              reads=[pb, v_b_], accum=[v_b_])
                        P.add("dve", lambda e, pvv=pvv: e.tensor_copy(out=v_sb_[:, 1, g * 4:(g + 1) * 4, 64:128], in_=pvv[:, :, 64:128]),
                              reads=[pb, v_b_], accum=[v_b_])
                    return c_v
                for g in range(4):
                    chunks.append(mk_v(g))
                return chunks

            for c_ in proj_chunks(0):
                c_()
            for hp in range(8):
                isA = hp < 4
                hpl = hp % 4
                q_sb, q_b = qT[hp % NPB]
                k_sb, k_b = kT[hp % NPB]
                v_sb, v_b = vpad[hp % NPB]
                next_chunks = proj_chunks(hp + 1) if hp + 1 < 8 else []
                bt = []
                for hh in range(2):
                    h = hpl * 2 + hh
                    tb, tb_b = btab[hh]
                    if isA:
                        P.add("sp", lambda e, tb=tb, h=h: e.dma_start(out=tb, in_=btabA_d[h]), writes=[tb_b], dma=f"btab{hh}")
                    else:
                        for g in range(4):
                            pv, pb = rot("pj", PS_PJ)
                            P.add("pe", lambda e, pv=pv, h=h, g=g: e.matmul(pv, lhsT=sel8[0:8, h * 128:(h + 1) * 128], rhs=csum[0:8, g * 512:(g + 1) * 512], start=True, stop=True),
                                  reads=[sel8_b, csum_b], writes=[pb])
                            P.add("act", lambda e, pv=pv, tb=tb, g=g: e.activation(out=tb[:, g * 512:(g + 1) * 512], in_=pv, func=AF.Copy),
                                  reads=[pb], **(dict(writes=[tb_b]) if g == 0 else dict(accum=[tb_b])))
                    bt.append((tb, tb_b))
                LOOK = 5
                tiles = [(qg, j, hh) for qg in range(4) for j in range(4 * qg + 4) for hh in range(2)]
                numv, numb = PS_NUM
                denv, denb = PS_DEN
                pend = {}

                def emit_score(n):
                    qg, j, hh = tiles[n]
                    h = hpl * 2 + hh
                    tb, tb_b = bt[hh]
                    sv, sb = PS_S4[n % len(PS_S4)]
                    lo, hi = hh * 64, hh * 64 + 64
                    P.add("pe", lambda e, sv=sv, lo=lo, hi=hi, j=j, qg=qg, k_sb=k_sb, q_sb=q_sb: e.matmul(
                        sv, lhsT=k_sb[lo:hi, j * 128:(j + 1) * 128], rhs=q_sb[lo:hi, qg * 512:(qg + 1) * 512], start=True, stop=True),
                        reads=[k_b, q_b], writes=[sb])
                    tm, tm_b = tmp4[n % NB4]
                    pt, pt_b = pt4[n % NB4]
                    T = 512 * qg - 128 * j
                    if isA:
                        x0 = T + 384
                        P.add("dve", lambda e, tm=tm, sv=sv, tb=tb, x0=x0: e.tensor_tensor(out=tm, in0=sv, in1=tb[:, x0:x0 + 512], op=ALU.add),
                              reads=[sb, tb_b], writes=[tm_b])
                        P.add("act", lambda e, tm=tm, pt=pt: e.activation(out=pt, in_=tm, func=AF.Exp), reads=[tm_b], writes=[pt_b])
                    else:
                        P.add("dve", lambda e, tm=tm, sv=sv, tb=tb, qg=qg: e.tensor_tensor(out=tm, in0=sv, in1=tb[:, qg * 512:(qg + 1) * 512], op=ALU.add),
                              reads=[sb, tb_b], writes=[tm_b])
                        if T <= 0:
                            x0 = T + 384
                            P.add("pool", lambda e, tm=tm, x0=x0: e.tensor_tensor(out=tm, in0=tm, in1=cbtab[:, x0:x0 + 512], op=ALU.add),
                                  reads=[tm_b, cbtab_b], writes=[tm_b])
                        P.add("act", lambda e, tm=tm, pt=pt, j=j, h=h: e.activation(out=pt, in_=tm, func=AF.Exp, bias=nck[:, j, h:h + 1]),
                              reads=[tm_b, nck_b], writes=[pt_b])
                    pend[n] = (pt, pt_b)

                def emit_pv(n):
                    qg, j, hh = tiles[n]
                    pt, pt_b = pend.pop(n)
                    first = (j == 0)
                    last = (j == 4 * qg + 3)
                    accv, accb = (numv, numb) if hh == 0 else (denv, denb)
                    P.add("pe", lambda e, pt=pt, hh=hh, j=j, first=first, last=last, v_sb=v_sb, accv=accv: e.matmul(accv, lhsT=v_sb[:, hh, j, :], rhs=pt, start=first, stop=last),
                          reads=[pt_b, v_b], **(dict(writes=[accb]) if first else dict(accum=[accb])))
                    if last:
                        rd, rd_b = rden_t[0]
                        nlo, nhi = hh * 64, hh * 64 + 64
                        dlo, dhi = (1 - hh) * 64, (1 - hh) * 64 + 64
                        P.add("dve", lambda e, rd=rd, accv=accv, nlo=nlo, nhi=nhi, dlo=dlo, dhi=dhi: e.reciprocal(out=rd[nlo:nhi, :], in_=accv[dlo:dhi, :]),
                              reads=[accb], writes=[rd_b])
                        P.add("dve", lambda e, rd=rd, qg=qg, hp=hp, accv=accv, nlo=nlo, nhi=nhi: e.tensor_tensor(
                            out=mixT[nlo:nhi, hp, qg * 512:(qg + 1) * 512], in0=accv[nlo:nhi, :], in1=rd[nlo:nhi, :], op=ALU.mult),
                              reads=[accb, rd_b], accum=[mixT_b])

                step = max(1, (len(tiles) - 8) // max(1, len(next_chunks)))
                for n in range(len(tiles) + LOOK):
                    if n < len(tiles):
                        emit_score(n)
                    if n - LOOK >= 0:
                        emit_pv(n - LOOK)
                    if next_chunks and n >= 4 and (n - 4) % step == 0:
                        next_chunks.pop(0)()
                while next_chunks:
                    next_chunks.pop(0)()
            checkpoint('A1')
            if debug:
                P.add("sp", lambda e, s=s: e.dma_start(out=dbg["mix"][s], in_=mixT.rearrange("p c t -> p (c t)")), reads=[mixT_b], dma="dbgmix")
            P.barrier()
            P.add("pool", lambda e: e.dma_start(out=wo_sb, in_=w_o_d.rearrange("(c p) n -> p c n", p=128)), writes=[wo_b], dma="wo")
            P.add("sp", lambda e: e.dma_start(out=gffn_b, in_=gvec_d[1:2, :].partition_broadcast(128)), writes=[gffn_bb], dma="gffnb")
            for i in range(16):
                ti = s * 16 + i
                r0 = tok0 + i * 128
                xt, xt_b = rot("x", xt_t)
                P.add("sp", lambda e, xt=xt, r0=r0: e.dma_start(out=xt, in_=x_d[r0:r0 + 128, :]), writes=[xt_b], dma=f"x{(ctr['x'] - 1) % 2}")
                h1, h1_b = rot("h1", h1_t)
                hi_ = (ctr["h1"] - 1) % 2
                u2f, u2f_b = u2f_t[hi_]
                u2b, u2b_b = u2b_t[hi_]
                u2T, u2T_b = u2T_t[hi_]
                st, st_b = rot("st", st_t)
                for dh in range(2):
                    pv, pb = rot("pj", PS_PJ)
                    for c in range(8):
                        kw = dict(writes=[pb]) if c == 0 else dict(accum=[pb])
                        P.add("pe", lambda e, pv=pv, c=c, i=i, dh=dh: e.matmul(pv, lhsT=mixT[:, c, i * 128:(i + 1) * 128], rhs=wo_sb[:, c, dh * 512:(dh + 1) * 512], start=(c == 0), stop=(c == 7)),
                              reads=[mixT_b, wo_b], **kw)
                    P.add("dve", lambda e, pv=pv, h1=h1, xt=xt, dh=dh: e.tensor_tensor(out=h1[:, dh * 512:(dh + 1) * 512], in0=pv, in1=xt[:, dh * 512:(dh + 1) * 512], op=ALU.add),
                          reads=[pb, xt_b], **(dict(writes=[h1_b]) if dh == 0 else dict(accum=[h1_b])))
                P.add("sp", lambda e, h1=h1, r0=r0: e.dma_start(out=h1_scr[r0:r0 + 128, :], in_=h1), reads=[h1_b], accum=[dram_h1], dma=f"h1st{hi_}")
                rms_stats(h1, h1_b, st, st_b)
                P.add("dve", lambda e, u2f=u2f, h1=h1, st=st: e.scalar_tensor_tensor(out=u2f, in0=h1, scalar=st[:, 2:3], in1=gffn_b, op0=ALU.mult, op1=ALU.mult),
                      reads=[h1_b, st_b, gffn_bb], writes=[u2f_b])
                transpose8(u2f, u2f_b, ident_f, ident_f_b)
                for half in range(2):
                    pv, pb = PS_TR[half]
                    P.add("act", lambda e, pv=pv, half=half, u2T=u2T: e.activation(out=u2T[:, half * 4:(half + 1) * 4, :], in_=pv.rearrange("p (c t) -> p c t", c=4), func=AF.Copy),
                          reads=[pb], **(dict(writes=[u2T_b]) if half == 0 else dict(accum=[u2T_b])))
                P.add("pool", lambda e, u2f=u2f, u2b=u2b: e.tensor_copy(out=u2b, in_=u2f), reads=[u2f_b], writes=[u2b_b])
                P.add("sp", lambda e, u2b=u2b, r0=r0: e.dma_start(out=u2_scr[r0:r0 + 128, :], in_=u2b), reads=[u2b_b], accum=[dram_u2], dma=f"u2st{hi_}")
                pv, pb = rot("pj", PS_PJ)
                for kc in range(8):
                    kw = dict(writes=[pb]) if kc == 0 else dict(accum=[pb])
                    P.add("pe", lambda e, pv=pv, kc=kc, u2T=u2T: e.matmul(pv[:, 0:NE], lhsT=u2T[:, kc, :], rhs=wr_sb[:, kc, :], start=(kc == 0), stop=(kc == 7)),
                          reads=[u2T_b, wr_b], **kw)
                P.add("dve", lambda e, pv=pv, ti=ti: e.tensor_tensor(out=logits_all[:, ti, :], in0=pv[:, 0:NE], in1=brb, op=ALU.add),
                      reads=[pb, brb_b], accum=[logits_b])
            P.barrier()
        try:
            for s_ in range(NSEQ):
                phase_a(s_)
        except StopBuild:
            pass
        if debug:
            P.add("sp", lambda e: e.dma_start(out=dbg["logits"], in_=logits_all.rearrange("p a b -> p (a b)")), reads=[logits_b], dma="dbglog")


        P.barrier()
        AR.reset(0)
        dram_xs = Buf("xs_scr")
        dram_y = Buf("y_scr")
        comb, comb_b = CA.alloc([NT, NE], F32, "comb")
        g4, g4_b = CA.alloc([NT, 4], F32, "g4")
        sel_i, sel_i_b = CA.alloc([4, NT], I32, "sel_i")
        idx_gu, idx_gu_b = CA.alloc([NMT], I32, "idx_gu")
        idx_dn, idx_dn_b = CA.alloc([NMT], I32, "idx_dn")
        idx_bg, idx_bg_b = CA.alloc([NMT], I32, "idx_bg")
        mx8, mx8_b = AR.alloc([NT, 8], F32, "mx8")
        rt = [AR.alloc([NE], F32, f"rt{i}") for i in range(3)]
        rs1, rs1_b = AR.alloc([NT, 4], F32, "rs1")
        mask_bf, mask_bf_b = AR.alloc([NT, NE], BF16, "mask_bf")
        runs, runs_b = AR.alloc([NT + 1, NE], F32, "runs")
        rank_all, rank_b = AR.alloc([NT, NE], F32, "rank_all")
        slotf, slotf_b = AR.alloc([NT, NE], F32, "slotf")
        eqt, eqt_b = AR.alloc([NT, NE], F32, "eqt")
        sel_f, sel_f_b = AR.alloc([4, NT], F32, "sel_f")
        sm = [AR.alloc([NE], F32, f"sm{i}") for i in range(6)]
        etf, etf_b = AR.alloc([NMT], F32, "etf")
        etc2, etc2_b = AR.alloc([NMT], F32, "etc2")
        idf, idf_b = AR.alloc([3, NMT], F32, "idf")
        d4, d4_b = AR.alloc([NT, 4], F32, "d4")
        u2ld = [AR.alloc([D], BF16, f"u2ld{i}") for i in range(4)]
        msk3, msk3_b = AR.alloc([NT, NE], F32, "msk3")
        ex3, ex3_b = AR.alloc([NT, NE], F32, "ex3")
        ssum, ssum_b = AR.alloc([NT], F32, "ssum")
        rsum, rsum_b = AR.alloc([NT], F32, "rsum")
        for ti in range(NT):
            P.add("dve", lambda e, ti=ti: e.max(out=mx8[:, ti, :], in_=logits_all[:, ti, :]), reads=[logits_b], accum=[mx8_b])
        P.add("dve", lambda e: e.tensor_tensor(out=msk3, in0=logits_all, in1=mx8[:, :, 3:4].to_broadcast([128, NT, NE]), op=ALU.is_ge),
              reads=[logits_b, mx8_b], writes=[msk3_b])
        P.add("dve", lambda e: e.tensor_copy(out=mask_bf, in_=msk3), reads=[msk3_b], writes=[mask_bf_b])
        P.add("dve", lambda e: e.tensor_tensor(out=ex3, in0=logits_all, in1=mx8[:, :, 0:1].to_broadcast([128, NT, NE]), op=ALU.subtract),
              reads=[logits_b, mx8_b], writes=[ex3_b])
        P.add("act", lambda e: e.activation(out=ex3, in_=ex3, func=AF.Exp), reads=[ex3_b], writes=[ex3_b])
        P.add("dve", lambda e: e.tensor_tensor(out=ex3, in0=ex3, in1=msk3, op=ALU.mult), reads=[ex3_b, msk3_b], writes=[ex3_b])
        P.add("dve", lambda e: e.reduce_sum(out=ssum, in_=ex3, axis=AX.X), reads=[ex3_b], writes=[ssum_b])
        P.add("dve", lambda e: e.reciprocal(out=rsum, in_=ssum), reads=[ssum_b], writes=[rsum_b])
        P.add("dve", lambda e: e.tensor_tensor(out=comb, in0=ex3, in1=rsum.unsqueeze(2).to_broadcast([128, NT, NE]), op=ALU.mult),
              reads=[ex3_b, rsum_b], writes=[comb_b])
        P.add("dve", lambda e: e.tensor_tensor(out=d4, in0=mx8[:, :, 0:4], in1=mx8[:, :, 0:1].to_broadcast([128, NT, 4]), op=ALU.subtract),
              reads=[mx8_b], writes=[d4_b])
        P.add("act", lambda e: e.activation(out=d4, in_=d4, func=AF.Exp), reads=[d4_b], writes=[d4_b])
        P.add("dve", lambda e: e.tensor_tensor(out=g4, in0=d4, in1=rsum.unsqueeze(2).to_broadcast([128, NT, 4]), op=ALU.mult),
              reads=[d4_b, rsum_b], writes=[g4_b])
        mflat = mask_bf.rearrange("p a b -> p (a b)")
        for half in range(2):
            pv, pb = psum[half]
            P.add("pe", lambda e, pv=pv, half=half: e.matmul(pv, lhsT=ustrict, rhs=mflat[:, half * 512:(half + 1) * 512], start=True, stop=True),
                  reads=[ustrict_b, mask_bf_b], writes=[pb])
            pv2, pb2 = psum[2 + half]
            P.add("pe", lambda e, pv2=pv2, half=half: e.matmul(pv2, lhsT=ones_b, rhs=mflat[:, half * 512:(half + 1) * 512], start=True, stop=True),
                  reads=[ones_b_b, mask_bf_b], writes=[pb2])
        P.add("dve", lambda e: e.memset(runs[:, 0, :], 0.0), writes=[runs_b])
        for i in range(NT):
            pv2, pb2 = psum[2 + i // 16]
            P.add("dve", lambda e, i=i, pv2=pv2: e.tensor_tensor(out=runs[:, i + 1, :], in0=runs[:, i, :], in1=pv2[:, (i % 16) * NE:(i % 16 + 1) * NE], op=ALU.add),
                  reads=[pb2, runs_b], writes=[runs_b])
        for half in range(2):
            pv, pb = psum[half]
            P.add("dve", lambda e, pv=pv, half=half: e.tensor_tensor(out=rank_all[:, half * 16:(half + 1) * 16, :], in0=pv.rearrange("p (a b) -> p a b", a=16),
                                                            in1=runs[:, half * 16:(half + 1) * 16, :], op=ALU.add),
                  reads=[pb, runs_b], **(dict(writes=[rank_b]) if half == 0 else dict(accum=[rank_b])))
        n_e = runs[:, NT, :]
        (ntl, ntl_b), (inc, inc_b), (bas, bas_b), (one32, one32_b), (cmpt, cmpt_b), _ = sm
        P.add("dve", lambda e: e.memset(ntl, 0.0), writes=[ntl_b])
        P.add("dve", lambda e: e.memset(one32, 1.0), writes=[one32_b])
        for j in range(NTOK // TS):
            P.add("dve", lambda e, j=j: e.scalar_tensor_tensor(out=ntl, in0=n_e, scalar=float(TS * j), in1=ntl, op0=ALU.is_gt, op1=ALU.add),
                  reads=[runs_b, ntl_b], writes=[ntl_b])
        P.add("dve", lambda e: e.tensor_tensor_scan(out=inc, data0=one32, data1=ntl, initial=0.0, op0=ALU.mult, op1=ALU.add),
              reads=[one32_b, ntl_b], writes=[inc_b])
        P.add("dve", lambda e: e.tensor_tensor(out=bas, in0=inc, in1=ntl, op=ALU.subtract), reads=[inc_b, ntl_b], writes=[bas_b])
        P.add("dve", lambda e: e.tensor_scalar(out=bas, in0=bas, scalar1=float(TS), scalar2=None, op0=ALU.mult), reads=[bas_b], writes=[bas_b])
        P.add("dve", lambda e: e.tensor_tensor(out=slotf, in0=rank_all, in1=bas.unsqueeze(1).to_broadcast([128, NT, NE]), op=ALU.add),
              reads=[rank_b, bas_b], writes=[slotf_b])
        for j in range(4):
            P.add("dve", lambda e, j=j: e.tensor_tensor(out=eqt, in0=logits_all, in1=mx8[:, :, j:j + 1].to_broadcast([128, NT, NE]), op=ALU.is_equal),
                  reads=[logits_b, mx8_b], writes=[eqt_b])
            P.add("dve", lambda e: e.tensor_tensor(out=eqt, in0=eqt, in1=slotf, op=ALU.mult), reads=[eqt_b, slotf_b], writes=[eqt_b])
            P.add("dve", lambda e, j=j: e.reduce_sum(out=sel_f[:, j, :], in_=eqt, axis=AX.X), reads=[eqt_b], **(dict(writes=[sel_f_b]) if j == 0 else dict(accum=[sel_f_b])))
        P.add("dve", lambda e: e.tensor_copy(out=sel_i, in_=sel_f), reads=[sel_f_b], writes=[sel_i_b])
        tvals = misc[:, 1:1 + NMT]
        P.add("dve", lambda e: e.memset(etf, 0.0), writes=[etf_b])
        for ex_i in range(NE):
            P.add("dve", lambda e, ex_i=ex_i: e.scalar_tensor_tensor(out=etf, in0=tvals, scalar=inc[:, ex_i:ex_i + 1], in1=etf, op0=ALU.is_ge, op1=ALU.add),
                  reads=[misc_b, inc_b, etf_b], writes=[etf_b])
        P.add("dve", lambda e: e.tensor_scalar(out=etc2, in0=etf, scalar1=float(NE - 1), scalar2=None, op0=ALU.min), reads=[etf_b], writes=[etc2_b])
        for r, mult in enumerate((4.0, 2.0, 1.0)):
            src, src_b = (etf, etf_b) if r == 0 else (etc2, etc2_b)
            P.add("dve", lambda e, r=r, mult=mult, src=src: e.tensor_scalar(out=idf[:, r, :], in0=src, scalar1=128.0, scalar2=misc[:, 0:1], op0=ALU.mult, op1=ALU.add),
                  reads=[src_b, misc_b], **(dict(writes=[idf_b]) if r == 0 else dict(accum=[idf_b])))
            if mult != 1.0:
                P.add("dve", lambda e, r=r, mult=mult: e.tensor_scalar(out=idf[:, r, :], in0=idf[:, r, :], scalar1=mult, scalar2=None, op0=ALU.mult),
                      reads=[idf_b], accum=[idf_b])
        P.add("dve", lambda e: e.tensor_copy(out=idx_gu, in_=idf[:, 0, :]), reads=[idf_b], writes=[idx_gu_b])
        P.add("dve", lambda e: e.tensor_copy(out=idx_dn, in_=idf[:, 1, :]), reads=[idf_b], writes=[idx_dn_b])
        P.add("dve", lambda e: e.tensor_copy(out=idx_bg, in_=idf[:, 2, :]), reads=[idf_b], writes=[idx_bg_b])
        if debug:
            P.add("sp", lambda e: e.dma_start(out=dbg["route"][:, 0:128], in_=sel_f.rearrange("p a b -> p (a b)")), reads=[sel_f_b], dma="dbgroute")
            P.add("sp", lambda e: e.dma_start(out=dbg["route"][:, 128:128 + NMT], in_=etf), reads=[etf_b], dma="dbgroute")
            P.add("sp", lambda e: e.dma_start(out=dbg["route"][:, 256:256 + NE], in_=n_e), reads=[runs_b], dma="dbgroute")
            P.add("sp", lambda e: e.dma_start(out=dbg["route"][:, 320:320 + 128], in_=sel_i.rearrange("p a b -> p (a b)").bitcast(F32)), reads=[sel_i_b], dma="dbgroute")
        for i in range(NT):
            ut, ut_b = u2ld[i % 4]
            P.add("sp", lambda e, ut=ut, i=i: e.dma_start(out=ut, in_=u2_scr[i * 128:(i + 1) * 128, :]), reads=[dram_u2], writes=[ut_b], dma=f"u2ld{i % 4}")
            for j in range(4):
                P.add("pool", lambda e, ut=ut, i=i, j=j: e.indirect_dma_start(out=xs_scr[:, :], out_offset=bass.IndirectOffsetOnAxis(ap=sel_i[:, j, i:i + 1], axis=0),
                                                                        in_=ut, in_offset=None),
                      reads=[ut_b, sel_i_b, dram_xs0], accum=[dram_xs], dma=f"scat{i % 4}")

        P.barrier()
        AR.reset(0)
        wgu_sb = [AR.alloc([8, 2048], BF16, f"wgu{i}") for i in range(2)]
        wd_sb = [AR.alloc([8, D], BF16, f"wd{i}") for i in range(2)]
        bgu_sb = [AR.alloc([16], F32, f"bgu{i}") for i in range(2)]
        xs_sb = [AR.alloc([4, D], BF16, f"xs{i}") for i in range(2)]
        xT_sb = [AR.alloc([8, TS], BF16, f"xT{i}") for i in range(2)]
        actT = [AR.alloc([8, TS], BF16, f"actT{i}") for i in range(2)]
        sw2 = [[AR.alloc([512], F32, f"sw{j}_{i}") for i in range(4)] for j in range(2)]
        yst = [AR.alloc([D], F32, f"yst{i}") for i in range(2)]
        wgu_rows = wgu_d
        wd_rows = wd_d
        xs_v = xs_scr.rearrange("(t s p) d -> t p s d", s=4, p=128)
        PS_T = [psum[0], psum[1]]
        PS_G = [psum[2], psum[3], psum[4], psum[5]]
        PS_D = [psum[6], psum[7]]
        cnt = {"d": 0, "y": 0, "ev": 0}

        _regs = {}

        def bound_reg(e, val):
            if val not in _regs:
                _regs[val] = e.to_reg(val)
            return _regs[val]

        def load_tile(t):
            wi = t % 2
            wg, wg_b = wgu_sb[wi]
            wdd, wdd_b = wd_sb[wi]
            bg, bg_b = bgu_sb[wi]
            xs, xs_b = xs_sb[wi]
            for c in range(4):
                P.add("pool", lambda e, wg=wg, t=t, c=c: e.indirect_dma_start(
                    out=wg.rearrange("p a b -> p (a b)")[:, c * 4096:(c + 1) * 4096], out_offset=None, in_=wgu_rows[:, :],
                    in_offset=bass.IndirectOffsetOnAxis(ap=idx_gu[:, t:t + 1], axis=0), element_offset=c * 4096,
                    bounds_check=bound_reg(e, NE * 128 * 4 - 1), oob_is_err=False),
                    reads=[idx_gu_b], **(dict(writes=[wg_b]) if c == 0 else dict(accum=[wg_b])), dma=f"wgu{wi}")
            for c in range(2):
                P.add("pool", lambda e, wdd=wdd, t=t, c=c: e.indirect_dma_start(
                    out=wdd.rearrange("p a b -> p (a b)")[:, c * 4096:(c + 1) * 4096], out_offset=None, in_=wd_rows[:, :],
                    in_offset=bass.IndirectOffsetOnAxis(ap=idx_dn[:, t:t + 1], axis=0), element_offset=c * 4096),
                    reads=[idx_dn_b], **(dict(writes=[wdd_b]) if c == 0 else dict(accum=[wdd_b])), dma=f"wd{wi}")
            P.add("pool", lambda e, bg=bg, t=t: e.indirect_dma_start(out=bg, out_offset=None, in_=bgu_d[:, :],
                                                                in_offset=bass.IndirectOffsetOnAxis(ap=idx_bg[:, t:t + 1], axis=0)),
                  reads=[idx_bg_b], writes=[bg_b], dma=f"bgu{wi}")
            P.add("sp", lambda e, xs=xs, t=t: e.dma_start(out=xs, in_=xs_v[t]), reads=[dram_xs, dram_xs0], writes=[xs_b], dma=f"xs{wi}")

        def transposes(t):
            xs, xs_b = xs_sb[t % 2]
            xT, xT_b = xT_sb[t % 2]
            for kc in range(8):
                pv, pb = PS_T[kc % 2]
                for sub in range(4):
                    kw = dict(writes=[pb]) if sub == 0 else dict(accum=[pb])
                    P.add("pe", lambda e, pv=pv, sub=sub, kc=kc, xs=xs: e.matmul(pv[:, sub * 128:(sub + 1) * 128], lhsT=xs[:, sub, kc * 128:(kc + 1) * 128], rhs=ident_b, start=True, stop=True),
                          reads=[xs_b, ident_b_b], **kw)
                kw = dict(writes=[xT_b]) if kc == 0 else dict(accum=[xT_b])
                if kc % 2 == 0:
                    P.add("act", lambda e, pv=pv, kc=kc, xT=xT: e.activation(out=xT[:, kc, :], in_=pv, func=AF.Copy), reads=[pb], **kw)
                else:
                    P.add("dve", lambda e, pv=pv, kc=kc, xT=xT: e.tensor_copy(out=xT[:, kc, :], in_=pv), reads=[pb], **kw)

        def gate_up(t):
            wg, wg_b = wgu_sb[t % 2]
            bg, bg_b = bgu_sb[t % 2]
            xT, xT_b = xT_sb[t % 2]
            aT, aT_b = actT[t % 2]
            for fc in range(8):
                pa, pa_b = PS_G[(fc % 2) * 2]
                pl, pl_b = PS_G[(fc % 2) * 2 + 1]
                for (pv, pb, coff) in ((pa, pa_b, 0), (pl, pl_b, 1024)):
                    for kc in range(8):
                        kw = dict(writes=[pb]) if kc == 0 else dict(accum=[pb])
                        P.add("pe", lambda e, pv=pv, kc=kc, fc=fc, coff=coff, wg=wg, xT=xT: e.matmul(
                            pv, lhsT=wg[:, kc, coff + fc * 128:coff + (fc + 1) * 128], rhs=xT[:, kc, :], start=(kc == 0), stop=(kc == 7)),
                            reads=[wg_b, xT_b], **kw)
                (xg, xg_b), (sg, sg_b), (xl, xl_b), (tt_, tt_b) = sw2[fc % 2]
                P.add("dve", lambda e, pa=pa, fc=fc, bg=bg, xg=xg: e.tensor_scalar(out=xg, in0=pa, scalar1=bg[:, fc:fc + 1], scalar2=7.0, op0=ALU.add, op1=ALU.min),
                      reads=[pa_b, bg_b], writes=[xg_b])
                P.add("act", lambda e, xg=xg, sg=sg: e.activation(out=sg, in_=xg, func=AF.Sigmoid, scale=1.702), reads=[xg_b], writes=[sg_b])
                P.add("act", lambda e, pl=pl, fc=fc, bg=bg, xl=xl: e.activation(out=xl, in_=pl, func=AF.Identity, bias=bg[:, 8 + fc:9 + fc]),
                      reads=[pl_b, bg_b], writes=[xl_b])
                P.add("dve", lambda e, xl=xl: e.tensor_scalar(out=xl, in0=xl, scalar1=7.0, scalar2=-7.0, op0=ALU.min, op1=ALU.max), reads=[xl_b], writes=[xl_b])
                P.add("dve", lambda e, xl=xl, xg=xg, tt_=tt_: e.scalar_tensor_tensor(out=tt_, in0=xl, scalar=1.0, in1=xg, op0=ALU.add, op1=ALU.mult),
                      reads=[xl_b, xg_b], writes=[tt_b])
                P.add("pool", lambda e, tt_=tt_, sg=sg, aT=aT, fc=fc: e.tensor_tensor(out=aT[:, fc, :], in0=tt_, in1=sg, op=ALU.mult),
                      reads=[tt_b, sg_b], **(dict(writes=[aT_b]) if fc == 0 else dict(accum=[aT_b])))

        def down(t):
            wdd, wdd_b = wd_sb[t % 2]
            aT, aT_b = actT[t % 2]
            for t4 in range(4):
                ys, ys_b = yst[cnt["y"] % 2]; cnt["y"] += 1
                for dh in range(2):
                    pv, pb = PS_D[cnt["d"] % 2]; cnt["d"] += 1
                    for fc in range(8):
                        kw = dict(writes=[pb]) if fc == 0 else dict(accum=[pb])
                        P.add("pe", lambda e, pv=pv, fc=fc, t4=t4, dh=dh, aT=aT, wdd=wdd: e.matmul(
                            pv, lhsT=aT[:, fc, t4 * 128:(t4 + 1) * 128], rhs=wdd[:, fc, dh * 512:(dh + 1) * 512], start=(fc == 0), stop=(fc == 7)),
                            reads=[aT_b, wdd_b], **kw)
                    kw = dict(writes=[ys_b]) if dh == 0 else dict(accum=[ys_b])
                    P.add("act", lambda e, pv=pv, ys=ys, dh=dh: e.activation(out=ys[:, dh * 512:(dh + 1) * 512], in_=pv, func=AF.Copy), reads=[pb], **kw)
                r0 = t * TS + t4 * 128
                P.add("sp", lambda e, ys=ys, r0=r0: e.dma_start(out=y_scr[r0:r0 + 128, :], in_=ys), reads=[ys_b], accum=[dram_y], dma=f"yst{(cnt['y'] - 1) % 2}")

        load_tile(0)
        transposes(0)
        for t in range(NMT):
            if t + 1 < NMT:
                load_tile(t + 1)
            gate_up(t)
            if t + 1 < NMT:
                transposes(t + 1)
            down(t)

        P.barrier()
        AR.reset(0)
        wpg_sb, wpg_b = AR.alloc([8, D], BF16, "wpg")
        P.add("pool", lambda e: e.dma_start(out=wpg_sb, in_=wpg_d.rearrange("(c p) n -> p c n", p=128)), writes=[wpg_b], dma="wpg")
        wpp_sb, wpp_b = AR.alloc([2, D], BF16, "wpp")
        P.add("pool", lambda e: e.dma_start(out=wpp_sb, in_=wpp_d.rearrange("(c p) n -> p c n", p=128)), writes=[wpp_b], dma="wpp")
        gple_b, gple_bb = AR.alloc([D], F32, "gple_b")
        P.add("sp", lambda e: e.dma_start(out=gple_b, in_=gvec_d[2:3, :].partition_broadcast(128)), writes=[gple_bb], dma="gpleb")
        bd_sb, bd_b = AR.alloc([D], F32, "bd_sb")
        P.add("sp", lambda e: e.dma_start(out=bd_sb[0:NE, :], in_=bd_d), writes=[bd_b], dma="bd")
        combT_t = [AR.alloc([128], F32, f"combT{i}") for i in range(3)]
        yg_t = [[AR.alloc([D], F32, f"yg{k}_{j}") for j in range(4)] for k in range(3)]
        h2_t = [AR.alloc([D], F32, f"h2t{i}") for i in range(3)]
        u3_t = [AR.alloc([D], BF16, f"u3{i}") for i in range(3)]
        u3T_t = [AR.alloc([8, 128], BF16, f"u3T{i}") for i in range(3)]
        gate_t = [AR.alloc([D], F32, f"gate{i}") for i in range(3)]
        pin_t = [AR.alloc([256], F32, f"pin{i}") for i in range(3)]
        pbf_t = [AR.alloc([256], BF16, f"pbf{i}") for i in range(3)]
        pT_t = [AR.alloc([2, 128], BF16, f"pT{i}") for i in range(3)]
        o_t = [AR.alloc([D], F32, f"o{i}") for i in range(3)]
        junk2, junk2_b = AR.alloc([D], BF16, "junk2")
        st2_t = [AR.alloc([4], F32, f"st2{i}") for i in range(3)]
        PS_TR2 = [psum[0], psum[1]]
        PS_GT = [psum[2], psum[3]]
        PS_BD = [psum[4], psum[5]]
        PS_PP = [psum[6], psum[7]]
        def c_stage1(ti):
            k = ti % 3
            h2, h2_b = h2_t[k]
            u3, u3_b = u3_t[k]
            u3T, u3T_b = u3T_t[k]
            gt, gt_b = gate_t[k]
            pin, pin_b = pin_t[k]
            pbf, pbf_b = pbf_t[k]
            pT, pT_b = pT_t[k]
            ot, ot_b = o_t[k]
            st, st_b = st2_t[k]
            cT, cT_b = combT_t[k]
            r0 = ti * 128
            P.add("sp", lambda e, h2=h2, r0=r0: e.dma_start(out=h2, in_=h1_scr[r0:r0 + 128, :]), reads=[dram_h1], writes=[h2_b], dma=f"h2ld{k}")
            P.add("sp", lambda e, pin=pin, r0=r0: e.dma_start(out=pin, in_=p_d[r0:r0 + 128, :]), writes=[pin_b], dma=f"pld{k}")
            for j in range(4):
                yg, yg_b = yg_t[k][j]
                P.add("pool", lambda e, yg=yg, j=j, ti=ti: e.indirect_dma_start(out=yg, out_offset=None, in_=y_scr[:, :],
                                                                        in_offset=bass.IndirectOffsetOnAxis(ap=sel_i[:, j, ti:ti + 1], axis=0)),
                      reads=[sel_i_b, dram_y], writes=[yg_b], dma=f"yg{k}_{j}")
            pv, pb = PS_BD[0]
            P.add("pe", lambda e, pv=pv, ti=ti: e.matmul(pv[0:NE, 0:128], lhsT=comb[:, ti, :], rhs=ident_f, start=True, stop=True),
                  reads=[comb_b, ident_f_b], writes=[pb])
            P.add("act", lambda e, pv=pv, cT=cT: e.activation(out=cT[0:NE, :], in_=pv[0:NE, 0:128], func=AF.Copy), reads=[pb], writes=[cT_b])
            for dh in range(2):
                pv2, pb2 = PS_BD[1] if dh == 0 else PS_BD[0]
                P.add("pe", lambda e, pv2=pv2, dh=dh, cT=cT: e.matmul(pv2, lhsT=cT[0:NE, :], rhs=bd_sb[0:NE, dh * 512:(dh + 1) * 512], start=True, stop=True),
                      reads=[cT_b, bd_b], writes=[pb2])
                P.add("dve", lambda e, pv2=pv2, dh=dh, h2=h2: e.tensor_tensor(out=h2[:, dh * 512:(dh + 1) * 512], in0=pv2, in1=h2[:, dh * 512:(dh + 1) * 512], op=ALU.add),
                      reads=[pb2, h2_b], writes=[h2_b])
            for j in range(4):
                yg, yg_b = yg_t[k][j]
                P.add("dve", lambda e, yg=yg, j=j, ti=ti, h2=h2: e.scalar_tensor_tensor(out=h2, in0=yg, scalar=g4[:, ti, j:j + 1], in1=h2, op0=ALU.mult, op1=ALU.add),
                      reads=[yg_b, g4_b, h2_b], writes=[h2_b])

        def c_stage1b(ti):
            k = ti % 3
            h2, h2_b = h2_t[k]
            u3, u3_b = u3_t[k]
            u3T, u3T_b = u3T_t[k]
            gt, gt_b = gate_t[k]
            pin, pin_b = pin_t[k]
            pbf, pbf_b = pbf_t[k]
            pT, pT_b = pT_t[k]
            ot, ot_b = o_t[k]
            st, st_b = st2_t[k]
            cT, cT_b = combT_t[k]
            r0 = ti * 128
            P.add("act", lambda e, h2=h2, st=st: e.activation(out=junk2, in_=h2, func=AF.Square, accum_out=st[:, 0:1]), reads=[h2_b], writes=[junk2_b, st_b])
            P.add("act", lambda e, st=st: e.activation(out=st[:, 1:2], in_=st[:, 0:1], func=AF.Ln, bias=ccol[:, 2:3], scale=1.0 / D), reads=[ccol_b], writes=[st_b])
            P.add("act", lambda e, st=st: e.activation(out=st[:, 2:3], in_=st[:, 1:2], func=AF.Exp, scale=-0.5), writes=[st_b])
            P.add("dve", lambda e, u3=u3, h2=h2, st=st: e.scalar_tensor_tensor(out=u3, in0=h2, scalar=st[:, 2:3], in1=gple_b, op0=ALU.mult, op1=ALU.mult),
                  reads=[h2_b, st_b, gple_bb], writes=[u3_b])
            for half in range(2):
                pv, pb = PS_TR2[half]
                for c in range(4):
                    cc = half * 4 + c
                    kw = dict(writes=[pb]) if c == 0 else dict(accum=[pb])
                    P.add("pe", lambda e, pv=pv, c=c, cc=cc, u3=u3: e.matmul(pv[:, c * 128:(c + 1) * 128], lhsT=u3[:, cc * 128:(cc + 1) * 128], rhs=ident_b, start=True, stop=True),
                          reads=[u3_b, ident_b_b], **kw)
                P.add("act", lambda e, pv=pv, half=half, u3T=u3T: e.activation(out=u3T[:, half * 4:(half + 1) * 4, :], in_=pv.rearrange("p (c t) -> p c t", c=4), func=AF.Copy),
                      reads=[pb], **(dict(writes=[u3T_b]) if half == 0 else dict(accum=[u3T_b])))
            P.add("pool", lambda e, pin=pin, pbf=pbf: e.tensor_copy(out=pbf, in_=pin), reads=[pin_b], writes=[pbf_b])
            pvp, pbp = PS_PP[0]
            for c in range(2):
                kw = dict(writes=[pbp]) if c == 0 else dict(accum=[pbp])
                P.add("pe", lambda e, c=c, pbf=pbf: e.matmul(pvp[:, c * 128:(c + 1) * 128], lhsT=pbf[:, c * 128:(c + 1) * 128], rhs=ident_b, start=True, stop=True),
                      reads=[pbf_b, ident_b_b], **kw)
            P.add("act", lambda e, pT=pT: e.activation(out=pT, in_=pvp[:, 0:256].rearrange("p (c t) -> p c t", c=2), func=AF.Copy), reads=[pbp], writes=[pT_b])

        def c_stage2(ti):
            k = ti % 3
            h2, h2_b = h2_t[k]
            u3, u3_b = u3_t[k]
            u3T, u3T_b = u3T_t[k]
            gt, gt_b = gate_t[k]
            pin, pin_b = pin_t[k]
            pbf, pbf_b = pbf_t[k]
            pT, pT_b = pT_t[k]
            ot, ot_b = o_t[k]
            st, st_b = st2_t[k]
            cT, cT_b = combT_t[k]
            r0 = ti * 128
            for dh in range(2):
                pv, pb = PS_GT[dh]
                for kc in range(8):
                    kw = dict(writes=[pb]) if kc == 0 else dict(accum=[pb])
                    P.add("pe", lambda e, pv=pv, kc=kc, dh=dh, u3T=u3T: e.matmul(pv, lhsT=u3T[:, kc, :], rhs=wpg_sb[:, kc, dh * 512:(dh + 1) * 512], start=(kc == 0), stop=(kc == 7)),
                          reads=[u3T_b, wpg_b], **kw)
                P.add("act", lambda e, pv=pv, dh=dh, gt=gt: e.activation(out=gt[:, dh * 512:(dh + 1) * 512], in_=pv, func=AF.Sigmoid),
                      reads=[pb], **(dict(writes=[gt_b]) if dh == 0 else dict(accum=[gt_b])))
            for dh in range(2):
                pv, pb = PS_PP[1] if dh == 0 else PS_PP[0]
                for c in range(2):
                    kw = dict(writes=[pb]) if c == 0 else dict(accum=[pb])
                    P.add("pe", lambda e, pv=pv, c=c, dh=dh, pT=pT: e.matmul(pv, lhsT=pT[:, c, :], rhs=wpp_sb[:, c, dh * 512:(dh + 1) * 512], start=(c == 0), stop=(c == 1)),
                          reads=[pT_b, wpp_b], **kw)
                P.add("dve", lambda e, pv=pv, dh=dh, gt=gt, ot=ot: e.tensor_tensor(out=ot[:, dh * 512:(dh + 1) * 512], in0=pv, in1=gt[:, dh * 512:(dh + 1) * 512], op=ALU.mult),
                      reads=[pb, gt_b], **(dict(writes=[ot_b]) if dh == 0 else dict(accum=[ot_b])))
            P.add("pool", lambda e, ot=ot, h2=h2: e.tensor_tensor(out=ot, in0=ot, in1=h2, op=ALU.add), reads=[ot_b, h2_b], writes=[ot_b])
            P.add("sp", lambda e, ot=ot, r0=r0: e.dma_start(out=out_d[r0:r0 + 128, :], in_=ot), reads=[ot_b], accum=[dram_out], dma=f"outst{k}")

        for it in range(NT + 2):
            if 0 <= it - 2 < NT:
                c_stage2(it - 2)
            if 0 <= it - 1 < NT:
                c_stage1b(it - 1)
            if it < NT:
                c_stage1(it)
        P.barrier()

        with nc.Block() as block:
            P.emit(block)
    return nc


def _consts():
    c = {}
    c["ident"] = np.eye(128, dtype=np.float32)
    kk = np.arange(128)[:, None]
    mm = np.arange(128)[None, :]
    c["ustrict"] = (kk < mm).astype(np.float32)
    c["blk64"] = ((kk // 64) == (mm // 64)).astype(np.float32)
    xs = np.arange(XW)[None, :]
    delta = xs - 384 - kk
    m = ((delta >= 0) & (delta <= 128)).astype(np.float64)
    m += ((delta >= 0) & (delta % 4 == 0) & (delta <= 512))
    m += ((delta >= 0) & (delta % 16 == 0) & (delta <= 2048))
    lm = np.where(m > 0, np.log(np.maximum(m, 1.0)), NEG)
    slopes = 2.0 ** (-(np.arange(8) + 1.0))
    c["btabA"] = (-(slopes[:, None, None]) * delta[None].astype(np.float64) + lm[None]).astype(np.float32)
    xs2 = np.arange(CBW)[None, :]
    d2 = xs2 - 384 - kk
    c["cbtab"] = np.where(d2 >= 0, 0.0, NEG).astype(np.float32)
    c["kpos"] = (np.arange(16)[None, :] * 128 + kk).astype(np.float32)
    sel8 = np.zeros((8, 8, 128), np.float32)
    for h in range(8):
        sel8[h, h, :] = 1.0
    c["sel8"] = sel8.reshape(8, 8 * 128)
    c["tokid"] = (np.arange(NT)[None, :] * 128 + kk).astype(np.int32)
    misc = np.zeros((128, 128), np.float32)
    misc[:, 0] = np.arange(128)
    misc[:, 1:65] = np.arange(64)[None, :]
    misc[:, 65:73] = (np.arange(8) * TS)[None, :]
    c["misc"] = misc
    return c


def _prep_inputs(x, p, g_mix, w_in, b_f, g_qa, g_ka, g_qb, g_kb, w_o, g_ffn, w_router, b_router,
                 w_gate_up, b_gate_up, w_down, b_down, g_ple, w_ple_gate, w_ple_proj):
    f = lambda a: np.ascontiguousarray(np.asarray(a, dtype=np.float32))
    x = f(x); p = f(p)
    shared = {}
    shared["w_in"] = f(w_in[0])
    shared["w_o"] = f(w_o[0])
    shared["w_router"] = f(w_router[0])
    wgu = np.asarray(w_gate_up[0], dtype=np.float32)
    wgu = np.concatenate([wgu[:, :, 0::2], wgu[:, :, 1::2]], axis=2)
    wgu = wgu.reshape(NE, 8, 128, 2048).transpose(0, 2, 1, 3)
    shared["wgu"] = np.ascontiguousarray(wgu).reshape(NE * 128 * 4, 4096)
    wd = np.asarray(w_down[0], dtype=np.float32).reshape(NE, 8, 128, D).transpose(0, 2, 1, 3)
    shared["wd"] = np.ascontiguousarray(wd).reshape(NE * 128 * 2, 4096)
    bgu = np.asarray(b_gate_up[0], dtype=np.float32)
    bgu = np.concatenate([bgu[:, 0::2], bgu[:, 1::2]], axis=1)
    shared["bgu"] = np.ascontiguousarray(bgu.reshape(NE, 16, 128).transpose(0, 2, 1)).reshape(NE * 128, 16)
    shared["b_down"] = f(b_down[0])
    shared["w_ple_gate"] = f(w_ple_gate[0])
    shared["w_ple_proj"] = f(w_ple_proj[0])
    shared["gvec"] = np.ascontiguousarray(np.stack([f(g_mix[0]), f(g_ffn[0]), f(g_ple[0])], axis=0))
    shared["gvecT"] = np.ascontiguousarray(shared["gvec"].reshape(3, 8, 128).transpose(2, 0, 1)).reshape(128, 24)
    gq = np.stack([np.tile(f(g_qa[0]), 2), np.tile(f(g_ka[0]), 2), np.tile(f(g_qb[0]), 2), np.tile(f(g_kb[0]), 2)], axis=1)
    shared["gqk"] = np.ascontiguousarray(gq)
    shared["bf"] = f(b_f[0]).reshape(8, 1)
    shared["b_router"] = f(b_router[0]).reshape(1, NE)
    shared.update(_consts())
    if STAGE not in ('', 'full'):
        shared["wgu"] = shared["wgu"][:128 * 4]
        shared["wd"] = shared["wd"][:128 * 2]
    in_maps = []
    for c in range(NCORES):
        m = dict(shared)
        m["x"] = x[c * NSEQ:(c + 1) * NSEQ].reshape(NTOK, D)
        m["p"] = p[0, c * NSEQ:(c + 1) * NSEQ].reshape(NTOK, 256)
        in_maps.append(m)
    return in_maps


_NC_CACHE = {}


def kernel(**inputs):
    in_maps = _prep_inputs(**inputs)
    if "nc" not in _NC_CACHE:
        _NC_CACHE["nc"] = build_program(DEBUG)
    nc = _NC_CACHE["nc"]
    res = run_bass_kernel_spmd(nc, in_maps, core_ids=list(range(NCORES)))
    outs = [np.asarray(r["out"]).reshape(NSEQ, SEQ, D) for r in res.results]
    if DEBUG:
        kernel.last_results = res.results
    return np.concatenate(outs, axis=0).astype(np.float32)
```

```python
import math
from contextlib import ExitStack

import numpy as np
import ml_dtypes

import concourse.bass as bass
import concourse.mybir as mybir
from concourse.bass_utils import run_bass_kernel_spmd

F32 = mybir.dt.float32
BF16 = mybir.dt.bfloat16
I32 = mybir.dt.int32
ALU = mybir.AluOpType
AF = mybir.ActivationFunctionType
AX = mybir.AxisListType

NCORES = 8
D = 1024
SEQ = 2048
NSEQ = 2
NTOK = NSEQ * SEQ
NT = NTOK // 128
NE = 32
TOPK = 4
TS = 512
NMT = NTOK * TOPK // TS + NE
NSLOT = NMT * TS
EPS = 1e-6
NEG = -30000.0
XW = 2432
CBW = 896

DEBUG = False
import os
STAGE = os.environ.get('KSTAGE', '')


class StopBuild(Exception):
    pass


def checkpoint(name):
    if STAGE == name:
        raise StopBuild()


class Buf:
    __slots__ = ("name", "writers", "readers")

    def __init__(self, name):
        self.name = name
        self.writers = []
        self.readers = []


class Op:
    __slots__ = ("eng", "fn", "deps", "is_dma", "sem", "val", "signal", "extra_waits")

    def __init__(self, eng, fn):
        self.eng = eng
        self.fn = fn
        self.deps = []
        self.is_dma = False
        self.sem = None
        self.val = 0
        self.signal = False


class Prog:
    ENG = ["pe", "act", "dve", "pool", "sp"]
    SAME_ENGINE_SYNC = {"act", "dve", "pool"}

    def __init__(self, nc, stack):
        self.nc = nc
        self.stack = stack
        self.ops = {e: [] for e in self.ENG}
        self.esem = {e: stack.enter_context(nc.semaphore("sem_" + e)) for e in self.ENG}
        self.dma_sems = {}
        self.last = {e: None for e in self.ENG}

    def dma_sem(self, name):
        if name not in self.dma_sems:
            self.dma_sems[name] = [self.stack.enter_context(self.nc.semaphore("dq_" + name)), 0, None]
        return self.dma_sems[name]

    def add(self, eng, fn, reads=(), writes=(), accum=(), dma=None):
        op = Op(eng, fn)
        deps = []
        for b in reads:
            deps.extend(b.writers)
        for b in writes:
            deps.extend(b.writers)
            deps.extend(b.readers)
        for b in accum:
            deps.extend(b.readers)
        seen = set()
        for d in deps:
            if id(d) not in seen and d is not op:
                seen.add(id(d))
                op.deps.append(d)
        if dma is not None:
            rec = self.dma_sem(dma)
            rec[1] += 16
            rec[2] = op
            op.is_dma = True
            op.sem = rec[0]
            op.val = rec[1]
        for b in reads:
            b.readers.append(op)
        for b in writes:
            b.writers = [op]
            b.readers = []
        for b in accum:
            if b.writers and all((w.eng == eng and not w.is_dma and not op.is_dma) for w in b.writers):
                b.writers = [op]
            else:
                b.writers.append(op)
            b.readers = []
        self.ops[eng].append(op)
        if not op.is_dma:
            self.last[eng] = op
        return op

    def barrier(self):
        deps = [o for o in self.last.values() if o is not None]
        deps += [rec[2] for rec in self.dma_sems.values() if rec[2] is not None]
        for e in self.ENG:
            op = Op(e, None)
            op.deps = [d for d in deps]
            self.ops[e].append(op)

    def emit(self, block):
        for e in self.ENG:
            for op in self.ops[e]:
                for d in op.deps:
                    if d.is_dma:
                        continue
                    if d.eng == op.eng and not op.is_dma and op.fn is not None and d.eng not in self.SAME_ENGINE_SYNC:
                        continue
                    d.signal = True
        for e in self.ENG:
            cnt = 0
            for op in self.ops[e]:
                if not op.is_dma and op.signal:
                    cnt += 1
                    op.val = cnt
                    op.sem = self.esem[e]
        self.counts = {}

        def run(e, engobj):
            seen = {}
            n = 0
            for op in self.ops[e]:
                need = {}
                for d in op.deps:
                    if not d.is_dma and not d.signal:
                        continue
                    key = id(d.sem)
                    if d.val > need.get(key, (0, None))[0]:
                        need[key] = (d.val, d.sem)
                for key, (val, sem) in need.items():
                    if seen.get(key, 0) >= val:
                        continue
                    seen[key] = val
                    engobj.wait_ge(sem, val)
                    n += 1
                if op.fn is None:
                    continue
                ins = op.fn(engobj)
                n += 1
                if op.is_dma:
                    ins.then_inc(op.sem, 16)
                elif op.signal:
                    ins.then_inc(op.sem, 1)
            self.counts[e] = n

        @block.tensor
        def _(eng):
            run("pe", eng)

        @block.scalar
        def _(eng):
            run("act", eng)

        @block.vector
        def _(eng):
            run("dve", eng)

        @block.gpsimd
        def _(eng):
            run("pool", eng)

        @block.sync
        def _(eng):
            run("sp", eng)


class Arena:
    def __init__(self, ap, nwords):
        self.ap = ap
        self.n = nwords
        self.off = 0
        self.cnt = 0

    def mark(self):
        return self.off

    def reset(self, m=0):
        self.off = m

    def alloc(self, free_shape, dtype, name=None):
        esz = 4 if dtype in (F32, I32) else 2
        nel = int(np.prod(free_shape))
        n32 = (nel * esz + 3) // 4
        assert self.off + n32 <= self.n, f"arena overflow {name} {self.off}+{n32}>{self.n}"
        v = self.ap[:, self.off:self.off + n32]
        self.off += n32
        if dtype != F32:
            v = v.bitcast(dtype)
        if len(free_shape) == 2:
            v = v.rearrange("p (a b) -> p a b", a=free_shape[0])
        elif len(free_shape) == 3:
            v = v.rearrange("p (a b c) -> p a b c", a=free_shape[0], b=free_shape[1])
        self.cnt += 1
        return v, Buf(name or f"buf{self.cnt}")


def build_program(debug=False):
    nc = bass.Bass("TRN2", target_bir_lowering=False)

    def din(name, shape, dt=F32):
        return nc.dram_tensor(name, list(shape), dt, kind="ExternalInput").ap()

    x_d = din("x", [NTOK, D])
    p_d = din("p", [NTOK, 256])
    w_in_d = din("w_in", [D, 3080])
    w_o_d = din("w_o", [D, D])
    w_r_d = din("w_router", [D, NE])
    NEW = NE if STAGE in ('', 'full') else 1
    wgu_d = din("wgu", [NEW * 128 * 4, 4096])
    wd_d = din("wd", [NEW * 128 * 2, 4096])
    bgu_d = din("bgu", [NE * 128, 16])
    bd_d = din("b_down", [NE, D])
    wpg_d = din("w_ple_gate", [D, D])
    wpp_d = din("w_ple_proj", [256, D])
    gvec_d = din("gvec", [3, D])
    gvecT_d = din("gvecT", [128, 24])
    gqk_d = din("gqk", [128, 4])
    nbf_d = din("bf", [8, 1])
    br_d = din("b_router", [1, NE])
    ident_d = din("ident", [128, 128])
    ustrict_d = din("ustrict", [128, 128])
    blk64_d = din("blk64", [128, 128])
    btabA_d = din("btabA", [8, 128, XW])
    cbtab_d = din("cbtab", [128, CBW])
    kpos_d = din("kpos", [128, 16])
    sel8_d = din("sel8", [8, 8 * 128])
    tokid_d = din("tokid", [128, NT], I32)
    misc_d = din("misc", [128, 128])
    out_d = nc.dram_tensor("out", [NTOK, D], F32, kind="ExternalOutput").ap()

    h1_scr = nc.dram_tensor("h1_scr", [NTOK, D], F32, kind="Internal").ap()
    u2_scr = nc.dram_tensor("u2_scr", [NTOK, D], BF16, kind="Internal").ap()
    xs_scr = nc.dram_tensor("xs_scr", [NSLOT, D], BF16, kind="Internal").ap()
    y_scr = nc.dram_tensor("y_scr", [NSLOT, D], F32, kind="Internal").ap()
    dbg = {}
    if debug:
        dbg["mix"] = nc.dram_tensor("dbg_mix", [NSEQ, 128, 8 * SEQ], BF16, kind="ExternalOutput").ap()
        dbg["logits"] = nc.dram_tensor("dbg_logits", [128, NT * NE], F32, kind="ExternalOutput").ap()
        dbg["route"] = nc.dram_tensor("dbg_route", [128, 1024], F32, kind="ExternalOutput").ap()
        dbg["h1"] = h1_scr
    stack = ExitStack()
    with stack:
        print("sbuf bytes remaining", nc.sbuf_bytes_remaining)
        ARW = 47 * 1024
        CAW = 4608
        arena_t = stack.enter_context(nc.sbuf_tensor("arena", [128, ARW], F32))
        const_t = stack.enter_context(nc.sbuf_tensor("consts", [128, CAW], F32))
        AR = Arena(arena_t[:, :], ARW)
        CA = Arena(const_t[:, :], CAW)
        psum = []
        for i in range(8):
            t = stack.enter_context(nc.psum_tensor(f"ps{i}", [128, 512], F32))
            psum.append((t[:, :], Buf(f"ps{i}")))
        P = Prog(nc, stack)
        dram_u2 = Buf("u2_scr")
        dram_h1 = Buf("h1_scr")
        dram_out = Buf("out")

        def load_const(src_ap, free_shape, dtype, name, eng="sp"):
            v, b = CA.alloc(free_shape, dtype, name)
            P.add(eng, lambda e, v=v, s=src_ap: e.dma_start(out=v, in_=s), writes=[b], dma="c_" + name)
            return v, b

        ident_f, ident_f_b = load_const(ident_d, [128], F32, "ident_f")
        ident_b, ident_b_b = load_const(ident_d, [128], BF16, "ident_b", eng="pool")
        ustrict, ustrict_b = load_const(ustrict_d, [128], BF16, "ustrict", eng="pool")
        blk64, blk64_b = load_const(blk64_d, [128], BF16, "blk64", eng="pool")
        ones_b, ones_b_b = CA.alloc([128], BF16, "ones_b")
        P.add("dve", lambda e: e.memset(ones_b, 1.0), writes=[ones_b_b])
        ccol, ccol_b = CA.alloc([4], F32, "ccol")
        P.add("dve", lambda e: e.memset(ccol[:, 0:1], 64.0 * EPS), writes=[ccol_b])
        P.add("dve", lambda e: e.memset(ccol[:, 1:2], 1.0), writes=[ccol_b])
        P.add("dve", lambda e: e.memset(ccol[:, 2:3], EPS), writes=[ccol_b])
        gT, gT_b = load_const(gvecT_d, [3, 8], F32, "gT")
        gqk, gqk_b = load_const(gqk_d, [4], F32, "gqk")
        P.add("dve", lambda e: e.tensor_scalar(out=gqk[:, 1:2], in0=gqk[:, 1:2], scalar1=8.0, scalar2=None, op0=ALU.mult), writes=[gqk_b])
        P.add("dve", lambda e: e.tensor_scalar(out=gqk[:, 3:4], in0=gqk[:, 3:4], scalar1=8.0, scalar2=None, op0=ALU.mult), writes=[gqk_b])
        nbf, nbf_b = CA.alloc([1], F32, "nbf")
        P.add("sp", lambda e: e.dma_start(out=nbf[0:8, :], in_=nbf_d), writes=[nbf_b], dma="c_nbf")
        P.add("dve", lambda e: e.tensor_scalar(out=nbf[0:8, :], in0=nbf[0:8, :], scalar1=-1.0, scalar2=None, op0=ALU.mult), writes=[nbf_b])
        kpos, kpos_b = load_const(kpos_d, [16], F32, "kpos")
        misc, misc_b = load_const(misc_d, [128], F32, "misc")
        tokid, tokid_b = load_const(tokid_d, [NT], I32, "tokid")


        zt, zt_b = CA.alloc([2048], BF16, "zt")
        P.add("pool", lambda e: e.memset(zt, 0.0), writes=[zt_b])
        dram_xs0 = Buf("xs_zero")
        xs_flat = xs_scr.rearrange("(p a) d -> p (a d)", p=128)
        logits_all, logits_b = CA.alloc([NT, NE], F32, "logits_all")

        uT, uT_b = AR.alloc([8, SEQ], BF16, "uT")
        mixT, mixT_b = AR.alloc([8, SEQ], BF16, "mixT")
        wr_sb, wr_b = AR.alloc([8, NE], F32, "w_r")
        P.add("sp", lambda e: e.dma_start(out=wr_sb, in_=w_r_d.rearrange("(c p) n -> p c n", p=128)), writes=[wr_b], dma="wr")
        brb, brb_b = AR.alloc([NE], F32, "brb")
        P.add("sp", lambda e: e.dma_start(out=brb, in_=br_d.partition_broadcast(128)), writes=[brb_b], dma="brb")
        cbtab, cbtab_b = AR.alloc([CBW], F32, "cbtab")
        P.add("sp", lambda e: e.dma_start(out=cbtab, in_=cbtab_d), writes=[cbtab_b], dma="cbtab")
        sel8, sel8_b = AR.alloc([8 * 128], F32, "sel8")
        P.add("sp", lambda e: e.dma_start(out=sel8[0:8, :], in_=sel8_d), writes=[sel8_b], dma="sel8")
        onespad, onespad_b = AR.alloc([2, 128], BF16, "onespad")
        P.add("pool", lambda e: e.memset(onespad, 0.0), writes=[onespad_b])
        P.add("pool", lambda e: e.memset(onespad[:, 0, 0:64], 1.0), writes=[onespad_b])
        P.add("pool", lambda e: e.memset(onespad[:, 1, 64:128], 1.0), writes=[onespad_b])
        wf_sb, wf_b = AR.alloc([8, 8], BF16, "wf")
        P.add("pool", lambda e: e.dma_start(out=wf_sb, in_=w_in_d.rearrange("(c p) n -> p c n", p=128)[:, :, 3072:3080]), writes=[wf_b], dma="wf")
        csum, csum_b = AR.alloc([SEQ], F32, "csum")
        ones8, ones8_b = AR.alloc([512], F32, "ones8")
        P.add("pool", lambda e: e.memset(ones8, 1.0), writes=[ones8_b])
        nck, nck_b = AR.alloc([16, 8], F32, "nck")
        NR = 5
        sq_t = [AR.alloc([512], BF16, f"sq{i}") for i in range(2)]
        rs_t = [AR.alloc([512], F32, f"rs{i}") for i in range(2)]
        tmp_t = [AR.alloc([512], F32, f"tmp{i}") for i in range(NR)]
        pt_t = [AR.alloc([512], BF16, f"pt{i}") for i in range(NR)]
        rden_t = [AR.alloc([512], F32, f"rden{i}") for i in range(1)]
        st_t = [AR.alloc([4], F32, f"st{i}") for i in range(4)]
        fl_t, fl_b = AR.alloc([2, 512], F32, "fl")
        x_mark = AR.mark()
        xt_t = [AR.alloc([D], F32, f"xt{i}") for i in range(2)]
        xn_t = [AR.alloc([D], BF16, f"xn{i}") for i in range(2)]
        junk, junk_b = AR.alloc([D], BF16, "junk")
        wo_sb, wo_b = AR.alloc([8, D], BF16, "w_o")
        gffn_b, gffn_bb = AR.alloc([D], F32, "gffn_b")
        h1_t = [AR.alloc([D], F32, f"h1t{i}") for i in range(2)]
        u2f_t = [AR.alloc([D], F32, f"u2f{i}") for i in range(2)]
        u2b_t = [AR.alloc([D], BF16, f"u2b{i}") for i in range(2)]
        u2T_t = [AR.alloc([8, 128], F32, f"u2T{i}") for i in range(2)]
        x_end1 = AR.mark()
        AR.reset(x_mark)
        btab = [AR.alloc([XW], F32, f"btab{i}") for i in range(2)]
        NPB = 2
        wq_sb = [AR.alloc([3, 8, 128], BF16, f"wqkv{i}") for i in range(NPB)]
        qT = [AR.alloc([SEQ], BF16, f"qT{i}") for i in range(NPB)]
        kT = [AR.alloc([SEQ], BF16, f"kT{i}") for i in range(NPB)]
        vpad = [AR.alloc([2, 16, 128], BF16, f"vpad{i}") for i in range(NPB)]
        AR.reset(max(x_end1, AR.mark()))

        PS_TR = [psum[0], psum[1]]
        PS_PJ = [psum[2], psum[3]]
        PS_S = [psum[4], psum[5]]
        PS_S4 = [psum[4], psum[5], psum[0], psum[1]]
        tmp4 = tmp_t + [(fl_t[:, 0, :], fl_b)]
        pt4 = pt_t + [(fl_t[:, 1, 0:256].bitcast(BF16), fl_b)]
        NB4 = len(tmp4)
        PS_NUM = psum[6]
        PS_DEN = psum[7]
        ctr = {"pj": 0, "s": 0, "tmp": 0, "sq": 0, "st": 0, "x": 0, "h1": 0}

        def rot(key, lst):
            i = ctr[key]
            ctr[key] = i + 1
            return lst[i % len(lst)]

        def rms_stats(src, src_b, st, st_b):
            P.add("act", lambda e: e.activation(out=junk, in_=src, func=AF.Square, accum_out=st[:, 0:1]),
                  reads=[src_b], writes=[junk_b, st_b])
            P.add("act", lambda e: e.activation(out=st[:, 1:2], in_=st[:, 0:1], func=AF.Ln, bias=ccol[:, 2:3], scale=1.0 / D),
                  reads=[ccol_b], writes=[st_b])
            P.add("act", lambda e: e.activation(out=st[:, 2:3], in_=st[:, 1:2], func=AF.Exp, scale=-0.5),
                  reads=[], writes=[st_b])

        def transpose8(src, src_b, ident, ident_buf, nchunks=8):
            for half in range((nchunks + 3) // 4):
                pv, pb = PS_TR[half]
                for c in range(4):
                    cc = half * 4 + c
                    if cc >= nchunks:
                        break
                    kw = dict(writes=[pb]) if c == 0 else dict(accum=[pb])
                    P.add("pe", lambda e, pv=pv, c=c, cc=cc: e.matmul(pv[:, c * 128:(c + 1) * 128], lhsT=src[:, cc * 128:(cc + 1) * 128], rhs=ident, start=True, stop=True),
                          reads=[src_b, ident_buf], **kw)

        def phase_a(s):
            tok0 = s * SEQ
            for i in range(16):
                xt, xt_b = rot("x", xt_t)
                xn, xn_b = xn_t[(ctr["x"] - 1) % 2]
                st, st_b = rot("st", st_t)
                r0 = tok0 + i * 128
                P.add("sp", lambda e, xt=xt, r0=r0: e.dma_start(out=xt, in_=x_d[r0:r0 + 128, :]), writes=[xt_b], dma=f"x{(ctr['x'] - 1) % 2}")
                rms_stats(xt, xt_b, st, st_b)
                P.add("act", lambda e, xn=xn, xt=xt, st=st: e.activation(out=xn, in_=xt, func=AF.Copy, scale=st[:, 2:3]),
                      reads=[xt_b, st_b], writes=[xn_b])
                transpose8(xn, xn_b, ident_b, ident_b_b)
                for half in range(2):
                    pv, pb = PS_TR[half]
                    P.add("dve", lambda e, pv=pv, half=half, i=i: e.tensor_tensor(
                        out=uT[:, half * 4:(half + 1) * 4, i * 128:(i + 1) * 128],
                        in0=pv.rearrange("p (c t) -> p c t", c=4),
                        in1=gT[:, 0, half * 4:(half + 1) * 4].unsqueeze(2).to_broadcast([128, 4, 128]), op=ALU.mult),
                        reads=[pb, gT_b], accum=[uT_b])
            checkpoint('A0')
            P.barrier()
            if s == 0:
                for c in range(NSLOT * D // 128 // 2048):
                    P.add("sp", lambda e, c=c: e.dma_start(out=xs_flat[:, c * 2048:(c + 1) * 2048], in_=zt), reads=[zt_b], accum=[dram_xs0], dma="xszero")
            for i in range(NPB):
                P.add("pool", lambda e, i=i: e.memset(vpad[i][0], 1.0), writes=[vpad[i][1]])
            for g in range(4):
                pv, pb = rot("pj", PS_PJ)
                for kc in range(8):
                    kw = dict(writes=[pb]) if kc == 0 else dict(accum=[pb])
                    P.add("pe", lambda e, pv=pv, kc=kc, g=g: e.matmul(pv[0:8, :], lhsT=wf_sb[:, kc, :], rhs=uT[:, kc, g * 512:(g + 1) * 512], start=(kc == 0), stop=(kc == 7)),
                          reads=[wf_b, uT_b], **kw)
                P.add("act", lambda e, pv=pv: e.activation(out=fl_t[0:8, 0, :], in_=pv[0:8, :], func=AF.Exp, bias=nbf[0:8, :], scale=-1.0),
                      reads=[pb, nbf_b], writes=[fl_b])
                P.add("act", lambda e: e.activation(out=fl_t[0:8, 1, :], in_=fl_t[0:8, 0, :], func=AF.Ln, bias=ccol[0:8, 1:2], scale=1.0),
                      reads=[fl_b, ccol_b], writes=[fl_b])
                if g == 0:
                    P.add("dve", lambda e: e.tensor_tensor_scan(out=csum[0:8, 0:512], data0=ones8[0:8, 0:512], data1=fl_t[0:8, 1, :], initial=0.0, op0=ALU.mult, op1=ALU.subtract),
                          reads=[fl_b, ones8_b], writes=[csum_b])
                else:
                    P.add("dve", lambda e, g=g: e.tensor_tensor_scan(out=csum[0:8, g * 512:(g + 1) * 512], data0=ones8[0:8, 0:512], data1=fl_t[0:8, 1, :],
                                                                initial=csum[0:8, g * 512 - 1:g * 512], op0=ALU.mult, op1=ALU.subtract),
                          reads=[fl_b, ones8_b, csum_b], writes=[csum_b])
            pv, pb = rot("pj", PS_PJ)
            for j in range(16):
                kw = dict(writes=[pb]) if j == 0 else dict(accum=[pb])
                P.add("pe", lambda e, pv=pv, j=j: e.matmul(pv[:, j * 8:(j + 1) * 8], lhsT=csum[0:8, j * 128:(j + 1) * 128], rhs=ident_f[0:8, 0:8], start=True, stop=True),
                      reads=[csum_b, ident_f_b], **kw)
            P.add("act", lambda e, pv=pv: e.activation(out=nck.rearrange("p a b -> p (a b)"), in_=pv[:, 0:128], func=AF.Copy, scale=-1.0),
                  reads=[pb], writes=[nck_b])

            checkpoint('fox')
            w_in_v = w_in_d.rearrange("(c p) n -> p c n", p=128)

            def proj_chunks(hp):
                isA_ = hp < 4
                hpl_ = hp % 4
                base_ = 0 if isA_ else 1536
                wq, wq_b = wq_sb[hp % NPB]
                q_sb_, q_b_ = qT[hp % NPB]
                k_sb_, k_b_ = kT[hp % NPB]
                v_sb_, v_b_ = vpad[hp % NPB]
                chunks = []

                def c_load():
                    for t in range(3):
                        c0 = base_ + t * 512 + hpl_ * 128
                        P.add("pool", lambda e, t=t, c0=c0: e.dma_start(out=wq[:, t], in_=w_in_v[:, :, c0:c0 + 128]),
                              **(dict(writes=[wq_b]) if t == 0 else dict(accum=[wq_b])), dma=f"wqkv{hp % NPB}")
                chunks.append(c_load)

                def mk_qk(t, dst, dst_b, gcol, g):
                    def c_qk():
                        pv, pb = rot("pj", PS_PJ)
                        for kc in range(8):
                            kw = dict(writes=[pb]) if kc == 0 else dict(accum=[pb])
                            P.add("pe", lambda e, pv=pv, kc=kc: e.matmul(pv, lhsT=wq[:, t, kc, :], rhs=uT[:, kc, g * 512:(g + 1) * 512], start=(kc == 0), stop=(kc == 7)),
                                  reads=[wq_b, uT_b], **kw)
                        sq, sq_b = rot("sq", sq_t)
                        rs, rs_b = rs_t[(ctr["sq"] - 1) % 2]
                        P.add("act", lambda e, pv=pv, sq=sq: e.activation(out=sq, in_=pv, func=AF.Square), reads=[pb], writes=[sq_b])
                        pv2, pb2 = rot("pj", PS_PJ)
                        P.add("pe", lambda e, pv2=pv2, sq=sq: e.matmul(pv2, lhsT=blk64, rhs=sq, start=True, stop=True), reads=[sq_b, blk64_b], writes=[pb2])
                        P.add("act", lambda e, pv2=pv2, rs=rs: e.activation(out=rs, in_=pv2, func=AF.Ln, bias=ccol[:, 0:1], scale=1.0),
                              reads=[pb2, ccol_b], writes=[rs_b])
                        P.add("act", lambda e, rs=rs: e.activation(out=rs, in_=rs, func=AF.Exp, scale=-0.5),
                              reads=[rs_b], writes=[rs_b])
                        P.add("dve", lambda e, pv=pv, rs=rs: e.scalar_tensor_tensor(
                            out=dst[:, g * 512:(g + 1) * 512], in0=pv, scalar=gqk[:, gcol:gcol + 1], in1=rs, op0=ALU.mult, op1=ALU.mult),
                            reads=[pb, rs_b, gqk_b], accum=[dst_b])
                    return c_qk
                for t, (dst, dst_b, gcol) in enumerate([(q_sb_, q_b_, 0 if isA_ else 2), (k_sb_, k_b_, 1 if isA_ else 3)]):
                    for g in range(4):
                        chunks.append(mk_qk(t, dst, dst_b, gcol, g))

                def mk_v(g):
                    def c_v():
                        pv, pb = rot("pj", PS_PJ)
                        for tt in range(4):
                            j = g * 4 + tt
                            for kc in range(8):
                                kw = dict(writes=[pb]) if (tt == 0 and kc == 0) else dict(accum=[pb])
                                P.add("pe", lambda e, pv=pv, kc=kc, j=j, tt=tt: e.matmul(pv[:, tt * 128:(tt + 1) * 128], lhsT=uT[:, kc, j * 128:(j + 1) * 128], rhs=wq[:, 2, kc, :], start=(kc == 0), stop=(kc == 7)),
                                      reads=[wq_b, uT_b], **kw)
                        pvv = pv.rearrange("p (t c) -> p t c", t=4)
                        P.add("act", lambda e, pvv=pvv: e.activation(out=v_sb_[:, 0, g * 4:(g + 1) * 4, 0:64], in_=pvv[:, :, 0:64], func=AF.Copy),
                              reads=[pb, v_b_], accum=[v_b_])
                        P.add("dve", lambda e, pvv=pvv: e.tensor_copy(out=v_sb_[:, 1, g * 4:(g + 1) * 4, 64:128], in_=pvv[:, :, 64:128]),
                              reads=[pb, v_b_], accum=[v_b_])
                    return c_v
                for g in range(4):
                    chunks.append(mk_v(g))
                return chunks

            for c_ in proj_chunks(0):
                c_()
            for hp in range(8):
                isA = hp < 4
                hpl = hp % 4
                q_sb, q_b = qT[hp % NPB]
                k_sb, k_b = kT[hp % NPB]
                v_sb, v_b = vpad[hp % NPB]
                next_chunks = proj_chunks(hp + 1) if hp + 1 < 8 else []
                bt = []
                for hh in range(2):
                    h = hpl * 2 + hh
                    tb, tb_b = btab[hh]
                    if isA:
                        P.add("sp", lambda e, tb=tb, h=h: e.dma_start(out=tb, in_=btabA_d[h]), writes=[tb_b], dma=f"btab{hh}")
                    else:
                        for g in range(4):
                            pv, pb = rot("pj", PS_PJ)
                            P.add("pe", lambda e, pv=pv, h=h, g=g: e.matmul(pv, lhsT=sel8[0:8, h * 128:(h + 1) * 128], rhs=csum[0:8, g * 512:(g + 1) * 512], start=True, stop=True),
                                  reads=[sel8_b, csum_b], writes=[pb])
                            P.add("act", lambda e, pv=pv, tb=tb, g=g: e.activation(out=tb[:, g * 512:(g + 1) * 512], in_=pv, func=AF.Copy),
                                  reads=[pb], **(dict(writes=[tb_b]) if g == 0 else dict(accum=[tb_b])))
                    bt.append((tb, tb_b))
                LOOK = 5
                tiles = [(qg, j, hh) for qg in range(4) for j in range(4 * qg + 4) for hh in range(2)]
                numv, numb = PS_NUM
                denv, denb = PS_DEN
                pend = {}

                def emit_score(n):
                    qg, j, hh = tiles[n]
                    h = hpl * 2 + hh
                    tb, tb_b = bt[hh]
                    sv, sb = PS_S4[n % len(PS_S4)]
                    lo, hi = hh * 64, hh * 64 + 64
                    P.add("pe", lambda e, sv=sv, lo=lo, hi=hi, j=j, qg=qg, k_sb=k_sb, q_sb=q_sb: e.matmul(
                        sv, lhsT=k_sb[lo:hi, j * 128:(j + 1) * 128], rhs=q_sb[lo:hi, qg * 512:(qg + 1) * 512], start=True, stop=True),
                        reads=[k_b, q_b], writes=[sb])
                    tm, tm_b = tmp4[n % NB4]
                    pt, pt_b = pt4[n % NB4]
                    T = 512 * qg - 128 * j
                    if isA:
                        x0 = T + 384
                        P.add("dve", lambda e, tm=tm, sv=sv, tb=tb, x0=x0: e.tensor_tensor(out=tm, in0=sv, in1=tb[:, x0:x0 + 512], op=ALU.add),
                              reads=[sb, tb_b], writes=[tm_b])
                        P.add("act", lambda e, tm=tm, pt=pt: e.activation(out=pt, in_=tm, func=AF.Exp), reads=[tm_b], writes=[pt_b])
                    else:
                        P.add("dve", lambda e, tm=tm, sv=sv, tb=tb, qg=qg: e.tensor_tensor(out=tm, in0=sv, in1=tb[:, qg * 512:(qg + 1) * 512], op=ALU.add),
                              reads=[sb, tb_b], writes=[tm_b])
                        if T <= 0:
                            x0 = T + 384
                            P.add("pool", lambda e, tm=tm, x0=x0: e.tensor_tensor(out=tm, in0=tm, in1=cbtab[:, x0:x0 + 512], op=ALU.add),
                                  reads=[tm_b, cbtab_b], writes=[tm_b])
                        P.add("act", lambda e, tm=tm, pt=pt, j=j, h=h: e.activation(out=pt, in_=tm, func=AF.Exp, bias=nck[:, j, h:h + 1]),
                              reads=[tm_b, nck_b], writes=[pt_b])
                    pend[n] = (pt, pt_b)

                def emit_pv(n):
                    qg, j, hh = tiles[n]
                    pt, pt_b = pend.pop(n)
                    first = (j == 0)
                    last = (j == 4 * qg + 3)
                    accv, accb = (numv, numb) if hh == 0 else (denv, denb)
                    P.add("pe", lambda e, pt=pt, hh=hh, j=j, first=first, last=last, v_sb=v_sb, accv=accv: e.matmul(accv, lhsT=v_sb[:, hh, j, :], rhs=pt, start=first, stop=last),
                          reads=[pt_b, v_b], **(dict(writes=[accb]) if first else dict(accum=[accb])))
                    if last:
                        rd, rd_b = rden_t[0]
                        nlo, nhi = hh * 64, hh * 64 + 64
                        dlo, dhi = (1 - hh) * 64, (1 - hh) * 64 + 64
                        P.add("dve", lambda e, rd=rd, accv=accv, nlo=nlo, nhi=nhi, dlo=dlo, dhi=dhi: e.reciprocal(out=rd[nlo:nhi, :], in_=accv[dlo:dhi, :]),
                              reads=[accb], writes=[rd_b])
                        P.add("dve", lambda e, rd=rd, qg=qg, hp=hp, accv=accv, nlo=nlo, nhi=nhi: e.tensor_tensor(
                            out=mixT[nlo:nhi, hp, qg * 512:(qg + 1) * 512], in0=accv[nlo:nhi, :], in1=rd[nlo:nhi, :], op=ALU.mult),
                              reads=[accb, rd_b], accum=[mixT_b])

                step = max(1, (len(tiles) - 8) // max(1, len(next_chunks)))
                for n in range(len(tiles) + LOOK):
                    if n < len(tiles):
                        emit_score(n)
                    if n - LOOK >= 0:
                        emit_pv(n - LOOK)
                    if next_chunks and n >= 4 and (n - 4) % step == 0:
                        next_chunks.pop(0)()
                while next_chunks:
                    next_chunks.pop(0)()
            checkpoint('A1')
            if debug:
                P.add("sp", lambda e, s=s: e.dma_start(out=dbg["mix"][s], in_=mixT.rearrange("p c t -> p (c t)")), reads=[mixT_b], dma="dbgmix")
            P.barrier()
            P.add("pool", lambda e: e.dma_start(out=wo_sb, in_=w_o_d.rearrange("(c p) n -> p c n", p=128)), writes=[wo_b], dma="wo")
            P.add("sp", lambda e: e.dma_start(out=gffn_b, in_=gvec_d[1:2, :].partition_broadcast(128)), writes=[gffn_bb], dma="gffnb")
            for i in range(16):
                ti = s * 16 + i
                r0 = tok0 + i * 128
                xt, xt_b = rot("x", xt_t)
                P.add("sp", lambda e, xt=xt, r0=r0: e.dma_start(out=xt, in_=x_d[r0:r0 + 128, :]), writes=[xt_b], dma=f"x{(ctr['x'] - 1) % 2}")
                h1, h1_b = rot("h1", h1_t)
                hi_ = (ctr["h1"] - 1) % 2
                u2f, u2f_b = u2f_t[hi_]
                u2b, u2b_b = u2b_t[hi_]
                u2T, u2T_b = u2T_t[hi_]
                st, st_b = rot("st", st_t)
                for dh in range(2):
                    pv, pb = rot("pj", PS_PJ)
                    for c in range(8):
                        kw = dict(writes=[pb]) if c == 0 else dict(accum=[pb])
                        P.add("pe", lambda e, pv=pv, c=c, i=i, dh=dh: e.matmul(pv, lhsT=mixT[:, c, i * 128:(i + 1) * 128], rhs=wo_sb[:, c, dh * 512:(dh + 1) * 512], start=(c == 0), stop=(c == 7)),
                              reads=[mixT_b, wo_b], **kw)
                    P.add("dve", lambda e, pv=pv, h1=h1, xt=xt, dh=dh: e.tensor_tensor(out=h1[:, dh * 512:(dh + 1) * 512], in0=pv, in1=xt[:, dh * 512:(dh + 1) * 512], op=ALU.add),
                          reads=[pb, xt_b], **(dict(writes=[h1_b]) if dh == 0 else dict(accum=[h1_b])))
                P.add("sp", lambda e, h1=h1, r0=r0: e.dma_start(out=h1_scr[r0:r0 + 128, :], in_=h1), reads=[h1_b], accum=[dram_h1], dma=f"h1st{hi_}")
                rms_stats(h1, h1_b, st, st_b)
                P.add("dve", lambda e, u2f=u2f, h1=h1, st=st: e.scalar_tensor_tensor(out=u2f, in0=h1, scalar=st[:, 2:3], in1=gffn_b, op0=ALU.mult, op1=ALU.mult),
                      reads=[h1_b, st_b, gffn_bb], writes=[u2f_b])
                transpose8(u2f, u2f_b, ident_f, ident_f_b)
                for half in range(2):
                    pv, pb = PS_TR[half]
                    P.add("act", lambda e, pv=pv, half=half, u2T=u2T: e.activation(out=u2T[:, half * 4:(half + 1) * 4, :], in_=pv.rearrange("p (c t) -> p c t", c=4), func=AF.Copy),
                          reads=[pb], **(dict(writes=[u2T_b]) if half == 0 else dict(accum=[u2T_b])))
                P.add("pool", lambda e, u2f=u2f, u2b=u2b: e.tensor_copy(out=u2b, in_=u2f), reads=[u2f_b], writes=[u2b_b])
                P.add("sp", lambda e, u2b=u2b, r0=r0: e.dma_start(out=u2_scr[r0:r0 + 128, :], in_=u2b), reads=[u2b_b], accum=[dram_u2], dma=f"u2st{hi_}")
                pv, pb = rot("pj", PS_PJ)
                for kc in range(8):
                    kw = dict(writes=[pb]) if kc == 0 else dict(accum=[pb])
                    P.add("pe", lambda e, pv=pv, kc=kc, u2T=u2T: e.matmul(pv[:, 0:NE], lhsT=u2T[:, kc, :], rhs=wr_sb[:, kc, :], start=(kc == 0), stop=(kc == 7)),
                          reads=[u2T_b, wr_b], **kw)
                P.add("dve", lambda e, pv=pv, ti=ti: e.tensor_tensor(out=logits_all[:, ti, :], in0=pv[:, 0:NE], in1=brb, op=ALU.add),
                      reads=[pb, brb_b], accum=[logits_b])
            P.barrier()
        try:
            for s_ in range(NSEQ):
                phase_a(s_)
        except StopBuild:
            pass
        if debug:
            P.add("sp", lambda e: e.dma_start(out=dbg["logits"], in_=logits_all.rearrange("p a b -> p (a b)")), reads=[logits_b], dma="dbglog")


        P.barrier()
        AR.reset(0)
        dram_xs = Buf("xs_scr")
        dram_y = Buf("y_scr")
        comb, comb_b = CA.alloc([NT, NE], F32, "comb")
        g4, g4_b = CA.alloc([NT, 4], F32, "g4")
        sel_i, sel_i_b = CA.alloc([4, NT], I32, "sel_i")
        idx_gu, idx_gu_b = CA.alloc([NMT], I32, "idx_gu")
        idx_dn, idx_dn_b = CA.alloc([NMT], I32, "idx_dn")
        idx_bg, idx_bg_b = CA.alloc([NMT], I32, "idx_bg")
        mx8, mx8_b = AR.alloc([NT, 8], F32, "mx8")
        rt = [AR.alloc([NE], F32, f"rt{i}") for i in range(3)]
        rs1, rs1_b = AR.alloc([NT, 4], F32, "rs1")
        mask_bf, mask_bf_b = AR.alloc([NT, NE], BF16, "mask_bf")
        runs, runs_b = AR.alloc([NT + 1, NE], F32, "runs")
        rank_all, rank_b = AR.alloc([NT, NE], F32, "rank_all")
        slotf, slotf_b = AR.alloc([NT, NE], F32, "slotf")
        eqt, eqt_b = AR.alloc([NT, NE], F32, "eqt")
        sel_f, sel_f_b = AR.alloc([4, NT], F32, "sel_f")
        sm = [AR.alloc([NE], F32, f"sm{i}") for i in range(6)]
        etf, etf_b = AR.alloc([NMT], F32, "etf")
        etc2, etc2_b = AR.alloc([NMT], F32, "etc2")
        idf, idf_b = AR.alloc([3, NMT], F32, "idf")
        d4, d4_b = AR.alloc([NT, 4], F32, "d4")
        u2ld = [AR.alloc([D], BF16, f"u2ld{i}") for i in range(4)]
        msk3, msk3_b = AR.alloc([NT, NE], F32, "msk3")
        ex3, ex3_b = AR.alloc([NT, NE], F32, "ex3")
        ssum, ssum_b = AR.alloc([NT], F32, "ssum")
        rsum, rsum_b = AR.alloc([NT], F32, "rsum")
        for ti in range(NT):
            P.add("dve", lambda e, ti=ti: e.max(out=mx8[:, ti, :], in_=logits_all[:, ti, :]), reads=[logits_b], accum=[mx8_b])
        P.add("dve", lambda e: e.tensor_tensor(out=msk3, in0=logits_all, in1=mx8[:, :, 3:4].to_broadcast([128, NT, NE]), op=ALU.is_ge),
              reads=[logits_b, mx8_b], writes=[msk3_b])
        P.add("dve", lambda e: e.tensor_copy(out=mask_bf, in_=msk3), reads=[msk3_b], writes=[mask_bf_b])
        P.add("dve", lambda e: e.tensor_tensor(out=ex3, in0=logits_all, in1=mx8[:, :, 0:1].to_broadcast([128, NT, NE]), op=ALU.subtract),
              reads=[logits_b, mx8_b], writes=[ex3_b])
        P.add("act", lambda e: e.activation(out=ex3, in_=ex3, func=AF.Exp), reads=[ex3_b], writes=[ex3_b])
        P.add("dve", lambda e: e.tensor_tensor(out=ex3, in0=ex3, in1=msk3, op=ALU.mult), reads=[ex3_b, msk3_b], writes=[ex3_b])
        P.add("dve", lambda e: e.reduce_sum(out=ssum, in_=ex3, axis=AX.X), reads=[ex3_b], writes=[ssum_b])
        P.add("dve", lambda e: e.reciprocal(out=rsum, in_=ssum), reads=[ssum_b], writes=[rsum_b])
        P.add("dve", lambda e: e.tensor_tensor(out=comb, in0=ex3, in1=rsum.unsqueeze(2).to_broadcast([128, NT, NE]), op=ALU.mult),
              reads=[ex3_b, rsum_b], writes=[comb_b])
        P.add("dve", lambda e: e.tensor_tensor(out=d4, in0=mx8[:, :, 0:4], in1=mx8[:, :, 0:1].to_broadcast([128, NT, 4]), op=ALU.subtract),
              reads=[mx8_b], writes=[d4_b])
        P.add("act", lambda e: e.activation(out=d4, in_=d4, func=AF.Exp), reads=[d4_b], writes=[d4_b])
        P.add("dve", lambda e: e.tensor_tensor(out=g4, in0=d4, in1=rsum.unsqueeze(2).to_broadcast([128, NT, 4]), op=ALU.mult),
              reads=[d4_b, rsum_b], writes=[g4_b])
        mflat = mask_bf.rearrange("p a b -> p (a b)")
        for half in range(2):
            pv, pb = psum[half]
            P.add("pe", lambda e, pv=pv, half=half: e.matmul(pv, lhsT=ustrict, rhs=mflat[:, half * 512:(half + 1) * 512], start=True, stop=True),
                  reads=[ustrict_b, mask_bf_b], writes=[pb])
            pv2, pb2 = psum[2 + half]
            P.add("pe", lambda e, pv2=pv2, half=half: e.matmul(pv2, lhsT=ones_b, rhs=mflat[:, half * 512:(half + 1) * 512], start=True, stop=True),
                  reads=[ones_b_b, mask_bf_b], writes=[pb2])
        P.add("dve", lambda e: e.memset(runs[:, 0, :], 0.0), writes=[runs_b])
        for i in range(NT):
            pv2, pb2 = psum[2 + i // 16]
            P.add("dve", lambda e, i=i, pv2=pv2: e.tensor_tensor(out=runs[:, i + 1, :], in0=runs[:, i, :], in1=pv2[:, (i % 16) * NE:(i % 16 + 1) * NE], op=ALU.add),
                  reads=[pb2, runs_b], writes=[runs_b])
        for half in range(2):
            pv, pb = psum[half]
            P.add("dve", lambda e, pv=pv, half=half: e.tensor_tensor(out=rank_all[:, half * 16:(half + 1) * 16, :], in0=pv.rearrange("p (a b) -> p a b", a=16),
                                                            in1=runs[:, half * 16:(half + 1) * 16, :], op=ALU.add),
                  reads=[pb, runs_b], **(dict(writes=[rank_b]) if half == 0 else dict(accum=[rank_b])))
        n_e = runs[:, NT, :]
        (ntl, ntl_b), (inc, inc_b), (bas, bas_b), (one32, one32_b), (cmpt, cmpt_b), _ = sm
        P.add("dve", lambda e: e.memset(ntl, 0.0), writes=[ntl_b])
        P.add("dve", lambda e: e.memset(one32, 1.0), writes=[one32_b])
        for j in range(NTOK // TS):
            P.add("dve", lambda e, j=j: e.scalar_tensor_tensor(out=ntl, in0=n_e, scalar=float(TS * j), in1=ntl, op0=ALU.is_gt, op1=ALU.add),
                  reads=[runs_b, ntl_b], writes=[ntl_b])
        P.add("dve", lambda e: e.tensor_tensor_scan(out=inc, data0=one32, data1=ntl, initial=0.0, op0=ALU.mult, op1=ALU.add),
              reads=[one32_b, ntl_b], writes=[inc_b])
        P.add("dve", lambda e: e.tensor_tensor(out=bas, in0=inc, in1=ntl, op=ALU.subtract), reads=[inc_b, ntl_b], writes=[bas_b])
        P.add("dve", lambda e: e.tensor_scalar(out=bas, in0=bas, scalar1=float(TS), scalar2=None, op0=ALU.mult), reads=[bas_b], writes=[bas_b])
        P.add("dve", lambda e: e.tensor_tensor(out=slotf, in0=rank_all, in1=bas.unsqueeze(1).to_broadcast([128, NT, NE]), op=ALU.add),
              reads=[rank_b, bas_b], writes=[slotf_b])
        for j in range(4):
            P.add("dve", lambda e, j=j: e.tensor_tensor(out=eqt, in0=logits_all, in1=mx8[:, :, j:j + 1].to_broadcast([128, NT, NE]), op=ALU.is_equal),
                  reads=[logits_b, mx8_b], writes=[eqt_b])
            P.add("dve", lambda e: e.tensor_tensor(out=eqt, in0=eqt, in1=slotf, op=ALU.mult), reads=[eqt_b, slotf_b], writes=[eqt_b])
            P.add("dve", lambda e, j=j: e.reduce_sum(out=sel_f[:, j, :], in_=eqt, axis=AX.X), reads=[eqt_b], **(dict(writes=[sel_f_b]) if j == 0 else dict(accum=[sel_f_b])))
        P.add("dve", lambda e: e.tensor_copy(out=sel_i, in_=sel_f), reads=[sel_f_b], writes=[sel_i_b])
        tvals = misc[:, 1:1 + NMT]
        P.add("dve", lambda e: e.memset(etf, 0.0), writes=[etf_b])
        for ex_i in range(NE):
            P.add("dve", lambda e, ex_i=ex_i: e.scalar_tensor_tensor(out=etf, in0=tvals, scalar=inc[:, ex_i:ex_i + 1], in1=etf, op0=ALU.is_ge, op1=ALU.add),
                  reads=[misc_b, inc_b, etf_b], writes=[etf_b])
        P.add("dve", lambda e: e.tensor_scalar(out=etc2, in0=etf, scalar1=float(NE - 1), scalar2=None, op0=ALU.min), reads=[etf_b], writes=[etc2_b])
        for r, mult in enumerate((4.0, 2.0, 1.0)):
            src, src_b = (etf, etf_b) if r == 0 else (etc2, etc2_b)
            P.add("dve", lambda e, r=r, mult=mult, src=src: e.tensor_scalar(out=idf[:, r, :], in0=src, scalar1=128.0, scalar2=misc[:, 0:1], op0=ALU.mult, op1=ALU.add),
                  reads=[src_b, misc_b], **(dict(writes=[idf_b]) if r == 0 else dict(accum=[idf_b])))
            if mult != 1.0:
                P.add("dve", lambda e, r=r, mult=mult: e.tensor_scalar(out=idf[:, r, :], in0=idf[:, r, :], scalar1=mult, scalar2=None, op0=ALU.mult),
                      reads=[idf_b], accum=[idf_b])
        P.add("dve", lambda e: e.tensor_copy(out=idx_gu, in_=idf[:, 0, :]), reads=[idf_b], writes=[idx_gu_b])
        P.add("dve", lambda e: e.tensor_copy(out=idx_dn, in_=idf[:, 1, :]), reads=[idf_b], writes=[idx_dn_b])
        P.add("dve", lambda e: e.tensor_copy(out=idx_bg, in_=idf[:, 2, :]), reads=[idf_b], writes=[idx_bg_b])
        if debug:
            P.add("sp", lambda e: e.dma_start(out=dbg["route"][:, 0:128], in_=sel_f.rearrange("p a b -> p (a b)")), reads=[sel_f_b], dma="dbgroute")
            P.add("sp", lambda e: e.dma_start(out=dbg["route"][:, 128:128 + NMT], in_=etf), reads=[etf_b], dma="dbgroute")
            P.add("sp", lambda e: e.dma_start(out=dbg["route"][:, 256:256 + NE], in_=n_e), reads=[runs_b], dma="dbgroute")
            P.add("sp", lambda e: e.dma_start(out=dbg["route"][:, 320:320 + 128], in_=sel_i.rearrange("p a b -> p (a b)").bitcast(F32)), reads=[sel_i_b], dma="dbgroute")
        for i in range(NT):
            ut, ut_b = u2ld[i % 4]
            P.add("sp", lambda e, ut=ut, i=i: e.dma_start(out=ut, in_=u2_scr[i * 128:(i + 1) * 128, :]), reads=[dram_u2], writes=[ut_b], dma=f"u2ld{i % 4}")
            for j in range(4):
                P.add("pool", lambda e, ut=ut, i=i, j=j: e.indirect_dma_start(out=xs_scr[:, :], out_offset=bass.IndirectOffsetOnAxis(ap=sel_i[:, j, i:i + 1], axis=0),
                                                                        in_=ut, in_offset=None),
                      reads=[ut_b, sel_i_b, dram_xs0], accum=[dram_xs], dma=f"scat{i % 4}")

        P.barrier()
        AR.reset(0)
        wgu_sb = [AR.alloc([8, 2048], BF16, f"wgu{i}") for i in range(2)]
        wd_sb = [AR.alloc([8, D], BF16, f"wd{i}") for i in range(2)]
        bgu_sb = [AR.alloc([16], F32, f"bgu{i}") for i in range(2)]
        xs_sb = [AR.alloc([4, D], BF16, f"xs{i}") for i in range(2)]
        xT_sb = [AR.alloc([8, TS], BF16, f"xT{i}") for i in range(2)]
        actT = [AR.alloc([8, TS], BF16, f"actT{i}") for i in range(2)]
        sw2 = [[AR.alloc([512], F32, f"sw{j}_{i}") for i in range(4)] for j in range(2)]
        yst = [AR.alloc([D], F32, f"yst{i}") for i in range(4)]
        wgu_rows = wgu_d
        wd_rows = wd_d
        xs_v = xs_scr.rearrange("(t s p) d -> t p s d", s=4, p=128)
        PS_T = [psum[0], psum[1]]
        PS_G = [psum[2], psum[3], psum[4], psum[5]]
        PS_D = [psum[6], psum[7]]
        cnt = {"d": 0, "y": 0, "ev": 0}

        _regs = {}

        def bound_reg(e, val):
            if val not in _regs:
                _regs[val] = e.to_reg(val)
            return _regs[val]

        def load_tile(t):
            wi = t % 2
            wg, wg_b = wgu_sb[wi]
            wdd, wdd_b = wd_sb[wi]
            bg, bg_b = bgu_sb[wi]
            xs, xs_b = xs_sb[wi]
            for c in range(4):
                P.add("pool", lambda e, wg=wg, t=t, c=c: e.indirect_dma_start(
                    out=wg.rearrange("p a b -> p (a b)")[:, c * 4096:(c + 1) * 4096], out_offset=None, in_=wgu_rows[:, :],
                    in_offset=bass.IndirectOffsetOnAxis(ap=idx_gu[:, t:t + 1], axis=0), element_offset=c * 4096,
                    bounds_check=bound_reg(e, NE * 128 * 4 - 1), oob_is_err=False),
                    reads=[idx_gu_b], **(dict(writes=[wg_b]) if c == 0 else dict(accum=[wg_b])), dma=f"wgu{wi}")
            for c in range(2):
                P.add("pool", lambda e, wdd=wdd, t=t, c=c: e.indirect_dma_start(
                    out=wdd.rearrange("p a b -> p (a b)")[:, c * 4096:(c + 1) * 4096], out_offset=None, in_=wd_rows[:, :],
                    in_offset=bass.IndirectOffsetOnAxis(ap=idx_dn[:, t:t + 1], axis=0), element_offset=c * 4096),
                    reads=[idx_dn_b], **(dict(writes=[wdd_b]) if c == 0 else dict(accum=[wdd_b])), dma=f"wd{wi}")
            P.add("pool", lambda e, bg=bg, t=t: e.indirect_dma_start(out=bg, out_offset=None, in_=bgu_d[:, :],
                                                                in_offset=bass.IndirectOffsetOnAxis(ap=idx_bg[:, t:t + 1], axis=0)),
                  reads=[idx_bg_b], writes=[bg_b], dma=f"bgu{wi}")
            P.add("sp", lambda e, xs=xs, t=t: e.dma_start(out=xs, in_=xs_v[t]), reads=[dram_xs, dram_xs0], writes=[xs_b], dma=f"xs{wi}")

        def transposes(t):
            xs, xs_b = xs_sb[t % 2]
            xT, xT_b = xT_sb[t % 2]
            for kc in range(8):
                pv, pb = PS_T[kc % 2]
                for sub in range(4):
                    kw = dict(writes=[pb]) if sub == 0 else dict(accum=[pb])
                    P.add("pe", lambda e, pv=pv, sub=sub, kc=kc, xs=xs: e.matmul(pv[:, sub * 128:(sub + 1) * 128], lhsT=xs[:, sub, kc * 128:(kc + 1) * 128], rhs=ident_b, start=True, stop=True),
                          reads=[xs_b, ident_b_b], **kw)
                kw = dict(writes=[xT_b]) if kc == 0 else dict(accum=[xT_b])
                if kc % 2 == 0:
                    P.add("act", lambda e, pv=pv, kc=kc, xT=xT: e.activation(out=xT[:, kc, :], in_=pv, func=AF.Copy), reads=[pb], **kw)
                else:
                    P.add("dve", lambda e, pv=pv, kc=kc, xT=xT: e.tensor_copy(out=xT[:, kc, :], in_=pv), reads=[pb], **kw)

        def gate_up(t):
            wg, wg_b = wgu_sb[t % 2]
            bg, bg_b = bgu_sb[t % 2]
            xT, xT_b = xT_sb[t % 2]
            aT, aT_b = actT[t % 2]
            for fc in range(8):
                pa, pa_b = PS_G[(fc % 2) * 2]
                pl, pl_b = PS_G[(fc % 2) * 2 + 1]
                for (pv, pb, coff) in ((pa, pa_b, 0), (pl, pl_b, 1024)):
                    for kc in range(8):
                        kw = dict(writes=[pb]) if kc == 0 else dict(accum=[pb])
                        P.add("pe", lambda e, pv=pv, kc=kc, fc=fc, coff=coff, wg=wg, xT=xT: e.matmul(
                            pv, lhsT=wg[:, kc, coff + fc * 128:coff + (fc + 1) * 128], rhs=xT[:, kc, :], start=(kc == 0), stop=(kc == 7)),
                            reads=[wg_b, xT_b], **kw)
                (xg, xg_b), (sg, sg_b), (xl, xl_b), (tt_, tt_b) = sw2[fc % 2]
                P.add("dve", lambda e, pa=pa, fc=fc, bg=bg, xg=xg: e.tensor_scalar(out=xg, in0=pa, scalar1=bg[:, fc:fc + 1], scalar2=7.0, op0=ALU.add, op1=ALU.min),
                      reads=[pa_b, bg_b], writes=[xg_b])
                P.add("act", lambda e, xg=xg, sg=sg: e.activation(out=sg, in_=xg, func=AF.Sigmoid, scale=1.702), reads=[xg_b], writes=[sg_b])
                P.add("act", lambda e, pl=pl, fc=fc, bg=bg, xl=xl: e.activation(out=xl, in_=pl, func=AF.Identity, bias=bg[:, 8 + fc:9 + fc]),
                      reads=[pl_b, bg_b], writes=[xl_b])
                P.add("dve", lambda e, xl=xl: e.tensor_scalar(out=xl, in0=xl, scalar1=7.0, scalar2=-7.0, op0=ALU.min, op1=ALU.max), reads=[xl_b], writes=[xl_b])
                P.add("dve", lambda e, xl=xl, xg=xg, tt_=tt_: e.scalar_tensor_tensor(out=tt_, in0=xl, scalar=1.0, in1=xg, op0=ALU.add, op1=ALU.mult),
                      reads=[xl_b, xg_b], writes=[tt_b])
                P.add("pool", lambda e, tt_=tt_, sg=sg, aT=aT, fc=fc: e.tensor_tensor(out=aT[:, fc, :], in0=tt_, in1=sg, op=ALU.mult),
                      reads=[tt_b, sg_b], **(dict(writes=[aT_b]) if fc == 0 else dict(accum=[aT_b])))

        def down(t):
            wdd, wdd_b = wd_sb[t % 2]
            aT, aT_b = actT[t % 2]
            for t4 in range(4):
                ys, ys_b = yst[cnt["y"] % 4]; cnt["y"] += 1
                for dh in range(2):
                    pv, pb = PS_D[cnt["d"] % 2]; cnt["d"] += 1
                    for fc in range(8):
                        kw = dict(writes=[pb]) if fc == 0 else dict(accum=[pb])
                        P.add("pe", lambda e, pv=pv, fc=fc, t4=t4, dh=dh, aT=aT, wdd=wdd: e.matmul(
                            pv, lhsT=aT[:, fc, t4 * 128:(t4 + 1) * 128], rhs=wdd[:, fc, dh * 512:(dh + 1) * 512], start=(fc == 0), stop=(fc == 7)),
                            reads=[aT_b, wdd_b], **kw)
                    kw = dict(writes=[ys_b]) if dh == 0 else dict(accum=[ys_b])
                    P.add("act", lambda e, pv=pv, ys=ys, dh=dh: e.activation(out=ys[:, dh * 512:(dh + 1) * 512], in_=pv, func=AF.Copy), reads=[pb], **kw)
                r0 = t * TS + t4 * 128
                P.add("sp", lambda e, ys=ys, r0=r0: e.dma_start(out=y_scr[r0:r0 + 128, :], in_=ys), reads=[ys_b], accum=[dram_y], dma=f"yst{(cnt['y'] - 1) % 4}")

        load_tile(0)
        transposes(0)
        for t in range(NMT):
            if t + 1 < NMT:
                load_tile(t + 1)
            gate_up(t)
            if t + 1 < NMT:
                transposes(t + 1)
            down(t)

        P.barrier()
        AR.reset(0)
        wpg_sb, wpg_b = AR.alloc([8, D], BF16, "wpg")
        P.add("pool", lambda e: e.dma_start(out=wpg_sb, in_=wpg_d.rearrange("(c p) n -> p c n", p=128)), writes=[wpg_b], dma="wpg")
        wpp_sb, wpp_b = AR.alloc([2, D], BF16, "wpp")
        P.add("pool", lambda e: e.dma_start(out=wpp_sb, in_=wpp_d.rearrange("(c p) n -> p c n", p=128)), writes=[wpp_b], dma="wpp")
        gple_b, gple_bb = AR.alloc([D], F32, "gple_b")
        P.add("sp", lambda e: e.dma_start(out=gple_b, in_=gvec_d[2:3, :].partition_broadcast(128)), writes=[gple_bb], dma="gpleb")
        bd_sb, bd_b = AR.alloc([D], F32, "bd_sb")
        P.add("sp", lambda e: e.dma_start(out=bd_sb[0:NE, :], in_=bd_d), writes=[bd_b], dma="bd")
        combT_t = [AR.alloc([128], F32, f"combT{i}") for i in range(3)]
        yg_t = [[AR.alloc([D], F32, f"yg{k}_{j}") for j in range(4)] for k in range(3)]
        h2_t = [AR.alloc([D], F32, f"h2t{i}") for i in range(3)]
        u3_t = [AR.alloc([D], BF16, f"u3{i}") for i in range(3)]
        u3T_t = [AR.alloc([8, 128], BF16, f"u3T{i}") for i in range(3)]
        gate_t = [AR.alloc([D], F32, f"gate{i}") for i in range(3)]
        pin_t = [AR.alloc([256], F32, f"pin{i}") for i in range(3)]
        pbf_t = [AR.alloc([256], BF16, f"pbf{i}") for i in range(3)]
        pT_t = [AR.alloc([2, 128], BF16, f"pT{i}") for i in range(3)]
        o_t = [AR.alloc([D], F32, f"o{i}") for i in range(3)]
        junk2, junk2_b = AR.alloc([D], BF16, "junk2")
        st2_t = [AR.alloc([4], F32, f"st2{i}") for i in range(3)]
        PS_TR2 = [psum[0], psum[1]]
        PS_GT = [psum[2], psum[3]]
        PS_BD = [psum[4], psum[5]]
        PS_PP = [psum[6], psum[7]]
        def c_stage1(ti):
            k = ti % 3
            h2, h2_b = h2_t[k]
            u3, u3_b = u3_t[k]
            u3T, u3T_b = u3T_t[k]
            gt, gt_b = gate_t[k]
            pin, pin_b = pin_t[k]
            pbf, pbf_b = pbf_t[k]
            pT, pT_b = pT_t[k]
            ot, ot_b = o_t[k]
            st, st_b = st2_t[k]
            cT, cT_b = combT_t[k]
            r0 = ti * 128
            P.add("sp", lambda e, h2=h2, r0=r0: e.dma_start(out=h2, in_=h1_scr[r0:r0 + 128, :]), reads=[dram_h1], writes=[h2_b], dma=f"h2ld{k}")
            P.add("sp", lambda e, pin=pin, r0=r0: e.dma_start(out=pin, in_=p_d[r0:r0 + 128, :]), writes=[pin_b], dma=f"pld{k}")
            for j in range(4):
                yg, yg_b = yg_t[k][j]
                P.add("pool", lambda e, yg=yg, j=j, ti=ti: e.indirect_dma_start(out=yg, out_offset=None, in_=y_scr[:, :],
                                                                        in_offset=bass.IndirectOffsetOnAxis(ap=sel_i[:, j, ti:ti + 1], axis=0)),
                      reads=[sel_i_b, dram_y], writes=[yg_b], dma=f"yg{k}_{j}")
            pv, pb = PS_BD[0]
            P.add("pe", lambda e, pv=pv, ti=ti: e.matmul(pv[0:NE, 0:128], lhsT=comb[:, ti, :], rhs=ident_f, start=True, stop=True),
                  reads=[comb_b, ident_f_b], writes=[pb])
            P.add("act", lambda e, pv=pv, cT=cT: e.activation(out=cT[0:NE, :], in_=pv[0:NE, 0:128], func=AF.Copy), reads=[pb], writes=[cT_b])
            for dh in range(2):
                pv2, pb2 = PS_BD[1] if dh == 0 else PS_BD[0]
                P.add("pe", lambda e, pv2=pv2, dh=dh, cT=cT: e.matmul(pv2, lhsT=cT[0:NE, :], rhs=bd_sb[0:NE, dh * 512:(dh + 1) * 512], start=True, stop=True),
                      reads=[cT_b, bd_b], writes=[pb2])
                P.add("dve", lambda e, pv2=pv2, dh=dh, h2=h2: e.tensor_tensor(out=h2[:, dh * 512:(dh + 1) * 512], in0=pv2, in1=h2[:, dh * 512:(dh + 1) * 512], op=ALU.add),
                      reads=[pb2, h2_b], writes=[h2_b])
            for j in range(4):
                yg, yg_b = yg_t[k][j]
                P.add("dve", lambda e, yg=yg, j=j, ti=ti, h2=h2: e.scalar_tensor_tensor(out=h2, in0=yg, scalar=g4[:, ti, j:j + 1], in1=h2, op0=ALU.mult, op1=ALU.add),
                      reads=[yg_b, g4_b, h2_b], writes=[h2_b])

        def c_stage1b(ti):
            k = ti % 3
            h2, h2_b = h2_t[k]
            u3, u3_b = u3_t[k]
            u3T, u3T_b = u3T_t[k]
            gt, gt_b = gate_t[k]
            pin, pin_b = pin_t[k]
            pbf, pbf_b = pbf_t[k]
            pT, pT_b = pT_t[k]
            ot, ot_b = o_t[k]
            st, st_b = st2_t[k]
            cT, cT_b = combT_t[k]
            r0 = ti * 128
            P.add("act", lambda e, h2=h2, st=st: e.activation(out=junk2, in_=h2, func=AF.Square, accum_out=st[:, 0:1]), reads=[h2_b], writes=[junk2_b, st_b])
            P.add("act", lambda e, st=st: e.activation(out=st[:, 1:2], in_=st[:, 0:1], func=AF.Ln, bias=ccol[:, 2:3], scale=1.0 / D), reads=[ccol_b], writes=[st_b])
            P.add("act", lambda e, st=st: e.activation(out=st[:, 2:3], in_=st[:, 1:2], func=AF.Exp, scale=-0.5), writes=[st_b])
            P.add("dve", lambda e, u3=u3, h2=h2, st=st: e.scalar_tensor_tensor(out=u3, in0=h2, scalar=st[:, 2:3], in1=gple_b, op0=ALU.mult, op1=ALU.mult),
                  reads=[h2_b, st_b, gple_bb], writes=[u3_b])
            for half in range(2):
                pv, pb = PS_TR2[half]
                for c in range(4):
                    cc = half * 4 + c
                    kw = dict(writes=[pb]) if c == 0 else dict(accum=[pb])
                    P.add("pe", lambda e, pv=pv, c=c, cc=cc, u3=u3: e.matmul(pv[:, c * 128:(c + 1) * 128], lhsT=u3[:, cc * 128:(cc + 1) * 128], rhs=ident_b, start=True, stop=True),
                          reads=[u3_b, ident_b_b], **kw)
                P.add("act", lambda e, pv=pv, half=half, u3T=u3T: e.activation(out=u3T[:, half * 4:(half + 1) * 4, :], in_=pv.rearrange("p (c t) -> p c t", c=4), func=AF.Copy),
                      reads=[pb], **(dict(writes=[u3T_b]) if half == 0 else dict(accum=[u3T_b])))
            P.add("pool", lambda e, pin=pin, pbf=pbf: e.tensor_copy(out=pbf, in_=pin), reads=[pin_b], writes=[pbf_b])
            pvp, pbp = PS_PP[0]
            for c in range(2):
                kw = dict(writes=[pbp]) if c == 0 else dict(accum=[pbp])
                P.add("pe", lambda e, c=c, pbf=pbf: e.matmul(pvp[:, c * 128:(c + 1) * 128], lhsT=pbf[:, c * 128:(c + 1) * 128], rhs=ident_b, start=True, stop=True),
                      reads=[pbf_b, ident_b_b], **kw)
            P.add("act", lambda e, pT=pT: e.activation(out=pT, in_=pvp[:, 0:256].rearrange("p (c t) -> p c t", c=2), func=AF.Copy), reads=[pbp], writes=[pT_b])

        def c_stage2(ti):
            k = ti % 3
            h2, h2_b = h2_t[k]
            u3, u3_b = u3_t[k]
            u3T, u3T_b = u3T_t[k]
            gt, gt_b = gate_t[k]
            pin, pin_b = pin_t[k]
            pbf, pbf_b = pbf_t[k]
            pT, pT_b = pT_t[k]
            ot, ot_b = o_t[k]
            st, st_b = st2_t[k]
            cT, cT_b = combT_t[k]
            r0 = ti * 128
            for dh in range(2):
                pv, pb = PS_GT[dh]
                for kc in range(8):
                    kw = dict(writes=[pb]) if kc == 0 else dict(accum=[pb])
                    P.add("pe", lambda e, pv=pv, kc=kc, dh=dh, u3T=u3T: e.matmul(pv, lhsT=u3T[:, kc, :], rhs=wpg_sb[:, kc, dh * 512:(dh + 1) * 512], start=(kc == 0), stop=(kc == 7)),
                          reads=[u3T_b, wpg_b], **kw)
                P.add("act", lambda e, pv=pv, dh=dh, gt=gt: e.activation(out=gt[:, dh * 512:(dh + 1) * 512], in_=pv, func=AF.Sigmoid),
                      reads=[pb], **(dict(writes=[gt_b]) if dh == 0 else dict(accum=[gt_b])))
            for dh in range(2):
                pv, pb = PS_PP[1] if dh == 0 else PS_PP[0]
                for c in range(2):
                    kw = dict(writes=[pb]) if c == 0 else dict(accum=[pb])
                    P.add("pe", lambda e, pv=pv, c=c, dh=dh, pT=pT: e.matmul(pv, lhsT=pT[:, c, :], rhs=wpp_sb[:, c, dh * 512:(dh + 1) * 512], start=(c == 0), stop=(c == 1)),
                          reads=[pT_b, wpp_b], **kw)
                P.add("dve", lambda e, pv=pv, dh=dh, gt=gt, ot=ot: e.tensor_tensor(out=ot[:, dh * 512:(dh + 1) * 512], in0=pv, in1=gt[:, dh * 512:(dh + 1) * 512], op=ALU.mult),
                      reads=[pb, gt_b], **(dict(writes=[ot_b]) if dh == 0 else dict(accum=[ot_b])))
            P.add("pool", lambda e, ot=ot, h2=h2: e.tensor_tensor(out=ot, in0=ot, in1=h2, op=ALU.add), reads=[ot_b, h2_b], writes=[ot_b])
            P.add("sp", lambda e, ot=ot, r0=r0: e.dma_start(out=out_d[r0:r0 + 128, :], in_=ot), reads=[ot_b], accum=[dram_out], dma=f"outst{k}")

        for it in range(NT + 2):
            if 0 <= it - 2 < NT:
                c_stage2(it - 2)
            if 0 <= it - 1 < NT:
                c_stage1b(it - 1)
            if it < NT:
                c_stage1(it)
        P.barrier()

        with nc.Block() as block:
            P.emit(block)
    return nc


def _consts():
    c = {}
    c["ident"] = np.eye(128, dtype=np.float32)
    kk = np.arange(128)[:, None]
    mm = np.arange(128)[None, :]
    c["ustrict"] = (kk < mm).astype(np.float32)
    c["blk64"] = ((kk // 64) == (mm // 64)).astype(np.float32)
    xs = np.arange(XW)[None, :]
    delta = xs - 384 - kk
    m = ((delta >= 0) & (delta <= 128)).astype(np.float64)
    m += ((delta >= 0) & (delta % 4 == 0) & (delta <= 512))
    m += ((delta >= 0) & (delta % 16 == 0) & (delta <= 2048))
    lm = np.where(m > 0, np.log(np.maximum(m, 1.0)), NEG)
    slopes = 2.0 ** (-(np.arange(8) + 1.0))
    c["btabA"] = (-(slopes[:, None, None]) * delta[None].astype(np.float64) + lm[None]).astype(np.float32)
    xs2 = np.arange(CBW)[None, :]
    d2 = xs2 - 384 - kk
    c["cbtab"] = np.where(d2 >= 0, 0.0, NEG).astype(np.float32)
    c["kpos"] = (np.arange(16)[None, :] * 128 + kk).astype(np.float32)
    sel8 = np.zeros((8, 8, 128), np.float32)
    for h in range(8):
        sel8[h, h, :] = 1.0
    c["sel8"] = sel8.reshape(8, 8 * 128)
    c["tokid"] = (np.arange(NT)[None, :] * 128 + kk).astype(np.int32)
    misc = np.zeros((128, 128), np.float32)
    misc[:, 0] = np.arange(128)
    misc[:, 1:65] = np.arange(64)[None, :]
    misc[:, 65:73] = (np.arange(8) * TS)[None, :]
    c["misc"] = misc
    return c


def _prep_inputs(x, p, g_mix, w_in, b_f, g_qa, g_ka, g_qb, g_kb, w_o, g_ffn, w_router, b_router,
                 w_gate_up, b_gate_up, w_down, b_down, g_ple, w_ple_gate, w_ple_proj):
    f = lambda a: np.ascontiguousarray(np.asarray(a, dtype=np.float32))
    x = f(x); p = f(p)
    shared = {}
    shared["w_in"] = f(w_in[0])
    shared["w_o"] = f(w_o[0])
    shared["w_router"] = f(w_router[0])
    wgu = np.asarray(w_gate_up[0], dtype=np.float32)
    wgu = np.concatenate([wgu[:, :, 0::2], wgu[:, :, 1::2]], axis=2)
    wgu = wgu.reshape(NE, 8, 128, 2048).transpose(0, 2, 1, 3)
    shared["wgu"] = np.ascontiguousarray(wgu).reshape(NE * 128 * 4, 4096)
    wd = np.asarray(w_down[0], dtype=np.float32).reshape(NE, 8, 128, D).transpose(0, 2, 1, 3)
    shared["wd"] = np.ascontiguousarray(wd).reshape(NE * 128 * 2, 4096)
    bgu = np.asarray(b_gate_up[0], dtype=np.float32)
    bgu = np.concatenate([bgu[:, 0::2], bgu[:, 1::2]], axis=1)
    shared["bgu"] = np.ascontiguousarray(bgu.reshape(NE, 16, 128).transpose(0, 2, 1)).reshape(NE * 128, 16)
    shared["b_down"] = f(b_down[0])
    shared["w_ple_gate"] = f(w_ple_gate[0])
    shared["w_ple_proj"] = f(w_ple_proj[0])
    shared["gvec"] = np.ascontiguousarray(np.stack([f(g_mix[0]), f(g_ffn[0]), f(g_ple[0])], axis=0))
    shared["gvecT"] = np.ascontiguousarray(shared["gvec"].reshape(3, 8, 128).transpose(2, 0, 1)).reshape(128, 24)
    gq = np.stack([np.tile(f(g_qa[0]), 2), np.tile(f(g_ka[0]), 2), np.tile(f(g_qb[0]), 2), np.tile(f(g_kb[0]), 2)], axis=1)
    shared["gqk"] = np.ascontiguousarray(gq)
    shared["bf"] = f(b_f[0]).reshape(8, 1)
    shared["b_router"] = f(b_router[0]).reshape(1, NE)
    shared.update(_consts())
    if STAGE not in ('', 'full'):
        shared["wgu"] = shared["wgu"][:128 * 4]
        shared["wd"] = shared["wd"][:128 * 2]
    in_maps = []
    for c in range(NCORES):
        m = dict(shared)
        m["x"] = x[c * NSEQ:(c + 1) * NSEQ].reshape(NTOK, D)
        m["p"] = p[0, c * NSEQ:(c + 1) * NSEQ].reshape(NTOK, 256)
        in_maps.append(m)
    return in_maps


_NC_CACHE = {}


def kernel(**inputs):
    in_maps = _prep_inputs(**inputs)
    if "nc" not in _NC_CACHE:
        _NC_CACHE["nc"] = build_program(DEBUG)
    nc = _NC_CACHE["nc"]
    res = run_bass_kernel_spmd(nc, in_maps, core_ids=list(range(NCORES)))
    outs = [np.asarray(r["out"]).reshape(NSEQ, SEQ, D) for r in res.results]
    if DEBUG:
        kernel.last_results = res.results
    return np.concatenate(outs, axis=0).astype(np.float32)
```

```python
import math
from contextlib import ExitStack

import numpy as np
import ml_dtypes

import concourse.bass as bass
import concourse.mybir as mybir
from concourse.bass_utils import run_bass_kernel_spmd

F32 = mybir.dt.float32
BF16 = mybir.dt.bfloat16
I32 = mybir.dt.int32
ALU = mybir.AluOpType
AF = mybir.ActivationFunctionType
AX = mybir.AxisListType

NCORES = 8
D = 1024
SEQ = 2048
NSEQ = 2
NTOK = NSEQ * SEQ
NT = NTOK // 128
NE = 32
TOPK = 4
TS = 512
NMT = NTOK * TOPK // TS + NE
NSLOT = NMT * TS
EPS = 1e-6
NEG = -30000.0
XW = 2432
CBW = 896

DEBUG = False
import os
STAGE = os.environ.get('KSTAGE', '')


class StopBuild(Exception):
    pass


def checkpoint(name):
    if STAGE == name:
        raise StopBuild()


class Buf:
    __slots__ = ("name", "writers", "readers")

    def __init__(self, name):
        self.name = name
        self.writers = []
        self.readers = []


class Op:
    __slots__ = ("eng", "fn", "deps", "is_dma", "sem", "val", "signal", "extra_waits")

    def __init__(self, eng, fn):
        self.eng = eng
        self.fn = fn
        self.deps = []
        self.is_dma = False
        self.sem = None
        self.val = 0
        self.signal = False


class Prog:
    ENG = ["pe", "act", "dve", "pool", "sp"]
    SAME_ENGINE_SYNC = {"act", "dve", "pool"}

    def __init__(self, nc, stack):
        self.nc = nc
        self.stack = stack
        self.ops = {e: [] for e in self.ENG}
        self.esem = {e: stack.enter_context(nc.semaphore("sem_" + e)) for e in self.ENG}
        self.dma_sems = {}
        self.last = {e: None for e in self.ENG}

    def dma_sem(self, name):
        if name not in self.dma_sems:
            self.dma_sems[name] = [self.stack.enter_context(self.nc.semaphore("dq_" + name)), 0, None]
        return self.dma_sems[name]

    def add(self, eng, fn, reads=(), writes=(), accum=(), dma=None):
        op = Op(eng, fn)
        deps = []
        for b in reads:
            deps.extend(b.writers)
        for b in writes:
            deps.extend(b.writers)
            deps.extend(b.readers)
        for b in accum:
            deps.extend(b.readers)
        seen = set()
        for d in deps:
            if id(d) not in seen and d is not op:
                seen.add(id(d))
                op.deps.append(d)
        if dma is not None:
            rec = self.dma_sem(dma)
            rec[1] += 16
            rec[2] = op
            op.is_dma = True
            op.sem = rec[0]
            op.val = rec[1]
        for b in reads:
            b.readers.append(op)
        for b in writes:
            b.writers = [op]
            b.readers = []
        for b in accum:
            if b.writers and all((w.eng == eng and not w.is_dma and not op.is_dma) for w in b.writers):
                b.writers = [op]
            else:
                b.writers.append(op)
            b.readers = []
        self.ops[eng].append(op)
        if not op.is_dma:
            self.last[eng] = op
        return op

    def barrier(self):
        deps = [o for o in self.last.values() if o is not None]
        deps += [rec[2] for rec in self.dma_sems.values() if rec[2] is not None]
        for e in self.ENG:
            op = Op(e, None)
            op.deps = [d for d in deps]
            self.ops[e].append(op)

    def emit(self, block):
        for e in self.ENG:
            for op in self.ops[e]:
                for d in op.deps:
                    if d.is_dma:
                        continue
                    if d.eng == op.eng and not op.is_dma and op.fn is not None and d.eng not in self.SAME_ENGINE_SYNC:
                        continue
                    d.signal = True
        for e in self.ENG:
            cnt = 0
            for op in self.ops[e]:
                if not op.is_dma and op.signal:
                    cnt += 1
                    op.val = cnt
                    op.sem = self.esem[e]
        self.counts = {}

        def run(e, engobj):
            seen = {}
            n = 0
            for op in self.ops[e]:
                need = {}
                for d in op.deps:
                    if not d.is_dma and not d.signal:
                        continue
                    key = id(d.sem)
                    if d.val > need.get(key, (0, None))[0]:
                        need[key] = (d.val, d.sem)
                for key, (val, sem) in need.items():
                    if seen.get(key, 0) >= val:
                        continue
                    seen[key] = val
                    engobj.wait_ge(sem, val)
                    n += 1
                if op.fn is None:
                    continue
                ins = op.fn(engobj)
                n += 1
                if op.is_dma:
                    ins.then_inc(op.sem, 16)
                elif op.signal:
                    ins.then_inc(op.sem, 1)
            self.counts[e] = n

        @block.tensor
        def _(eng):
            run("pe", eng)

        @block.scalar
        def _(eng):
            run("act", eng)

        @block.vector
        def _(eng):
            run("dve", eng)

        @block.gpsimd
        def _(eng):
            run("pool", eng)

        @block.sync
        def _(eng):
            run("sp", eng)


class Arena:
    def __init__(self, ap, nwords):
        self.ap = ap
        self.n = nwords
        self.off = 0
        self.cnt = 0

    def mark(self):
        return self.off

    def reset(self, m=0):
        self.off = m

    def alloc(self, free_shape, dtype, name=None):
        esz = 4 if dtype in (F32, I32) else 2
        nel = int(np.prod(free_shape))
        n32 = (nel * esz + 3) // 4
        assert self.off + n32 <= self.n, f"arena overflow {name} {self.off}+{n32}>{self.n}"
        v = self.ap[:, self.off:self.off + n32]
        self.off += n32
        if dtype != F32:
            v = v.bitcast(dtype)
        if len(free_shape) == 2:
            v = v.rearrange("p (a b) -> p a b", a=free_shape[0])
        elif len(free_shape) == 3:
            v = v.rearrange("p (a b c) -> p a b c", a=free_shape[0], b=free_shape[1])
        self.cnt += 1
        return v, Buf(name or f"buf{self.cnt}")


def build_program(debug=False):
    nc = bass.Bass("TRN2", target_bir_lowering=False)

    def din(name, shape, dt=F32):
        return nc.dram_tensor(name, list(shape), dt, kind="ExternalInput").ap()

    x_d = din("x", [NTOK, D])
    p_d = din("p", [NTOK, 256])
    w_in_d = din("w_in", [D, 3080])
    w_o_d = din("w_o", [D, D])
    w_r_d = din("w_router", [D, NE])
    NEW = NE if STAGE in ('', 'full') else 1
    wgu_d = din("wgu", [NEW * 128 * 4, 4096])
    wd_d = din("wd", [NEW * 128 * 2, 4096])
    bgu_d = din("bgu", [NE * 128, 16])
    bd_d = din("b_down", [NE, D])
    wpg_d = din("w_ple_gate", [D, D])
    wpp_d = din("w_ple_proj", [256, D])
    gvec_d = din("gvec", [3, D])
    gvecT_d = din("gvecT", [128, 24])
    gqk_d = din("gqk", [128, 4])
    nbf_d = din("bf", [8, 1])
    br_d = din("b_router", [1, NE])
    ident_d = din("ident", [128, 128])
    ustrict_d = din("ustrict", [128, 128])
    blk64_d = din("blk64", [128, 128])
    btabA_d = din("btabA", [8, 128, XW])
    cbtab_d = din("cbtab", [128, CBW])
    kpos_d = din("kpos", [128, 16])
    sel8_d = din("sel8", [8, 8 * 128])
    tokid_d = din("tokid", [128, NT], I32)
    misc_d = din("misc", [128, 128])
    out_d = nc.dram_tensor("out", [NTOK, D], F32, kind="ExternalOutput").ap()

    h1_scr = nc.dram_tensor("h1_scr", [NTOK, D], F32, kind="Internal").ap()
    u2_scr = nc.dram_tensor("u2_scr", [NTOK, D], BF16, kind="Internal").ap()
    xs_scr = nc.dram_tensor("xs_scr", [NSLOT, D], BF16, kind="Internal").ap()
    y_scr = nc.dram_tensor("y_scr", [NSLOT, D], F32, kind="Internal").ap()
    dbg = {}
    if debug:
        dbg["mix"] = nc.dram_tensor("dbg_mix", [NSEQ, 128, 8 * SEQ], BF16, kind="ExternalOutput").ap()
        dbg["logits"] = nc.dram_tensor("dbg_logits", [128, NT * NE], F32, kind="ExternalOutput").ap()
        dbg["route"] = nc.dram_tensor("dbg_route", [128, 1024], F32, kind="ExternalOutput").ap()
        dbg["h1"] = h1_scr
    stack = ExitStack()
    with stack:
        print("sbuf bytes remaining", nc.sbuf_bytes_remaining)
        ARW = 47 * 1024
        CAW = 4608
        arena_t = stack.enter_context(nc.sbuf_tensor("arena", [128, ARW], F32))
        const_t = stack.enter_context(nc.sbuf_tensor("consts", [128, CAW], F32))
        AR = Arena(arena_t[:, :], ARW)
        CA = Arena(const_t[:, :], CAW)
        psum = []
        for i in range(8):
            t = stack.enter_context(nc.psum_tensor(f"ps{i}", [128, 512], F32))
            psum.append((t[:, :], Buf(f"ps{i}")))
        P = Prog(nc, stack)
        dram_u2 = Buf("u2_scr")
        dram_h1 = Buf("h1_scr")
        dram_out = Buf("out")

        def load_const(src_ap, free_shape, dtype, name, eng="sp"):
            v, b = CA.alloc(free_shape, dtype, name)
            P.add(eng, lambda e, v=v, s=src_ap: e.dma_start(out=v, in_=s), writes=[b], dma="c_" + name)
            return v, b

        ident_f, ident_f_b = load_const(ident_d, [128], F32, "ident_f")
        ident_b, ident_b_b = load_const(ident_d, [128], BF16, "ident_b", eng="pool")
        ustrict, ustrict_b = load_const(ustrict_d, [128], BF16, "ustrict", eng="pool")
        blk64, blk64_b = load_const(blk64_d, [128], BF16, "blk64", eng="pool")
        ones_b, ones_b_b = CA.alloc([128], BF16, "ones_b")
        P.add("dve", lambda e: e.memset(ones_b, 1.0), writes=[ones_b_b])
        ccol, ccol_b = CA.alloc([4], F32, "ccol")
        P.add("dve", lambda e: e.memset(ccol[:, 0:1], 64.0 * EPS), writes=[ccol_b])
        P.add("dve", lambda e: e.memset(ccol[:, 1:2], 1.0), writes=[ccol_b])
        P.add("dve", lambda e: e.memset(ccol[:, 2:3], EPS), writes=[ccol_b])
        gT, gT_b = load_const(gvecT_d, [3, 8], F32, "gT")
        gqk, gqk_b = load_const(gqk_d, [4], F32, "gqk")
        P.add("dve", lambda e: e.tensor_scalar(out=gqk[:, 1:2], in0=gqk[:, 1:2], scalar1=8.0, scalar2=None, op0=ALU.mult), writes=[gqk_b])
        P.add("dve", lambda e: e.tensor_scalar(out=gqk[:, 3:4], in0=gqk[:, 3:4], scalar1=8.0, scalar2=None, op0=ALU.mult), writes=[gqk_b])
        nbf, nbf_b = CA.alloc([1], F32, "nbf")
        P.add("sp", lambda e: e.dma_start(out=nbf[0:8, :], in_=nbf_d), writes=[nbf_b], dma="c_nbf")
        P.add("dve", lambda e: e.tensor_scalar(out=nbf[0:8, :], in0=nbf[0:8, :], scalar1=-1.0, scalar2=None, op0=ALU.mult), writes=[nbf_b])
        kpos, kpos_b = load_const(kpos_d, [16], F32, "kpos")
        misc, misc_b = load_const(misc_d, [128], F32, "misc")
        tokid, tokid_b = load_const(tokid_d, [NT], I32, "tokid")


        zt, zt_b = CA.alloc([2048], BF16, "zt")
        P.add("pool", lambda e: e.memset(zt, 0.0), writes=[zt_b])
        dram_xs0 = Buf("xs_zero")
        xs_flat = xs_scr.rearrange("(p a) d -> p (a d)", p=128)
        logits_all, logits_b = CA.alloc([NT, NE], F32, "logits_all")

        uT, uT_b = AR.alloc([8, SEQ], BF16, "uT")
        mixT, mixT_b = AR.alloc([8, SEQ], BF16, "mixT")
        wr_sb, wr_b = AR.alloc([8, NE], F32, "w_r")
        P.add("sp", lambda e: e.dma_start(out=wr_sb, in_=w_r_d.rearrange("(c p) n -> p c n", p=128)), writes=[wr_b], dma="wr")
        brb, brb_b = AR.alloc([NE], F32, "brb")
        P.add("sp", lambda e: e.dma_start(out=brb, in_=br_d.partition_broadcast(128)), writes=[brb_b], dma="brb")
        cbtab, cbtab_b = AR.alloc([CBW], F32, "cbtab")
        P.add("sp", lambda e: e.dma_start(out=cbtab, in_=cbtab_d), writes=[cbtab_b], dma="cbtab")
        sel8, sel8_b = AR.alloc([8 * 128], F32, "sel8")
        P.add("sp", lambda e: e.dma_start(out=sel8[0:8, :], in_=sel8_d), writes=[sel8_b], dma="sel8")
        onespad, onespad_b = AR.alloc([2, 128], BF16, "onespad")
        P.add("pool", lambda e: e.memset(onespad, 0.0), writes=[onespad_b])
        P.add("pool", lambda e: e.memset(onespad[:, 0, 0:64], 1.0), writes=[onespad_b])
        P.add("pool", lambda e: e.memset(onespad[:, 1, 64:128], 1.0), writes=[onespad_b])
        wf_sb, wf_b = AR.alloc([8, 8], BF16, "wf")
        P.add("pool", lambda e: e.dma_start(out=wf_sb, in_=w_in_d.rearrange("(c p) n -> p c n", p=128)[:, :, 3072:3080]), writes=[wf_b], dma="wf")
        csum, csum_b = AR.alloc([SEQ], F32, "csum")
        ones8, ones8_b = AR.alloc([512], F32, "ones8")
        P.add("pool", lambda e: e.memset(ones8, 1.0), writes=[ones8_b])
        nck, nck_b = AR.alloc([16, 8], F32, "nck")
        NR = 5
        sq_t = [AR.alloc([512], BF16, f"sq{i}") for i in range(2)]
        rs_t = [AR.alloc([512], F32, f"rs{i}") for i in range(2)]
        tmp_t = [AR.alloc([512], F32, f"tmp{i}") for i in range(NR)]
        pt_t = [AR.alloc([512], BF16, f"pt{i}") for i in range(NR)]
        rden_t = [AR.alloc([512], F32, f"rden{i}") for i in range(1)]
        st_t = [AR.alloc([4], F32, f"st{i}") for i in range(4)]
        fl_t, fl_b = AR.alloc([2, 512], F32, "fl")
        x_mark = AR.mark()
        xt_t = [AR.alloc([D], F32, f"xt{i}") for i in range(2)]
        xn_t = [AR.alloc([D], BF16, f"xn{i}") for i in range(2)]
        junk, junk_b = AR.alloc([D], BF16, "junk")
        wo_sb, wo_b = AR.alloc([8, D], BF16, "w_o")
        gffn_b, gffn_bb = AR.alloc([D], F32, "gffn_b")
        h1_t = [AR.alloc([D], F32, f"h1t{i}") for i in range(2)]
        u2f_t = [AR.alloc([D], F32, f"u2f{i}") for i in range(2)]
        u2b_t = [AR.alloc([D], BF16, f"u2b{i}") for i in range(2)]
        u2T_t = [AR.alloc([8, 128], F32, f"u2T{i}") for i in range(2)]
        x_end1 = AR.mark()
        AR.reset(x_mark)
        btab = [AR.alloc([XW], F32, f"btab{i}") for i in range(2)]
        NPB = 2
        wq_sb = [AR.alloc([3, 8, 128], BF16, f"wqkv{i}") for i in range(NPB)]
        qT = [AR.alloc([SEQ], BF16, f"qT{i}") for i in range(NPB)]
        kT = [AR.alloc([SEQ], BF16, f"kT{i}") for i in range(NPB)]
        vpad = [AR.alloc([2, 16, 128], BF16, f"vpad{i}") for i in range(NPB)]
        AR.reset(max(x_end1, AR.mark()))

        PS_TR = [psum[0], psum[1]]
        PS_PJ = [psum[2], psum[3]]
        PS_S = [psum[4], psum[5]]
        PS_S4 = [psum[4], psum[5], psum[0], psum[1]]
        tmp4 = tmp_t + [(fl_t[:, 0, :], fl_b)]
        pt4 = pt_t + [(fl_t[:, 1, 0:256].bitcast(BF16), fl_b)]
        NB4 = len(tmp4)
        PS_NUM = psum[6]
        PS_DEN = psum[7]
        ctr = {"pj": 0, "s": 0, "tmp": 0, "sq": 0, "st": 0, "x": 0, "h1": 0}

        def rot(key, lst):
            i = ctr[key]
            ctr[key] = i + 1
            return lst[i % len(lst)]

        def rms_stats(src, src_b, st, st_b):
            P.add("act", lambda e: e.activation(out=junk, in_=src, func=AF.Square, accum_out=st[:, 0:1]),
                  reads=[src_b], writes=[junk_b, st_b])
            P.add("act", lambda e: e.activation(out=st[:, 1:2], in_=st[:, 0:1], func=AF.Ln, bias=ccol[:, 2:3], scale=1.0 / D),
                  reads=[ccol_b], writes=[st_b])
            P.add("act", lambda e: e.activation(out=st[:, 2:3], in_=st[:, 1:2], func=AF.Exp, scale=-0.5),
                  reads=[], writes=[st_b])

        def transpose8(src, src_b, ident, ident_buf, nchunks=8):
            for half in range((nchunks + 3) // 4):
                pv, pb = PS_TR[half]
                for c in range(4):
                    cc = half * 4 + c
                    if cc >= nchunks:
                        break
                    kw = dict(writes=[pb]) if c == 0 else dict(accum=[pb])
                    P.add("pe", lambda e, pv=pv, c=c, cc=cc: e.matmul(pv[:, c * 128:(c + 1) * 128], lhsT=src[:, cc * 128:(cc + 1) * 128], rhs=ident, start=True, stop=True),
                          reads=[src_b, ident_buf], **kw)

        def phase_a(s):
            tok0 = s * SEQ
            for i in range(16):
                xt, xt_b = rot("x", xt_t)
                xn, xn_b = xn_t[(ctr["x"] - 1) % 2]
                st, st_b = rot("st", st_t)
                r0 = tok0 + i * 128
                P.add("sp", lambda e, xt=xt, r0=r0: e.dma_start(out=xt, in_=x_d[r0:r0 + 128, :]), writes=[xt_b], dma=f"x{(ctr['x'] - 1) % 2}")
                rms_stats(xt, xt_b, st, st_b)
                P.add("act", lambda e, xn=xn, xt=xt, st=st: e.activation(out=xn, in_=xt, func=AF.Copy, scale=st[:, 2:3]),
                      reads=[xt_b, st_b], writes=[xn_b])
                transpose8(xn, xn_b, ident_b, ident_b_b)
                for half in range(2):
                    pv, pb = PS_TR[half]
                    P.add("dve", lambda e, pv=pv, half=half, i=i: e.tensor_tensor(
                        out=uT[:, half * 4:(half + 1) * 4, i * 128:(i + 1) * 128],
                        in0=pv.rearrange("p (c t) -> p c t", c=4),
                        in1=gT[:, 0, half * 4:(half + 1) * 4].unsqueeze(2).to_broadcast([128, 4, 128]), op=ALU.mult),
                        reads=[pb, gT_b], accum=[uT_b])
            checkpoint('A0')
            P.barrier()
            if s == 0:
                for c in range(NSLOT * D // 128 // 2048):
                    P.add("sp", lambda e, c=c: e.dma_start(out=xs_flat[:, c * 2048:(c + 1) * 2048], in_=zt), reads=[zt_b], accum=[dram_xs0], dma="xszero")
            for i in range(NPB):
                P.add("pool", lambda e, i=i: e.memset(vpad[i][0], 1.0), writes=[vpad[i][1]])
            for g in range(4):
                pv, pb = rot("pj", PS_PJ)
                for kc in range(8):
                    kw = dict(writes=[pb]) if kc == 0 else dict(accum=[pb])
                    P.add("pe", lambda e, pv=pv, kc=kc, g=g: e.matmul(pv[0:8, :], lhsT=wf_sb[:, kc, :], rhs=uT[:, kc, g * 512:(g + 1) * 512], start=(kc == 0), stop=(kc == 7)),
                          reads=[wf_b, uT_b], **kw)
                P.add("act", lambda e, pv=pv: e.activation(out=fl_t[0:8, 0, :], in_=pv[0:8, :], func=AF.Exp, bias=nbf[0:8, :], scale=-1.0),
                      reads=[pb, nbf_b], writes=[fl_b])
                P.add("act", lambda e: e.activation(out=fl_t[0:8, 1, :], in_=fl_t[0:8, 0, :], func=AF.Ln, bias=ccol[0:8, 1:2], scale=1.0),
                      reads=[fl_b, ccol_b], writes=[fl_b])
                if g == 0:
                    P.add("dve", lambda e: e.tensor_tensor_scan(out=csum[0:8, 0:512], data0=ones8[0:8, 0:512], data1=fl_t[0:8, 1, :], initial=0.0, op0=ALU.mult, op1=ALU.subtract),
                          reads=[fl_b, ones8_b], writes=[csum_b])
                else:
                    P.add("dve", lambda e, g=g: e.tensor_tensor_scan(out=csum[0:8, g * 512:(g + 1) * 512], data0=ones8[0:8, 0:512], data1=fl_t[0:8, 1, :],
                                                                initial=csum[0:8, g * 512 - 1:g * 512], op0=ALU.mult, op1=ALU.subtract),
                          reads=[fl_b, ones8_b, csum_b], writes=[csum_b])
            pv, pb = rot("pj", PS_PJ)
            for j in range(16):
                kw = dict(writes=[pb]) if j == 0 else dict(accum=[pb])
                P.add("pe", lambda e, pv=pv, j=j: e.matmul(pv[:, j * 8:(j + 1) * 8], lhsT=csum[0:8, j * 128:(j + 1) * 128], rhs=ident_f[0:8, 0:8], start=True, stop=True),
                      reads=[csum_b, ident_f_b], **kw)
            P.add("act", lambda e, pv=pv: e.activation(out=nck.rearrange("p a b -> p (a b)"), in_=pv[:, 0:128], func=AF.Copy, scale=-1.0),
                  reads=[pb], writes=[nck_b])

            checkpoint('fox')
            w_in_v = w_in_d.rearrange("(c p) n -> p c n", p=128)

            def proj_chunks(hp):
                isA_ = hp < 4
                hpl_ = hp % 4
                base_ = 0 if isA_ else 1536
                wq, wq_b = wq_sb[hp % NPB]
                q_sb_, q_b_ = qT[hp % NPB]
                k_sb_, k_b_ = kT[hp % NPB]
                v_sb_, v_b_ = vpad[hp % NPB]
                chunks = []

                def c_load():
                    for t in range(3):
                        c0 = base_ + t * 512 + hpl_ * 128
                        P.add("pool", lambda e, t=t, c0=c0: e.dma_start(out=wq[:, t], in_=w_in_v[:, :, c0:c0 + 128]),
                              **(dict(writes=[wq_b]) if t == 0 else dict(accum=[wq_b])), dma=f"wqkv{hp % NPB}")
                chunks.append(c_load)

                def mk_qk(t, dst, dst_b, gcol, g):
                    def c_qk():
                        pv, pb = rot("pj", PS_PJ)
                        for kc in range(8):
                            kw = dict(writes=[pb]) if kc == 0 else dict(accum=[pb])
                            P.add("pe", lambda e, pv=pv, kc=kc: e.matmul(pv, lhsT=wq[:, t, kc, :], rhs=uT[:, kc, g * 512:(g + 1) * 512], start=(kc == 0), stop=(kc == 7)),
                                  reads=[wq_b, uT_b], **kw)
                        sq, sq_b = rot("sq", sq_t)
                        rs, rs_b = rs_t[(ctr["sq"] - 1) % 2]
                        P.add("act", lambda e, pv=pv, sq=sq: e.activation(out=sq, in_=pv, func=AF.Square), reads=[pb], writes=[sq_b])
                        pv2, pb2 = rot("pj", PS_PJ)
                        P.add("pe", lambda e, pv2=pv2, sq=sq: e.matmul(pv2, lhsT=blk64, rhs=sq, start=True, stop=True), reads=[sq_b, blk64_b], writes=[pb2])
                        P.add("act", lambda e, pv2=pv2, rs=rs: e.activation(out=rs, in_=pv2, func=AF.Ln, bias=ccol[:, 0:1], scale=1.0),
                              reads=[pb2, ccol_b], writes=[rs_b])
                        P.add("act", lambda e, rs=rs: e.activation(out=rs, in_=rs, func=AF.Exp, scale=-0.5),
                              reads=[rs_b], writes=[rs_b])
                        P.add("dve", lambda e, pv=pv, rs=rs: e.scalar_tensor_tensor(
                            out=dst[:, g * 512:(g + 1) * 512], in0=pv, scalar=gqk[:, gcol:gcol + 1], in1=rs, op0=ALU.mult, op1=ALU.mult),
                            reads=[pb, rs_b, gqk_b], accum=[dst_b])
                    return c_qk
                for t, (dst, dst_b, gcol) in enumerate([(q_sb_, q_b_, 0 if isA_ else 2), (k_sb_, k_b_, 1 if isA_ else 3)]):
                    for g in range(4):
                        chunks.append(mk_qk(t, dst, dst_b, gcol, g))

                def mk_v(g):
                    def c_v():
                        pv, pb = rot("pj", PS_PJ)
                        for tt in range(4):
                            j = g * 4 + tt
                            for kc in range(8):
                                kw = dict(writes=[pb]) if (tt == 0 and kc == 0) else dict(accum=[pb])
                                P.add("pe", lambda e, pv=pv, kc=kc, j=j, tt=tt: e.matmul(pv[:, tt * 128:(tt + 1) * 128], lhsT=uT[:, kc, j * 128:(j + 1) * 128], rhs=wq[:, 2, kc, :], start=(kc == 0), stop=(kc == 7)),
                                      reads=[wq_b, uT_b], **kw)
                        pvv = pv.rearrange("p (t c) -> p t c", t=4)
                        P.add("act", lambda e, pvv=pvv: e.activation(out=v_sb_[:, 0, g * 4:(g + 1) * 4, 0:64], in_=pvv[:, :, 0:64], func=AF.Copy),
                              reads=[pb, v_b_], accum=[v_b_])
                        P.add("dve", lambda e, pvv=pvv: e.tensor_copy(out=v_sb_[:, 1, g * 4:(g + 1) * 4, 64:128], in_=pvv[:, :, 64:128]),
                              reads=[pb, v_b_], accum=[v_b_])
                    return c_v
                for g in range(4):
                    chunks.append(mk_v(g))
                return chunks

            for c_ in proj_chunks(0):
                c_()
            for hp in range(8):
                isA = hp < 4
                hpl = hp % 4
                q_sb, q_b = qT[hp % NPB]
                k_sb, k_b = kT[hp % NPB]
                v_sb, v_b = vpad[hp % NPB]
                next_chunks = proj_chunks(hp + 1) if hp + 1 < 8 else []
                bt = []
                for hh in range(2):
                    h = hpl * 2 + hh
                    tb, tb_b = btab[hh]
                    if isA:
                        P.add("sp", lambda e, tb=tb, h=h: e.dma_start(out=tb, in_=btabA_d[h]), writes=[tb_b], dma=f"btab{hh}")
                    else:
                        for g in range(4):
                            pv, pb = rot("pj", PS_PJ)
                            P.add("pe", lambda e, pv=pv, h=h, g=g: e.matmul(pv, lhsT=sel8[0:8, h * 128:(h + 1) * 128], rhs=csum[0:8, g * 512:(g + 1) * 512], start=True, stop=True),
                                  reads=[sel8_b, csum_b], writes=[pb])
                            P.add("act", lambda e, pv=pv, tb=tb, g=g: e.activation(out=tb[:, g * 512:(g + 1) * 512], in_=pv, func=AF.Copy),
                                  reads=[pb], **(dict(writes=[tb_b]) if g == 0 else dict(accum=[tb_b])))
                    bt.append((tb, tb_b))
                LOOK = 5
                tiles = [(qg, j, hh) for qg in range(4) for j in range(4 * qg + 4) for hh in range(2)]
                numv, numb = PS_NUM
                denv, denb = PS_DEN
                pend = {}

                def emit_score(n):
                    qg, j, hh = tiles[n]
                    h = hpl * 2 + hh
                    tb, tb_b = bt[hh]
                    sv, sb = PS_S4[n % len(PS_S4)]
                    lo, hi = hh * 64, hh * 64 + 64
                    P.add("pe", lambda e, sv=sv, lo=lo, hi=hi, j=j, qg=qg, k_sb=k_sb, q_sb=q_sb: e.matmul(
                        sv, lhsT=k_sb[lo:hi, j * 128:(j + 1) * 128], rhs=q_sb[lo:hi, qg * 512:(qg + 1) * 512], start=True, stop=True),
                        reads=[k_b, q_b], writes=[sb])
                    tm, tm_b = tmp4[n % NB4]
                    pt, pt_b = pt4[n % NB4]
                    T = 512 * qg - 128 * j
                    if isA:
                        x0 = T + 384
                        P.add("dve", lambda e, tm=tm, sv=sv, tb=tb, x0=x0: e.tensor_tensor(out=tm, in0=sv, in1=tb[:, x0:x0 + 512], op=ALU.add),
                              reads=[sb, tb_b], writes=[tm_b])
                        P.add("act", lambda e, tm=tm, pt=pt: e.activation(out=pt, in_=tm, func=AF.Exp), reads=[tm_b], writes=[pt_b])
                    else:
                        P.add("dve", lambda e, tm=tm, sv=sv, tb=tb, qg=qg: e.tensor_tensor(out=tm, in0=sv, in1=tb[:, qg * 512:(qg + 1) * 512], op=ALU.add),
                              reads=[sb, tb_b], writes=[tm_b])
                        if T <= 0:
                            x0 = T + 384
                            P.add("pool", lambda e, tm=tm, x0=x0: e.tensor_tensor(out=tm, in0=tm, in1=cbtab[:, x0:x0 + 512], op=ALU.add),
                                  reads=[tm_b, cbtab_b], writes=[tm_b])
                        P.add("act", lambda e, tm=tm, pt=pt, j=j, h=h: e.activation(out=pt, in_=tm, func=AF.Exp, bias=nck[:, j, h:h + 1]),
                              reads=[tm_b, nck_b], writes=[pt_b])
                    pend[n] = (pt, pt_b)

                def emit_pv(n):
                    qg, j, hh = tiles[n]
                    pt, pt_b = pend.pop(n)
                    first = (j == 0)
                    last = (j == 4 * qg + 3)
                    accv, accb = (numv, numb) if hh == 0 else (denv, denb)
                    P.add("pe", lambda e, pt=pt, hh=hh, j=j, first=first, last=last, v_sb=v_sb, accv=accv: e.matmul(accv, lhsT=v_sb[:, hh, j, :], rhs=pt, start=first, stop=last),
                          reads=[pt_b, v_b], **(dict(writes=[accb]) if first else dict(accum=[accb])))
                    if last:
                        rd, rd_b = rden_t[0]
                        nlo, nhi = hh * 64, hh * 64 + 64
                        dlo, dhi = (1 - hh) * 64, (1 - hh) * 64 + 64
                        P.add("dve", lambda e, rd=rd, accv=accv, nlo=nlo, nhi=nhi, dlo=dlo, dhi=dhi: e.reciprocal(out=rd[nlo:nhi, :], in_=accv[dlo:dhi, :]),
                              reads=[accb], writes=[rd_b])
                        P.add("dve", lambda e, rd=rd, qg=qg, hp=hp, accv=accv, nlo=nlo, nhi=nhi: e.tensor_tensor(
                            out=mixT[nlo:nhi, hp, qg * 512:(qg + 1) * 512], in0=accv[nlo:nhi, :], in1=rd[nlo:nhi, :], op=ALU.mult),
                              reads=[accb, rd_b], accum=[mixT_b])

                step = max(1, (len(tiles) - 8) // max(1, len(next_chunks)))
                for n in range(len(tiles) + LOOK):
                    if n < len(tiles):
                        emit_score(n)
                    if n - LOOK >= 0:
                        emit_pv(n - LOOK)
                    if next_chunks and n >= 4 and (n - 4) % step == 0:
                        next_chunks.pop(0)()
                while next_chunks:
                    next_chunks.pop(0)()
            checkpoint('A1')
            if debug:
                P.add("sp", lambda e, s=s: e.dma_start(out=dbg["mix"][s], in_=mixT.rearrange("p c t -> p (c t)")), reads=[mixT_b], dma="dbgmix")
            P.barrier()
            P.add("pool", lambda e: e.dma_start(out=wo_sb, in_=w_o_d.rearrange("(c p) n -> p c n", p=128)), writes=[wo_b], dma="wo")
            P.add("sp", lambda e: e.dma_start(out=gffn_b, in_=gvec_d[1:2, :].partition_broadcast(128)), writes=[gffn_bb], dma="gffnb")
            for i in range(16):
                ti = s * 16 + i
                r0 = tok0 + i * 128
                xt, xt_b = rot("x", xt_t)
                P.add("sp", lambda e, xt=xt, r0=r0: e.dma_start(out=xt, in_=x_d[r0:r0 + 128, :]), writes=[xt_b], dma=f"x{(ctr['x'] - 1) % 2}")
                h1, h1_b = rot("h1", h1_t)
                hi_ = (ctr["h1"] - 1) % 2
                u2f, u2f_b = u2f_t[hi_]
                u2b, u2b_b = u2b_t[hi_]
                u2T, u2T_b = u2T_t[hi_]
                st, st_b = rot("st", st_t)
                for dh in range(2):
                    pv, pb = rot("pj", PS_PJ)
                    for c in range(8):
                        kw = dict(writes=[pb]) if c == 0 else dict(accum=[pb])
                        P.add("pe", lambda e, pv=pv, c=c, i=i, dh=dh: e.matmul(pv, lhsT=mixT[:, c, i * 128:(i + 1) * 128], rhs=wo_sb[:, c, dh * 512:(dh + 1) * 512], start=(c == 0), stop=(c == 7)),
                              reads=[mixT_b, wo_b], **kw)
                    P.add("dve", lambda e, pv=pv, h1=h1, xt=xt, dh=dh: e.tensor_tensor(out=h1[:, dh * 512:(dh + 1) * 512], in0=pv, in1=xt[:, dh * 512:(dh + 1) * 512], op=ALU.add),
                          reads=[pb, xt_b], **(dict(writes=[h1_b]) if dh == 0 else dict(accum=[h1_b])))
                P.add("sp", lambda e, h1=h1, r0=r0: e.dma_start(out=h1_scr[r0:r0 + 128, :], in_=h1), reads=[h1_b], accum=[dram_h1], dma=f"h1st{hi_}")
                rms_stats(h1, h1_b, st, st_b)
                P.add("dve", lambda e, u2f=u2f, h1=h1, st=st: e.scalar_tensor_tensor(out=u2f, in0=h1, scalar=st[:, 2:3], in1=gffn_b, op0=ALU.mult, op1=ALU.mult),
                      reads=[h1_b, st_b, gffn_bb], writes=[u2f_b])
                transpose8(u2f, u2f_b, ident_f, ident_f_b)
                for half in range(2):
                    pv, pb = PS_TR[half]
                    P.add("act", lambda e, pv=pv, half=half, u2T=u2T: e.activation(out=u2T[:, half * 4:(half + 1) * 4, :], in_=pv.rearrange("p (c t) -> p c t", c=4), func=AF.Copy),
                          reads=[pb], **(dict(writes=[u2T_b]) if half == 0 else dict(accum=[u2T_b])))
                P.add("pool", lambda e, u2f=u2f, u2b=u2b: e.tensor_copy(out=u2b, in_=u2f), reads=[u2f_b], writes=[u2b_b])
                P.add("sp", lambda e, u2b=u2b, r0=r0: e.dma_start(out=u2_scr[r0:r0 + 128, :], in_=u2b), reads=[u2b_b], accum=[dram_u2], dma=f"u2st{hi_}")
                pv, pb = rot("pj", PS_PJ)
                for kc in range(8):
                    kw = dict(writes=[pb]) if kc == 0 else dict(accum=[pb])
                    P.add("pe", lambda e, pv=pv, kc=kc, u2T=u2T: e.matmul(pv[:, 0:NE], lhsT=u2T[:, kc, :], rhs=wr_sb[:, kc, :], start=(kc == 0), stop=(kc == 7)),
                          reads=[u2T_b, wr_b], **kw)
                P.add("dve", lambda e, pv=pv, ti=ti: e.tensor_tensor(out=logits_all[:, ti, :], in0=pv[:, 0:NE], in1=brb, op=ALU.add),
                      reads=[pb, brb_b], accum=[logits_b])
            P.barrier()
        try:
            for s_ in range(NSEQ):
                phase_a(s_)
        except StopBuild:
            pass
        if debug:
            P.add("sp", lambda e: e.dma_start(out=dbg["logits"], in_=logits_all.rearrange("p a b -> p (a b)")), reads=[logits_b], dma="dbglog")


        P.barrier()
        AR.reset(0)
        dram_xs = Buf("xs_scr")
        dram_y = Buf("y_scr")
        comb, comb_b = CA.alloc([NT, NE], F32, "comb")
        g4, g4_b = CA.alloc([NT, 4], F32, "g4")
        sel_i, sel_i_b = CA.alloc([4, NT], I32, "sel_i")
        idx_gu, idx_gu_b = CA.alloc([NMT], I32, "idx_gu")
        idx_dn, idx_dn_b = CA.alloc([NMT], I32, "idx_dn")
        idx_bg, idx_bg_b = CA.alloc([NMT], I32, "idx_bg")
        mx8, mx8_b = AR.alloc([NT, 8], F32, "mx8")
        rt = [AR.alloc([NE], F32, f"rt{i}") for i in range(3)]
        rs1, rs1_b = AR.alloc([NT, 4], F32, "rs1")
        mask_bf, mask_bf_b = AR.alloc([NT, NE], BF16, "mask_bf")
        runs, runs_b = AR.alloc([NT + 1, NE], F32, "runs")
        rank_all, rank_b = AR.alloc([NT, NE], F32, "rank_all")
        slotf, slotf_b = AR.alloc([NT, NE], F32, "slotf")
        eqt, eqt_b = AR.alloc([NT, NE], F32, "eqt")
        sel_f, sel_f_b = AR.alloc([4, NT], F32, "sel_f")
        sm = [AR.alloc([NE], F32, f"sm{i}") for i in range(6)]
        etf, etf_b = AR.alloc([NMT], F32, "etf")
        etc2, etc2_b = AR.alloc([NMT], F32, "etc2")
        idf, idf_b = AR.alloc([3, NMT], F32, "idf")
        d4, d4_b = AR.alloc([NT, 4], F32, "d4")
        u2ld = [AR.alloc([D], BF16, f"u2ld{i}") for i in range(4)]
        msk3, msk3_b = AR.alloc([NT, NE], F32, "msk3")
        ex3, ex3_b = AR.alloc([NT, NE], F32, "ex3")
        ssum, ssum_b = AR.alloc([NT], F32, "ssum")
        rsum, rsum_b = AR.alloc([NT], F32, "rsum")
        for ti in range(NT):
            P.add("dve", lambda e, ti=ti: e.max(out=mx8[:, ti, :], in_=logits_all[:, ti, :]), reads=[logits_b], accum=[mx8_b])
        P.add("dve", lambda e: e.tensor_tensor(out=msk3, in0=logits_all, in1=mx8[:, :, 3:4].to_broadcast([128, NT, NE]), op=ALU.is_ge),
              reads=[logits_b, mx8_b], writes=[msk3_b])
        P.add("dve", lambda e: e.tensor_copy(out=mask_bf, in_=msk3), reads=[msk3_b], writes=[mask_bf_b])
        P.add("dve", lambda e: e.tensor_tensor(out=ex3, in0=logits_all, in1=mx8[:, :, 0:1].to_broadcast([128, NT, NE]), op=ALU.subtract),
              reads=[logits_b, mx8_b], writes=[ex3_b])
        P.add("act", lambda e: e.activation(out=ex3, in_=ex3, func=AF.Exp), reads=[ex3_b], writes=[ex3_b])
        P.add("dve", lambda e: e.tensor_tensor(out=ex3, in0=ex3, in1=msk3, op=ALU.mult), reads=[ex3_b, msk3_b], writes=[ex3_b])
        P.add("dve", lambda e: e.reduce_sum(out=ssum, in_=ex3, axis=AX.X), reads=[ex3_b], writes=[ssum_b])
        P.add("dve", lambda e: e.reciprocal(out=rsum, in_=ssum), reads=[ssum_b], writes=[rsum_b])
        P.add("dve", lambda e: e.tensor_tensor(out=comb, in0=ex3, in1=rsum.unsqueeze(2).to_broadcast([128, NT, NE]), op=ALU.mult),
              reads=[ex3_b, rsum_b], writes=[comb_b])
        P.add("dve", lambda e: e.tensor_tensor(out=d4, in0=mx8[:, :, 0:4], in1=mx8[:, :, 0:1].to_broadcast([128, NT, 4]), op=ALU.subtract),
              reads=[mx8_b], writes=[d4_b])
        P.add("act", lambda e: e.activation(out=d4, in_=d4, func=AF.Exp), reads=[d4_b], writes=[d4_b])
        P.add("dve", lambda e: e.tensor_tensor(out=g4, in0=d4, in1=rsum.unsqueeze(2).to_broadcast([128, NT, 4]), op=ALU.mult),
              reads=[d4_b, rsum_b], writes=[g4_b])
        mflat = mask_bf.rearrange("p a b -> p (a b)")
        for half in range(2):
            pv, pb = psum[half]
            P.add("pe", lambda e, pv=pv, half=half: e.matmul(pv, lhsT=ustrict, rhs=mflat[:, half * 512:(half + 1) * 512], start=True, stop=True),
                  reads=[ustrict_b, mask_bf_b], writes=[pb])
            pv2, pb2 = psum[2 + half]
            P.add("pe", lambda e, pv2=pv2, half=half: e.matmul(pv2, lhsT=ones_b, rhs=mflat[:, half * 512:(half + 1) * 512], start=True, stop=True),
                  reads=[ones_b_b, mask_bf_b], writes=[pb2])
        P.add("dve", lambda e: e.memset(runs[:, 0, :], 0.0), writes=[runs_b])
        for i in range(NT):
            pv2, pb2 = psum[2 + i // 16]
            P.add("dve", lambda e, i=i, pv2=pv2: e.tensor_tensor(out=runs[:, i + 1, :], in0=runs[:, i, :], in1=pv2[:, (i % 16) * NE:(i % 16 + 1) * NE], op=ALU.add),
                  reads=[pb2, runs_b], writes=[runs_b])
        for half in range(2):
            pv, pb = psum[half]
            P.add("dve", lambda e, pv=pv, half=half: e.tensor_tensor(out=rank_all[:, half * 16:(half + 1) * 16, :], in0=pv.rearrange("p (a b) -> p a b", a=16),
                                                            in1=runs[:, half * 16:(half + 1) * 16, :], op=ALU.add),
                  reads=[pb, runs_b], **(dict(writes=[rank_b]) if half == 0 else dict(accum=[rank_b])))
        n_e = runs[:, NT, :]
        (ntl, ntl_b), (inc, inc_b), (bas, bas_b), (one32, one32_b), (cmpt, cmpt_b), _ = sm
        P.add("dve", lambda e: e.memset(ntl, 0.0), writes=[ntl_b])
        P.add("dve", lambda e: e.memset(one32, 1.0), writes=[one32_b])
        for j in range(NTOK // TS):
            P.add("dve", lambda e, j=j: e.scalar_tensor_tensor(out=ntl, in0=n_e, scalar=float(TS * j), in1=ntl, op0=ALU.is_gt, op1=ALU.add),
                  reads=[runs_b, ntl_b], writes=[ntl_b])
        P.add("dve", lambda e: e.tensor_tensor_scan(out=inc, data0=one32, data1=ntl, initial=0.0, op0=ALU.mult, op1=ALU.add),
              reads=[one32_b, ntl_b], writes=[inc_b])
        P.add("dve", lambda e: e.tensor_tensor(out=bas, in0=inc, in1=ntl, op=ALU.subtract), reads=[inc_b, ntl_b], writes=[bas_b])
        P.add("dve", lambda e: e.tensor_scalar(out=bas, in0=bas, scalar1=float(TS), scalar2=None, op0=ALU.mult), reads=[bas_b], writes=[bas_b])
        P.add("dve", lambda e: e.tensor_tensor(out=slotf, in0=rank_all, in1=bas.unsqueeze(1).to_broadcast([128, NT, NE]), op=ALU.add),
              reads=[rank_b, bas_b], writes=[slotf_b])
        for j in range(4):
            P.add("dve", lambda e, j=j: e.tensor_tensor(out=eqt, in0=logits_all, in1=mx8[:, :, j:j + 1].to_broadcast([128, NT, NE]), op=ALU.is_equal),
                  reads=[logits_b, mx8_b], writes=[eqt_b])
            P.add("dve", lambda e: e.tensor_tensor(out=eqt, in0=eqt, in1=slotf, op=ALU.mult), reads=[eqt_b, slotf_b], writes=[eqt_b])
            P.add("dve", lambda e, j=j: e.reduce_sum(out=sel_f[:, j, :], in_=eqt, axis=AX.X), reads=[eqt_b], **(dict(writes=[sel_f_b]) if j == 0 else dict(accum=[sel_f_b])))
        P.add("dve", lambda e: e.tensor_copy(out=sel_i, in_=sel_f), reads=[sel_f_b], writes=[sel_i_b])
        tvals = misc[:, 1:1 + NMT]
        P.add("dve", lambda e: e.memset(etf, 0.0), writes=[etf_b])
        for ex_i in range(NE):
            P.add("dve", lambda e, ex_i=ex_i: e.scalar_tensor_tensor(out=etf, in0=tvals, scalar=inc[:, ex_i:ex_i + 1], in1=etf, op0=ALU.is_ge, op1=ALU.add),
                  reads=[misc_b, inc_b, etf_b], writes=[etf_b])
        P.add("dve", lambda e: e.tensor_scalar(out=etc2, in0=etf, scalar1=float(NE - 1), scalar2=None, op0=ALU.min), reads=[etf_b], writes=[etc2_b])
        for r, mult in enumerate((4.0, 2.0, 1.0)):
            src, src_b = (etf, etf_b) if r == 0 else (etc2, etc2_b)
            P.add("dve", lambda e, r=r, mult=mult, src=src: e.tensor_scalar(out=idf[:, r, :], in0=src, scalar1=128.0, scalar2=misc[:, 0:1], op0=ALU.mult, op1=ALU.add),
                  reads=[src_b, misc_b], **(dict(writes=[idf_b]) if r == 0 else dict(accum=[idf_b])))
            if mult != 1.0:
                P.add("dve", lambda e, r=r, mult=mult: e.tensor_scalar(out=idf[:, r, :], in0=idf[:, r, :], scalar1=mult, scalar2=None, op0=ALU.mult),
                      reads=[idf_b], accum=[idf_b])
        P.add("dve", lambda e: e.tensor_copy(out=idx_gu, in_=idf[:, 0, :]), reads=[idf_b], writes=[idx_gu_b])
        P.add("dve", lambda e: e.tensor_copy(out=idx_dn, in_=idf[:, 1, :]), reads=[idf_b], writes=[idx_dn_b])
        P.add("dve", lambda e: e.tensor_copy(out=idx_bg, in_=idf[:, 2, :]), reads=[idf_b], writes=[idx_bg_b])
        if debug:
            P.add("sp", lambda e: e.dma_start(out=dbg["route"][:, 0:128], in_=sel_f.rearrange("p a b -> p (a b)")), reads=[sel_f_b], dma="dbgroute")
            P.add("sp", lambda e: e.dma_start(out=dbg["route"][:, 128:128 + NMT], in_=etf), reads=[etf_b], dma="dbgroute")
            P.add("sp", lambda e: e.dma_start(out=dbg["route"][:, 256:256 + NE], in_=n_e), reads=[runs_b], dma="dbgroute")
            P.add("sp", lambda e: e.dma_start(out=dbg["route"][:, 320:320 + 128], in_=sel_i.rearrange("p a b -> p (a b)").bitcast(F32)), reads=[sel_i_b], dma="dbgroute")
        for i in range(NT):
            ut, ut_b = u2ld[i % 4]
            P.add("sp", lambda e, ut=ut, i=i: e.dma_start(out=ut, in_=u2_scr[i * 128:(i + 1) * 128, :]), reads=[dram_u2], writes=[ut_b], dma=f"u2ld{i % 4}")
            for j in range(4):
                P.add("pool", lambda e, ut=ut, i=i, j=j: e.indirect_dma_start(out=xs_scr[:, :], out_offset=bass.IndirectOffsetOnAxis(ap=sel_i[:, j, i:i + 1], axis=0),
                                                                        in_=ut, in_offset=None),
                      reads=[ut_b, sel_i_b, dram_xs0], accum=[dram_xs], dma=f"scat{i % 4}")

        P.barrier()
        AR.reset(0)
        wgu_sb = [AR.alloc([8, 2048], BF16, f"wgu{i}") for i in range(2)]
        wd_sb = [AR.alloc([8, D], BF16, f"wd{i}") for i in range(2)]
        bgu_sb = [AR.alloc([16], F32, f"bgu{i}") for i in range(2)]
        xs_sb = [AR.alloc([4, D], BF16, f"xs{i}") for i in range(2)]
        xT_sb = [AR.alloc([8, TS], BF16, f"xT{i}") for i in range(2)]
        actT = [AR.alloc([8, TS], BF16, f"actT{i}") for i in range(2)]
        sw2 = [[AR.alloc([512], F32, f"sw{j}_{i}") for i in range(4)] for j in range(3)]
        yst = [AR.alloc([D], F32, f"yst{i}") for i in range(4)]
        wgu_rows = wgu_d
        wd_rows = wd_d
        xs_v = xs_scr.rearrange("(t s p) d -> t p s d", s=4, p=128)
        PS_T = [psum[0], psum[1]]
        PS_G = [psum[2], psum[3], psum[4], psum[5]]
        PS_D = [psum[6], psum[7]]
        cnt = {"d": 0, "y": 0, "ev": 0}

        _regs = {}

        def bound_reg(e, val):
            if val not in _regs:
                _regs[val] = e.to_reg(val)
            return _regs[val]

        def load_tile(t):
            wi = t % 2
            wg, wg_b = wgu_sb[wi]
            wdd, wdd_b = wd_sb[wi]
            bg, bg_b = bgu_sb[wi]
            xs, xs_b = xs_sb[wi]
            for c in range(4):
                P.add("pool", lambda e, wg=wg, t=t, c=c: e.indirect_dma_start(
                    out=wg.rearrange("p a b -> p (a b)")[:, c * 4096:(c + 1) * 4096], out_offset=None, in_=wgu_rows[:, :],
                    in_offset=bass.IndirectOffsetOnAxis(ap=idx_gu[:, t:t + 1], axis=0), element_offset=c * 4096,
                    bounds_check=bound_reg(e, NE * 128 * 4 - 1), oob_is_err=False),
                    reads=[idx_gu_b], **(dict(writes=[wg_b]) if c == 0 else dict(accum=[wg_b])), dma=f"wgu{wi}")
            for c in range(2):
                P.add("pool", lambda e, wdd=wdd, t=t, c=c: e.indirect_dma_start(
                    out=wdd.rearrange("p a b -> p (a b)")[:, c * 4096:(c + 1) * 4096], out_offset=None, in_=wd_rows[:, :],
                    in_offset=bass.IndirectOffsetOnAxis(ap=idx_dn[:, t:t + 1], axis=0), element_offset=c * 4096),
                    reads=[idx_dn_b], **(dict(writes=[wdd_b]) if c == 0 else dict(accum=[wdd_b])), dma=f"wd{wi}")
            P.add("pool", lambda e, bg=bg, t=t: e.indirect_dma_start(out=bg, out_offset=None, in_=bgu_d[:, :],
                                                                in_offset=bass.IndirectOffsetOnAxis(ap=idx_bg[:, t:t + 1], axis=0)),
                  reads=[idx_bg_b], writes=[bg_b], dma=f"bgu{wi}")
            P.add("sp", lambda e, xs=xs, t=t: e.dma_start(out=xs, in_=xs_v[t]), reads=[dram_xs, dram_xs0], writes=[xs_b], dma=f"xs{wi}")

        def transposes(t):
            xs, xs_b = xs_sb[t % 2]
            xT, xT_b = xT_sb[t % 2]
            for kc in range(8):
                pv, pb = PS_T[kc % 2]
                for sub in range(4):
                    kw = dict(writes=[pb]) if sub == 0 else dict(accum=[pb])
                    P.add("pe", lambda e, pv=pv, sub=sub, kc=kc, xs=xs: e.matmul(pv[:, sub * 128:(sub + 1) * 128], lhsT=xs[:, sub, kc * 128:(kc + 1) * 128], rhs=ident_b, start=True, stop=True),
                          reads=[xs_b, ident_b_b], **kw)
                kw = dict(writes=[xT_b]) if kc == 0 else dict(accum=[xT_b])
                if kc % 2 == 0:
                    P.add("act", lambda e, pv=pv, kc=kc, xT=xT: e.activation(out=xT[:, kc, :], in_=pv, func=AF.Copy), reads=[pb], **kw)
                else:
                    P.add("dve", lambda e, pv=pv, kc=kc, xT=xT: e.tensor_copy(out=xT[:, kc, :], in_=pv), reads=[pb], **kw)

        def gate_up(t):
            wg, wg_b = wgu_sb[t % 2]
            bg, bg_b = bgu_sb[t % 2]
            xT, xT_b = xT_sb[t % 2]
            aT, aT_b = actT[t % 2]
            for fc in range(8):
                pa, pa_b = PS_G[(fc % 2) * 2]
                pl, pl_b = PS_G[(fc % 2) * 2 + 1]
                for (pv, pb, coff) in ((pa, pa_b, 0), (pl, pl_b, 1024)):
                    for kc in range(8):
                        kw = dict(writes=[pb]) if kc == 0 else dict(accum=[pb])
                        P.add("pe", lambda e, pv=pv, kc=kc, fc=fc, coff=coff, wg=wg, xT=xT: e.matmul(
                            pv, lhsT=wg[:, kc, coff + fc * 128:coff + (fc + 1) * 128], rhs=xT[:, kc, :], start=(kc == 0), stop=(kc == 7)),
                            reads=[wg_b, xT_b], **kw)
                (xg, xg_b), (sg, sg_b), (xl, xl_b), (tt_, tt_b) = sw2[fc % 3]
                P.add("dve", lambda e, pa=pa, fc=fc, bg=bg, xg=xg: e.tensor_scalar(out=xg, in0=pa, scalar1=bg[:, fc:fc + 1], scalar2=7.0, op0=ALU.add, op1=ALU.min),
                      reads=[pa_b, bg_b], writes=[xg_b])
                P.add("act", lambda e, xg=xg, sg=sg: e.activation(out=sg, in_=xg, func=AF.Sigmoid, scale=1.702), reads=[xg_b], writes=[sg_b])
                P.add("act", lambda e, pl=pl, fc=fc, bg=bg, xl=xl: e.activation(out=xl, in_=pl, func=AF.Identity, bias=bg[:, 8 + fc:9 + fc]),
                      reads=[pl_b, bg_b], writes=[xl_b])
                P.add("dve", lambda e, xl=xl: e.tensor_scalar(out=xl, in0=xl, scalar1=7.0, scalar2=-7.0, op0=ALU.min, op1=ALU.max), reads=[xl_b], writes=[xl_b])
                P.add("dve", lambda e, xl=xl, xg=xg, tt_=tt_: e.scalar_tensor_tensor(out=tt_, in0=xl, scalar=1.0, in1=xg, op0=ALU.add, op1=ALU.mult),
                      reads=[xl_b, xg_b], writes=[tt_b])
                P.add("pool", lambda e, tt_=tt_, sg=sg, aT=aT, fc=fc: e.tensor_tensor(out=aT[:, fc, :], in0=tt_, in1=sg, op=ALU.mult),
                      reads=[tt_b, sg_b], **(dict(writes=[aT_b]) if fc == 0 else dict(accum=[aT_b])))

        def down(t):
            wdd, wdd_b = wd_sb[t % 2]
            aT, aT_b = actT[t % 2]
            for t4 in range(4):
                ys, ys_b = yst[cnt["y"] % 4]; cnt["y"] += 1
                for dh in range(2):
                    pv, pb = PS_D[cnt["d"] % 2]; cnt["d"] += 1
                    for fc in range(8):
                        kw = dict(writes=[pb]) if fc == 0 else dict(accum=[pb])
                        P.add("pe", lambda e, pv=pv, fc=fc, t4=t4, dh=dh, aT=aT, wdd=wdd: e.matmul(
                            pv, lhsT=aT[:, fc, t4 * 128:(t4 + 1) * 128], rhs=wdd[:, fc, dh * 512:(dh + 1) * 512], start=(fc == 0), stop=(fc == 7)),
                            reads=[aT_b, wdd_b], **kw)
                    kw = dict(writes=[ys_b]) if dh == 0 else dict(accum=[ys_b])
                    P.add("act", lambda e, pv=pv, ys=ys, dh=dh: e.activation(out=ys[:, dh * 512:(dh + 1) * 512], in_=pv, func=AF.Copy), reads=[pb], **kw)
                r0 = t * TS + t4 * 128
                P.add("sp", lambda e, ys=ys, r0=r0: e.dma_start(out=y_scr[r0:r0 + 128, :], in_=ys), reads=[ys_b], accum=[dram_y], dma=f"yst{(cnt['y'] - 1) % 4}")

        load_tile(0)
        transposes(0)
        for t in range(NMT):
            if t + 1 < NMT:
                load_tile(t + 1)
            gate_up(t)
            if t + 1 < NMT:
                transposes(t + 1)
            down(t)

        P.barrier()
        AR.reset(0)
        wpg_sb, wpg_b = AR.alloc([8, D], BF16, "wpg")
        P.add("pool", lambda e: e.dma_start(out=wpg_sb, in_=wpg_d.rearrange("(c p) n -> p c n", p=128)), writes=[wpg_b], dma="wpg")
        wpp_sb, wpp_b = AR.alloc([2, D], BF16, "wpp")
        P.add("pool", lambda e: e.dma_start(out=wpp_sb, in_=wpp_d.rearrange("(c p) n -> p c n", p=128)), writes=[wpp_b], dma="wpp")
        gple_b, gple_bb = AR.alloc([D], F32, "gple_b")
        P.add("sp", lambda e: e.dma_start(out=gple_b, in_=gvec_d[2:3, :].partition_broadcast(128)), writes=[gple_bb], dma="gpleb")
        bd_sb, bd_b = AR.alloc([D], F32, "bd_sb")
        P.add("sp", lambda e: e.dma_start(out=bd_sb[0:NE, :], in_=bd_d), writes=[bd_b], dma="bd")
        combT_t = [AR.alloc([128], F32, f"combT{i}") for i in range(3)]
        yg_t = [[AR.alloc([D], F32, f"yg{k}_{j}") for j in range(4)] for k in range(3)]
        h2_t = [AR.alloc([D], F32, f"h2t{i}") for i in range(3)]
        u3_t = [AR.alloc([D], BF16, f"u3{i}") for i in range(3)]
        u3T_t = [AR.alloc([8, 128], BF16, f"u3T{i}") for i in range(3)]
        gate_t = [AR.alloc([D], F32, f"gate{i}") for i in range(3)]
        pin_t = [AR.alloc([256], F32, f"pin{i}") for i in range(3)]
        pbf_t = [AR.alloc([256], BF16, f"pbf{i}") for i in range(3)]
        pT_t = [AR.alloc([2, 128], BF16, f"pT{i}") for i in range(3)]
        o_t = [AR.alloc([D], F32, f"o{i}") for i in range(3)]
        junk2, junk2_b = AR.alloc([D], BF16, "junk2")
        st2_t = [AR.alloc([4], F32, f"st2{i}") for i in range(3)]
        PS_TR2 = [psum[0], psum[1]]
        PS_GT = [psum[2], psum[3]]
        PS_BD = [psum[4], psum[5]]
        PS_PP = [psum[6], psum[7]]
        def c_stage1(ti):
            k = ti % 3
            h2, h2_b = h2_t[k]
            u3, u3_b = u3_t[k]
            u3T, u3T_b = u3T_t[k]
            gt, gt_b = gate_t[k]
            pin, pin_b = pin_t[k]
            pbf, pbf_b = pbf_t[k]
            pT, pT_b = pT_t[k]
            ot, ot_b = o_t[k]
            st, st_b = st2_t[k]
            cT, cT_b = combT_t[k]
            r0 = ti * 128
            P.add("sp", lambda e, h2=h2, r0=r0: e.dma_start(out=h2, in_=h1_scr[r0:r0 + 128, :]), reads=[dram_h1], writes=[h2_b], dma=f"h2ld{k}")
            P.add("sp", lambda e, pin=pin, r0=r0: e.dma_start(out=pin, in_=p_d[r0:r0 + 128, :]), writes=[pin_b], dma=f"pld{k}")
            for j in range(4):
                yg, yg_b = yg_t[k][j]
                P.add("pool", lambda e, yg=yg, j=j, ti=ti: e.indirect_dma_start(out=yg, out_offset=None, in_=y_scr[:, :],
                                                                        in_offset=bass.IndirectOffsetOnAxis(ap=sel_i[:, j, ti:ti + 1], axis=0)),
                      reads=[sel_i_b, dram_y], writes=[yg_b], dma=f"yg{k}_{j}")
            pv, pb = PS_BD[0]
            P.add("pe", lambda e, pv=pv, ti=ti: e.matmul(pv[0:NE, 0:128], lhsT=comb[:, ti, :], rhs=ident_f, start=True, stop=True),
                  reads=[comb_b, ident_f_b], writes=[pb])
            P.add("act", lambda e, pv=pv, cT=cT: e.activation(out=cT[0:NE, :], in_=pv[0:NE, 0:128], func=AF.Copy), reads=[pb], writes=[cT_b])
            for dh in range(2):
                pv2, pb2 = PS_BD[1] if dh == 0 else PS_BD[0]
                P.add("pe", lambda e, pv2=pv2, dh=dh, cT=cT: e.matmul(pv2, lhsT=cT[0:NE, :], rhs=bd_sb[0:NE, dh * 512:(dh + 1) * 512], start=True, stop=True),
                      reads=[cT_b, bd_b], writes=[pb2])
                P.add("dve", lambda e, pv2=pv2, dh=dh, h2=h2: e.tensor_tensor(out=h2[:, dh * 512:(dh + 1) * 512], in0=pv2, in1=h2[:, dh * 512:(dh + 1) * 512], op=ALU.add),
                      reads=[pb2, h2_b], writes=[h2_b])
            for j in range(4):
                yg, yg_b = yg_t[k][j]
                P.add("dve", lambda e, yg=yg, j=j, ti=ti, h2=h2: e.scalar_tensor_tensor(out=h2, in0=yg, scalar=g4[:, ti, j:j + 1], in1=h2, op0=ALU.mult, op1=ALU.add),
                      reads=[yg_b, g4_b, h2_b], writes=[h2_b])

        def c_stage1b(ti):
            k = ti % 3
            h2, h2_b = h2_t[k]
            u3, u3_b = u3_t[k]
            u3T, u3T_b = u3T_t[k]
            gt, gt_b = gate_t[k]
            pin, pin_b = pin_t[k]
            pbf, pbf_b = pbf_t[k]
            pT, pT_b = pT_t[k]
            ot, ot_b = o_t[k]
            st, st_b = st2_t[k]
            cT, cT_b = combT_t[k]
            r0 = ti * 128
            P.add("act", lambda e, h2=h2, st=st: e.activation(out=junk2, in_=h2, func=AF.Square, accum_out=st[:, 0:1]), reads=[h2_b], writes=[junk2_b, st_b])
            P.add("act", lambda e, st=st: e.activation(out=st[:, 1:2], in_=st[:, 0:1], func=AF.Ln, bias=ccol[:, 2:3], scale=1.0 / D), reads=[ccol_b], writes=[st_b])
            P.add("act", lambda e, st=st: e.activation(out=st[:, 2:3], in_=st[:, 1:2], func=AF.Exp, scale=-0.5), writes=[st_b])
            P.add("dve", lambda e, u3=u3, h2=h2, st=st: e.scalar_tensor_tensor(out=u3, in0=h2, scalar=st[:, 2:3], in1=gple_b, op0=ALU.mult, op1=ALU.mult),
                  reads=[h2_b, st_b, gple_bb], writes=[u3_b])
            for half in range(2):
                pv, pb = PS_TR2[half]
                for c in range(4):
                    cc = half * 4 + c
                    kw = dict(writes=[pb]) if c == 0 else dict(accum=[pb])
                    P.add("pe", lambda e, pv=pv, c=c, cc=cc, u3=u3: e.matmul(pv[:, c * 128:(c + 1) * 128], lhsT=u3[:, cc * 128:(cc + 1) * 128], rhs=ident_b, start=True, stop=True),
                          reads=[u3_b, ident_b_b], **kw)
                P.add("act", lambda e, pv=pv, half=half, u3T=u3T: e.activation(out=u3T[:, half * 4:(half + 1) * 4, :], in_=pv.rearrange("p (c t) -> p c t", c=4), func=AF.Copy),
                      reads=[pb], **(dict(writes=[u3T_b]) if half == 0 else dict(accum=[u3T_b])))
            P.add("pool", lambda e, pin=pin, pbf=pbf: e.tensor_copy(out=pbf, in_=pin), reads=[pin_b], writes=[pbf_b])
            pvp, pbp = PS_PP[0]
            for c in range(2):
                kw = dict(writes=[pbp]) if c == 0 else dict(accum=[pbp])
                P.add("pe", lambda e, c=c, pbf=pbf: e.matmul(pvp[:, c * 128:(c + 1) * 128], lhsT=pbf[:, c * 128:(c + 1) * 128], rhs=ident_b, start=True, stop=True),
                      reads=[pbf_b, ident_b_b], **kw)
            P.add("act", lambda e, pT=pT: e.activation(out=pT, in_=pvp[:, 0:256].rearrange("p (c t) -> p c t", c=2), func=AF.Copy), reads=[pbp], writes=[pT_b])

        def c_stage2(ti):
            k = ti % 3
            h2, h2_b = h2_t[k]
            u3, u3_b = u3_t[k]
            u3T, u3T_b = u3T_t[k]
            gt, gt_b = gate_t[k]
            pin, pin_b = pin_t[k]
            pbf, pbf_b = pbf_t[k]
            pT, pT_b = pT_t[k]
            ot, ot_b = o_t[k]
            st, st_b = st2_t[k]
            cT, cT_b = combT_t[k]
            r0 = ti * 128
            for dh in range(2):
                pv, pb = PS_GT[dh]
                for kc in range(8):
                    kw = dict(writes=[pb]) if kc == 0 else dict(accum=[pb])
                    P.add("pe", lambda e, pv=pv, kc=kc, dh=dh, u3T=u3T: e.matmul(pv, lhsT=u3T[:, kc, :], rhs=wpg_sb[:, kc, dh * 512:(dh + 1) * 512], start=(kc == 0), stop=(kc == 7)),
                          reads=[u3T_b, wpg_b], **kw)
                P.add("act", lambda e, pv=pv, dh=dh, gt=gt: e.activation(out=gt[:, dh * 512:(dh + 1) * 512], in_=pv, func=AF.Sigmoid),
                      reads=[pb], **(dict(writes=[gt_b]) if dh == 0 else dict(accum=[gt_b])))
            for dh in range(2):
                pv, pb = PS_PP[1] if dh == 0 else PS_PP[0]
                for c in range(2):
                    kw = dict(writes=[pb]) if c == 0 else dict(accum=[pb])
                    P.add("pe", lambda e, pv=pv, c=c, dh=dh, pT=pT: e.matmul(pv, lhsT=pT[:, c, :], rhs=wpp_sb[:, c, dh * 512:(dh + 1) * 512], start=(c == 0), stop=(c == 1)),
                          reads=[pT_b, wpp_b], **kw)
                P.add("dve", lambda e, pv=pv, dh=dh, gt=gt, ot=ot: e.tensor_tensor(out=ot[:, dh * 512:(dh + 1) * 512], in0=pv, in1=gt[:, dh * 512:(dh + 1) * 512], op=ALU.mult),
                      reads=[pb, gt_b], **(dict(writes=[ot_b]) if dh == 0 else dict(accum=[ot_b])))
            P.add("pool", lambda e, ot=ot, h2=h2: e.tensor_tensor(out=ot, in0=ot, in1=h2, op=ALU.add), reads=[ot_b, h2_b], writes=[ot_b])
            P.add("sp", lambda e, ot=ot, r0=r0: e.dma_start(out=out_d[r0:r0 + 128, :], in_=ot), reads=[ot_b], accum=[dram_out], dma=f"outst{k}")

        for it in range(NT + 2):
            if 0 <= it - 2 < NT:
                c_stage2(it - 2)
            if 0 <= it - 1 < NT:
                c_stage1b(it - 1)
            if it < NT:
                c_stage1(it)
        P.barrier()

        with nc.Block() as block:
            P.emit(block)
    return nc


def _consts():
    c = {}
    c["ident"] = np.eye(128, dtype=np.float32)
    kk = np.arange(128)[:, None]
    mm = np.arange(128)[None, :]
    c["ustrict"] = (kk < mm).astype(np.float32)
    c["blk64"] = ((kk // 64) == (mm // 64)).astype(np.float32)
    xs = np.arange(XW)[None, :]
    delta = xs - 384 - kk
    m = ((delta >= 0) & (delta <= 128)).astype(np.float64)
    m += ((delta >= 0) & (delta % 4 == 0) & (delta <= 512))
    m += ((delta >= 0) & (delta % 16 == 0) & (delta <= 2048))
    lm = np.where(m > 0, np.log(np.maximum(m, 1.0)), NEG)
    slopes = 2.0 ** (-(np.arange(8) + 1.0))
    c["btabA"] = (-(slopes[:, None, None]) * delta[None].astype(np.float64) + lm[None]).astype(np.float32)
    xs2 = np.arange(CBW)[None, :]
    d2 = xs2 - 384 - kk
    c["cbtab"] = np.where(d2 >= 0, 0.0, NEG).astype(np.float32)
    c["kpos"] = (np.arange(16)[None, :] * 128 + kk).astype(np.float32)
    sel8 = np.zeros((8, 8, 128), np.float32)
    for h in range(8):
        sel8[h, h, :] = 1.0
    c["sel8"] = sel8.reshape(8, 8 * 128)
    c["tokid"] = (np.arange(NT)[None, :] * 128 + kk).astype(np.int32)
    misc = np.zeros((128, 128), np.float32)
    misc[:, 0] = np.arange(128)
    misc[:, 1:65] = np.arange(64)[None, :]
    misc[:, 65:73] = (np.arange(8) * TS)[None, :]
    c["misc"] = misc
    return c


def _prep_inputs(x, p, g_mix, w_in, b_f, g_qa, g_ka, g_qb, g_kb, w_o, g_ffn, w_router, b_router,
                 w_gate_up, b_gate_up, w_down, b_down, g_ple, w_ple_gate, w_ple_proj):
    f = lambda a: np.ascontiguousarray(np.asarray(a, dtype=np.float32))
    x = f(x); p = f(p)
    shared = {}
    shared["w_in"] = f(w_in[0])
    shared["w_o"] = f(w_o[0])
    shared["w_router"] = f(w_router[0])
    wgu = np.asarray(w_gate_up[0], dtype=np.float32)
    wgu = np.concatenate([wgu[:, :, 0::2], wgu[:, :, 1::2]], axis=2)
    wgu = wgu.reshape(NE, 8, 128, 2048).transpose(0, 2, 1, 3)
    shared["wgu"] = np.ascontiguousarray(wgu).reshape(NE * 128 * 4, 4096)
    wd = np.asarray(w_down[0], dtype=np.float32).reshape(NE, 8, 128, D).transpose(0, 2, 1, 3)
    shared["wd"] = np.ascontiguousarray(wd).reshape(NE * 128 * 2, 4096)
    bgu = np.asarray(b_gate_up[0], dtype=np.float32)
    bgu = np.concatenate([bgu[:, 0::2], bgu[:, 1::2]], axis=1)
    shared["bgu"] = np.ascontiguousarray(bgu.reshape(NE, 16, 128).transpose(0, 2, 1)).reshape(NE * 128, 16)
    shared["b_down"] = f(b_down[0])
    shared["w_ple_gate"] = f(w_ple_gate[0])
    shared["w_ple_proj"] = f(w_ple_proj[0])
    shared["gvec"] = np.ascontiguousarray(np.stack([f(g_mix[0]), f(g_ffn[0]), f(g_ple[0])], axis=0))
    shared["gvecT"] = np.ascontiguousarray(shared["gvec"].reshape(3, 8, 128).transpose(2, 0, 1)).reshape(128, 24)
    gq = np.stack([np.tile(f(g_qa[0]), 2), np.tile(f(g_ka[0]), 2), np.tile(f(g_qb[0]), 2), np.tile(f(g_kb[0]), 2)], axis=1)
    shared["gqk"] = np.ascontiguousarray(gq)
    shared["bf"] = f(b_f[0]).reshape(8, 1)
    shared["b_router"] = f(b_router[0]).reshape(1, NE)
    shared.update(_consts())
    if STAGE not in ('', 'full'):
        shared["wgu"] = shared["wgu"][:128 * 4]
        shared["wd"] = shared["wd"][:128 * 2]
    in_maps = []
    for c in range(NCORES):
        m = dict(shared)
        m["x"] = x[c * NSEQ:(c + 1) * NSEQ].reshape(NTOK, D)
        m["p"] = p[0, c * NSEQ:(c + 1) * NSEQ].reshape(NTOK, 256)
        in_maps.append(m)
    return in_maps


_NC_CACHE = {}


def kernel(**inputs):
    in_maps = _prep_inputs(**inputs)
    if "nc" not in _NC_CACHE:
        _NC_CACHE["nc"] = build_program(DEBUG)
    nc = _NC_CACHE["nc"]
    res = run_bass_kernel_spmd(nc, in_maps, core_ids=list(range(NCORES)))
    outs = [np.asarray(r["out"]).reshape(NSEQ, SEQ, D) for r in res.results]
    if DEBUG:
        kernel.last_results = res.results
    return np.concatenate(outs, axis=0).astype(np.float32)
```
